# Optimizing a Trainium2 kernel written in Bass

```python
import jax, jax.numpy as jnp
from jax import lax
import numpy as np

D_MODEL = 1024
BATCH = 8
SEQ = 4096
DEPTH = 2

HEAD_DIM = 64
SB_HEADS = 4
DSA_HEADS = 4
NSA_HEADS = 4
BRANCH_WIDTH = 4 * HEAD_DIM
IDX_HEADS = 4
IDX_DIM = 64
DSA_TOPK = 256
CMP_LEN = 32
CMP_STRIDE = 16
CMP_HIDDEN = 128
SEL_LEN = 64
SEL_TOPN = 16
WINDOW = 512
N_MEM = 256
MEM_HEADS = 4
MEM_HEAD_DIM = 64
D_FF = ((8 * D_MODEL + 3 * 256 - 1) // (3 * 256)) * 256
Q_BLOCK = 128
ROPE_THETA = 10000.0
RMS_EPS = 1e-6
FORCED_SCORE = 1e4

IN_SPLITS = (
    ('q_a', SB_HEADS * HEAD_DIM), ('k_a', SB_HEADS * HEAD_DIM), ('v_a', SB_HEADS * HEAD_DIM),
    ('q_b', DSA_HEADS * HEAD_DIM), ('k_b', HEAD_DIM), ('v_b', HEAD_DIM),
    ('q_idx', IDX_HEADS * IDX_DIM), ('k_idx', IDX_DIM), ('w_idx', IDX_HEADS),
    ('q_c', NSA_HEADS * HEAD_DIM), ('k_cmp', HEAD_DIM), ('v_cmp', HEAD_DIM),
    ('k_sel', HEAD_DIM), ('v_sel', HEAD_DIM), ('k_win', HEAD_DIM), ('v_win', HEAD_DIM),
    ('g_c', NSA_HEADS * 3),
    ('g_merge', 3 * D_MODEL),
)
D_IN = sum(w for _, w in IN_SPLITS)

kernel_name = 'hybrid_sb_dsa_nsa_gated_block'


def _rmsnorm(x, g):
    xf = x.astype(jnp.float32)
    y = xf * lax.rsqrt(jnp.mean(xf * xf, axis=-1, keepdims=True) + RMS_EPS)
    return (y * g.astype(jnp.float32)).astype(x.dtype)


def _rope(x):
    S, hd = x.shape[1], x.shape[-1]
    inv = ROPE_THETA ** (-jnp.arange(0, hd, 2, dtype=jnp.float32) / hd)
    ang = jnp.arange(S, dtype=jnp.float32)[:, None] * inv[None, :]
    cos = jnp.cos(ang)[None, :, None, :]
    sin = jnp.sin(ang)[None, :, None, :]
    xf = x.astype(jnp.float32)
    x1, x2 = xf[..., : hd // 2], xf[..., hd // 2:]
    return jnp.concatenate([x1 * cos - x2 * sin, x2 * cos + x1 * sin], axis=-1).astype(x.dtype)


def _masked_softmax(s, mask):
    s = jnp.where(mask, s.astype(jnp.float32), -jnp.inf)
    m = jnp.max(s, axis=-1, keepdims=True)
    m = jnp.where(jnp.isfinite(m), m, 0.0)
    e = jnp.where(mask, jnp.exp(s - m), 0.0)
    den = jnp.sum(e, axis=-1, keepdims=True)
    return e / jnp.maximum(den, 1e-30)


def _sweep(block_fn, S):
    out = lax.map(block_fn, jnp.arange(S // Q_BLOCK))
    out = jnp.moveaxis(out, 0, 1)
    return out.reshape((out.shape[0], S) + out.shape[3:])


def _split_columns(p):
    parts, off = {}, 0
    for name, w in IN_SPLITS:
        parts[name] = p[..., off:off + w]
        off += w
    return parts


def _stick_breaking(q, k, v):
    B, S, H, hd = q.shape
    scale = hd ** -0.5
    kpos = jnp.arange(S)

    def block(i):
        q0 = i * Q_BLOCK
        qpos = q0 + jnp.arange(Q_BLOCK)
        qb = lax.dynamic_slice_in_dim(q, q0, Q_BLOCK, axis=1)
        z = jnp.einsum('bqhd,bkhd->bhqk', qb, k).astype(jnp.float32) * scale
        strict = kpos[None, :] < qpos[:, None]
        log_1m = jnp.where(strict, jax.nn.log_sigmoid(-z), 0.0)
        suffix = lax.cumsum(log_1m, axis=3, reverse=True) - log_1m
        w = jnp.where(strict, jnp.exp(jax.nn.log_sigmoid(z) + suffix), 0.0)
        return jnp.einsum('bhqk,bkhd->bqhd', w.astype(v.dtype), v)

    return _sweep(block, S)


def _dsa_attention(q, k, v, q_idx, k_idx, w_idx):
    B, S, H, hd = q.shape
    topk = min(DSA_TOPK, S // 4)
    kpos = jnp.arange(S)
    bidx = jnp.arange(B)[:, None, None]
    w_scale = (IDX_HEADS * IDX_DIM) ** -0.5
    scale = hd ** -0.5

    def block(i):
        q0 = i * Q_BLOCK
        qpos = q0 + jnp.arange(Q_BLOCK)
        qi = lax.dynamic_slice_in_dim(q_idx, q0, Q_BLOCK, axis=1)
        wi = lax.dynamic_slice_in_dim(w_idx, q0, Q_BLOCK, axis=1).astype(jnp.float32) * w_scale
        rel = jax.nn.relu(jnp.einsum('bqhd,bkd->bqhk', qi, k_idx).astype(jnp.float32))
        score = jnp.einsum('bqh,bqhk->bqk', wi, rel)
        score = jnp.where(kpos[None, None, :] <= qpos[None, :, None], score, -jnp.inf)
        _, sel = lax.top_k(score, topk)
        kg = k[bidx, sel]
        vg = v[bidx, sel]
        qb = lax.dynamic_slice_in_dim(q, q0, Q_BLOCK, axis=1)
        s = jnp.einsum('bqhd,bqkd->bhqk', qb, kg).astype(jnp.float32) * scale
        valid = (sel <= qpos[None, :, None])[:, None]
        p = _masked_softmax(s, valid)
        return jnp.einsum('bhqk,bqkd->bqhd', p.astype(v.dtype), vg)

    return _sweep(block, S)


def _compress(tok, pos_emb, w1, w2):
    B, S, hd = tok.shape
    n_cmp = (S - CMP_LEN) // CMP_STRIDE + 1
    idx = np.arange(n_cmp)[:, None] * CMP_STRIDE + np.arange(CMP_LEN)[None, :]
    blocks = tok[:, idx] + pos_emb
    hid = jax.nn.gelu(blocks.reshape(B, n_cmp, CMP_LEN * hd) @ w1)
    return hid @ w2


def _nsa_attention(q, k_cmp, v_cmp, k_sel, v_sel, k_win, v_win, gates,
                   cmp_pos_k, cmp_w1_k, cmp_w2_k, cmp_pos_v, cmp_w1_v, cmp_w2_v):
    B, S, H, hd = q.shape
    scale = hd ** -0.5
    kc = _compress(k_cmp, cmp_pos_k, cmp_w1_k, cmp_w2_k)
    vc = _compress(v_cmp, cmp_pos_v, cmp_w1_v, cmp_w2_v)
    n_cmp = kc.shape[1]
    cmp_end = jnp.arange(n_cmp) * CMP_STRIDE + CMP_LEN - 1
    n_sel = S // SEL_LEN
    topn = min(SEL_TOPN, n_sel)
    cs = np.arange(n_cmp) * CMP_STRIDE
    ss = np.arange(n_sel) * SEL_LEN
    overlap = jnp.asarray(((cs[:, None] < ss[None, :] + SEL_LEN) &
                           (cs[:, None] + CMP_LEN > ss[None, :])).astype(np.float32))
    k_blocks = k_sel.reshape(B, n_sel, SEL_LEN, hd)
    v_blocks = v_sel.reshape(B, n_sel, SEL_LEN, hd)
    k_pad = jnp.pad(k_win, ((0, 0), (WINDOW, 0), (0, 0)))
    v_pad = jnp.pad(v_win, ((0, 0), (WINDOW, 0), (0, 0)))
    sel_ids = jnp.arange(n_sel)
    offs = jnp.arange(SEL_LEN)
    band_offs = jnp.arange(WINDOW + Q_BLOCK)
    bidx = jnp.arange(B)[:, None, None]

    def block(i):
        q0 = i * Q_BLOCK
        qpos = q0 + jnp.arange(Q_BLOCK)
        qb = lax.dynamic_slice_in_dim(q, q0, Q_BLOCK, axis=1)
        gb = lax.dynamic_slice_in_dim(gates, q0, Q_BLOCK, axis=1)
        s_c = jnp.einsum('bqhd,bnd->bhqn', qb, kc).astype(jnp.float32) * scale
        p_c = _masked_softmax(s_c, (cmp_end[None, :] <= qpos[:, None])[None, None])
        o_cmp = jnp.einsum('bhqn,bnd->bqhd', p_c.astype(vc.dtype), vc)
        imp = jnp.einsum('bhqn,nj->bqj', p_c, overlap)
        cur = qpos // SEL_LEN
        forced = ((sel_ids[None, :] == 0) | (sel_ids[None, :] == cur[:, None]) |
                  (sel_ids[None, :] == cur[:, None] - 1))
        admissible = sel_ids[None, :] * SEL_LEN <= qpos[:, None]
        imp = jnp.where(forced[None], FORCED_SCORE, imp)
        imp = jnp.where(admissible[None], imp, -jnp.inf)
        _, sel = lax.top_k(imp, topn)
        kg = k_blocks[bidx, sel].reshape(B, Q_BLOCK, topn * SEL_LEN, hd)
        vg = v_blocks[bidx, sel].reshape(B, Q_BLOCK, topn * SEL_LEN, hd)
        tok = (sel[..., None] * SEL_LEN + offs).reshape(B, Q_BLOCK, topn * SEL_LEN)
        s_s = jnp.einsum('bqhd,bqkd->bhqk', qb, kg).astype(jnp.float32) * scale
        p_s = _masked_softmax(s_s, (tok <= qpos[None, :, None])[:, None])
        o_sel = jnp.einsum('bhqk,bqkd->bqhd', p_s.astype(vg.dtype), vg)
        kw = lax.dynamic_slice_in_dim(k_pad, q0, WINDOW + Q_BLOCK, axis=1)
        vw = lax.dynamic_slice_in_dim(v_pad, q0, WINDOW + Q_BLOCK, axis=1)
        wpos = q0 - WINDOW + band_offs
        diff = qpos[:, None] - wpos[None, :]
        wmask = (diff >= 0) & (diff < WINDOW) & (wpos[None, :] >= 0)
        s_w = jnp.einsum('bqhd,bkd->bhqk', qb, kw).astype(jnp.float32) * scale
        p_w = _masked_softmax(s_w, wmask[None, None])
        o_win = jnp.einsum('bhqk,bkd->bqhd', p_w.astype(vw.dtype), vw)
        return gb[..., 0:1] * o_cmp + gb[..., 1:2] * o_sel + gb[..., 2:3] * o_win

    return _sweep(block, S)


def _hybrid_mixer(u, w_in, cmp_pos_k, cmp_w1_k, cmp_w2_k, cmp_pos_v, cmp_w1_v, cmp_w2_v, w_up, w_out):
    B, S, _ = u.shape
    p = _split_columns(u @ w_in)

    def heads(t, h, d=HEAD_DIM):
        return t.reshape(B, S, h, d)

    def rope1(t):
        return _rope(t[:, :, None, :])[:, :, 0]

    o_a = _stick_breaking(heads(p['q_a'], SB_HEADS), heads(p['k_a'], SB_HEADS), heads(p['v_a'], SB_HEADS))
    o_b = _dsa_attention(_rope(heads(p['q_b'], DSA_HEADS)), rope1(p['k_b']), p['v_b'],
                         _rope(heads(p['q_idx'], IDX_HEADS, IDX_DIM)), rope1(p['k_idx']), p['w_idx'])
    gates_c = jax.nn.sigmoid(p['g_c'].reshape(B, S, NSA_HEADS, 3))
    o_c = _nsa_attention(_rope(heads(p['q_c'], NSA_HEADS)), rope1(p['k_cmp']), p['v_cmp'],
                         rope1(p['k_sel']), p['v_sel'], rope1(p['k_win']), p['v_win'], gates_c,
                         cmp_pos_k, cmp_w1_k, cmp_w2_k, cmp_pos_v, cmp_w1_v, cmp_w2_v)
    g = jax.nn.sigmoid(p['g_merge'].reshape(B, S, 3, D_MODEL))
    merged = (g[:, :, 0] * (o_a.reshape(B, S, BRANCH_WIDTH) @ w_up[0]) +
              g[:, :, 1] * (o_b.reshape(B, S, BRANCH_WIDTH) @ w_up[1]) +
              g[:, :, 2] * (o_c.reshape(B, S, BRANCH_WIDTH) @ w_up[2]))
    return merged @ w_out


def _memory_attention(h, mem, g_q, g_kv, w_q, w_kv, w_o):
    B, S, _ = h.shape
    M = mem.shape[1]
    q = (_rmsnorm(h, g_q) @ w_q).reshape(B, S, MEM_HEADS, MEM_HEAD_DIM)
    kv = (_rmsnorm(mem, g_kv) @ w_kv).reshape(B, M, 2, MEM_HEADS, MEM_HEAD_DIM)
    s = jnp.einsum('bshd,bmhd->bhsm', q, kv[:, :, 0]).astype(jnp.float32) * MEM_HEAD_DIM ** -0.5
    pr = jax.nn.softmax(s, axis=-1)
    o = jnp.einsum('bhsm,bmhd->bshd', pr.astype(h.dtype), kv[:, :, 1])
    return o.reshape(B, S, MEM_HEADS * MEM_HEAD_DIM) @ w_o


def _swiglu(u, w_in, w_out):
    a = u @ w_in
    return (jax.nn.silu(a[..., :D_FF]) * a[..., D_FF:]) @ w_out


def setup_inputs(seed: int = 0) -> dict:
    key = jax.random.key(seed)
    ks = jax.random.split(key, 21)

    def nrm(k, shape, fan_in):
        return jax.random.normal(k, shape, jnp.float32) * fan_in ** -0.5

    def gain(k, shape):
        return 1.0 + 0.05 * jax.random.normal(k, shape, jnp.float32)

    L = DEPTH
    mem_w = MEM_HEADS * MEM_HEAD_DIM
    return {
        'x': jax.random.normal(ks[0], (BATCH, SEQ, D_MODEL), jnp.float32),
        'mem': jax.random.normal(ks[1], (BATCH, N_MEM, D_MODEL), jnp.float32),
        'norm_mix': gain(ks[2], (L, D_MODEL)),
        'w_in': nrm(ks[3], (L, D_MODEL, D_IN), D_MODEL),
        'cmp_pos_k': 0.1 * jax.random.normal(ks[4], (L, CMP_LEN, HEAD_DIM), jnp.float32),
        'cmp_w1_k': nrm(ks[5], (L, CMP_LEN * HEAD_DIM, CMP_HIDDEN), CMP_LEN * HEAD_DIM),
        'cmp_w2_k': nrm(ks[6], (L, CMP_HIDDEN, HEAD_DIM), CMP_HIDDEN),
        'cmp_pos_v': 0.1 * jax.random.normal(ks[7], (L, CMP_LEN, HEAD_DIM), jnp.float32),
        'cmp_w1_v': nrm(ks[8], (L, CMP_LEN * HEAD_DIM, CMP_HIDDEN), CMP_LEN * HEAD_DIM),
        'cmp_w2_v': nrm(ks[9], (L, CMP_HIDDEN, HEAD_DIM), CMP_HIDDEN),
        'w_up': nrm(ks[10], (L, 3, BRANCH_WIDTH, D_MODEL), BRANCH_WIDTH),
        'w_out': nrm(ks[11], (L, D_MODEL, D_MODEL), D_MODEL),
        'norm_mem_q': gain(ks[12], (L, D_MODEL)),
        'norm_mem_kv': gain(ks[13], (L, D_MODEL)),
        'w_mem_q': nrm(ks[14], (L, D_MODEL, mem_w), D_MODEL),
        'w_mem_kv': nrm(ks[15], (L, D_MODEL, 2 * mem_w), D_MODEL),
        'w_mem_o': nrm(ks[16], (L, mem_w, D_MODEL), mem_w),
        'norm_ffn': gain(ks[17], (L, D_MODEL)),
        'w_ffn_in': nrm(ks[18], (L, D_MODEL, 2 * D_FF), D_MODEL),
        'w_ffn_out': nrm(ks[19], (L, D_FF, D_MODEL), D_FF),
        'norm_final': gain(ks[20], (D_MODEL,)),
    }


def reference(x, mem, norm_mix, w_in, cmp_pos_k, cmp_w1_k, cmp_w2_k, cmp_pos_v, cmp_w1_v, cmp_w2_v,
              w_up, w_out, norm_mem_q, norm_mem_kv, w_mem_q, w_mem_kv, w_mem_o,
              norm_ffn, w_ffn_in, w_ffn_out, norm_final):
    h = x
    for l in range(DEPTH):
        h = h + _hybrid_mixer(_rmsnorm(h, norm_mix[l]), w_in[l],
                              cmp_pos_k[l], cmp_w1_k[l], cmp_w2_k[l],
                              cmp_pos_v[l], cmp_w1_v[l], cmp_w2_v[l], w_up[l], w_out[l])
        h = h + _memory_attention(h, mem, norm_mem_q[l], norm_mem_kv[l], w_mem_q[l], w_mem_kv[l], w_mem_o[l])
        h = h + _swiglu(_rmsnorm(h, norm_ffn[l]), w_ffn_in[l], w_ffn_out[l])
    return _rmsnorm(h, norm_final)
```

```python
import numpy as np
import concourse.bass as bass
import concourse.mybir as mybir
from concourse.bass_utils import run_bass_kernel_spmd
from contextlib import ExitStack
import os

F32 = mybir.dt.float32
BF16 = mybir.dt.bfloat16
AF = mybir.ActivationFunctionType
ALU = mybir.AluOpType
AX = mybir.AxisListType

EPOCH = 30000
D = 1024
DFF = 2816
NEG = -30000.0
BIGF = 1.0e30
NMEM = 256
TOPK = 256
NA_FM = 9 * 64 + 17 * 2 * 64 + 12 + 3072
NA = NA_FM + 452


class _Rec:
    def __init__(self):
        self.calls = []

    def __getattr__(self, name):
        def f(*a, **kw):
            self.calls.append((name, a, kw))
            return self
        return f


class Prog:
    ENGS = ("pe", "act", "dve", "pool", "sp")

    def __init__(self, nc, es):
        self.nc = nc
        self.es = es
        self.ops = {e: [] for e in self.ENGS}
        self.cnt = {e: 0 for e in self.ENGS}
        self.seen = {e: {} for e in self.ENGS}
        self.sems = {}
        self.latest = {}
        self.res_w = {}
        self.res_r = {}
        self.dcnt = {}

    def sem(self, key):
        if key not in self.sems:
            self.sems[key] = self.es.enter_context(
                self.nc.semaphore("s_" + key.replace("#", "_").replace(":", "_")))
        return self.sems[key]

    def _deps(self, eng, reads, writes):
        deps = {}
        for r in reads:
            ev = self.res_w.get(r)
            if ev is not None:
                deps[ev[0]] = max(deps.get(ev[0], 0), ev[1])
        for w in writes:
            ev = self.res_w.get(w)
            if ev is not None:
                deps[ev[0]] = max(deps.get(ev[0], 0), ev[1])
            for ev in self.res_r.get(w, ()):
                deps[ev[0]] = max(deps.get(ev[0], 0), ev[1])
        out = []
        for k, v in deps.items():
            if eng == "pe" and k.startswith("pe#"):
                continue
            if self.seen[eng].get(k, 0) >= v:
                continue
            self.seen[eng][k] = v
            out.append((k, v))
        return out

    def _commit(self, ev, reads, writes):
        self.latest[ev[0]] = max(self.latest.get(ev[0], 0), ev[1])
        for w in writes:
            self.res_w[w] = ev
            self.res_r[w] = []
        for r in reads:
            if r in writes:
                continue
            lst = self.res_r.setdefault(r, [])
            lst.append(ev)
            if len(lst) > 16:
                d = {}
                for k, v in lst:
                    d[k] = max(d.get(k, 0), v)
                self.res_r[r] = list(d.items())

    def op(self, eng, fns, reads=(), writes=()):
        if callable(fns):
            fns = [fns]
        rec = _Rec()
        for f in fns:
            f(rec)
        calls = rec.calls
        px = [r for r in reads if r.startswith("pb") or r == "ptr"]
        if px:
            reads = [r for r in reads if r not in px]
            writes = list(writes) + [r for r in px if r not in writes]
        waits = self._deps(eng, reads, writes)
        c = self.cnt[eng] + 1
        self.cnt[eng] = c
        key = "%s#%d" % (eng, (c - 1) // EPOCH)
        val = (c - 1) % EPOCH + 1
        semh = self.sem(key)
        wl = [(self.sem(k), v) for k, v in waits]

        def emit(e, calls=calls, wl=wl, semh=semh):
            for s, v in wl:
                e.wait_ge(s, v)
            ins = None
            for name, a, kw in calls:
                if name == "matmul":
                    kw = dict(kw)
                    kw["skip_group_check"] = True
                ins = getattr(e, name)(*a, **kw)
            ins.then_inc(semh, 1)

        self.ops[eng].append(emit)
        ev = (key, val)
        self._commit(ev, reads, writes)
        return ev

    def dma(self, q, out, in_, reads=(), writes=(), key=None):
        waits = self._deps(q, reads, writes)
        dk = "dma:" + key
        semh = self.sem(dk)
        self.dcnt[dk] = self.dcnt.get(dk, 0) + 16
        wl = [(self.sem(k), v) for k, v in waits]

        def emit(e, wl=wl, semh=semh, out=out, in_=in_):
            for s, v in wl:
                e.wait_ge(s, v)
            e.dma_start(out=out, in_=in_).then_inc(semh, 16)

        self.ops[q].append(emit)
        ev = (dk, self.dcnt[dk])
        self._commit(ev, reads, writes)
        return ev

    def barrier(self):
        for eng in self.ENGS:
            wl = []
            for k, v in self.latest.items():
                if self.seen[eng].get(k, 0) >= v:
                    continue
                self.seen[eng][k] = v
                wl.append((self.sem(k), v))

            def emit(e, wl=wl):
                for s, v in wl:
                    e.wait_ge(s, v)

            self.ops[eng].append(emit)

    def emit_all(self):
        nc = self.nc
        ops = self.ops
        with nc.Block() as block:
            @block.tensor
            def _(e):
                for f in ops["pe"]:
                    f(e)

            @block.scalar
            def _(e):
                for f in ops["act"]:
                    f(e)

            @block.vector
            def _(e):
                for f in ops["dve"]:
                    f(e)

            @block.gpsimd
            def _(e):
                for f in ops["pool"]:
                    f(e)

            @block.sync
            def _(e):
                for f in ops["sp"]:
                    f(e)
        self.ops = {e: [] for e in self.ENGS}


OFF = {}
_o = 0
for _n, _w in (('q_a', 256), ('k_a', 256), ('v_a', 256), ('q_b', 256), ('k_b', 64), ('v_b', 64),
               ('q_idx', 256), ('k_idx', 64), ('w_idx', 4), ('q_c', 256), ('k_cmp', 64), ('v_cmp', 64),
               ('k_sel', 64), ('v_sel', 64), ('k_win', 64), ('v_win', 64), ('g_c', 12), ('g_merge', 3072)):
    OFF[_n] = _o
    _o += _w

FM_PLAIN = [('q_a', h) for h in range(4)] + [('k_a', h) for h in range(4)] + [('v_cmp', 0)]
FM_ROPE = ([('q_b', h) for h in range(4)] + [('k_b', 0)] + [('q_idx', h) for h in range(4)] + [('k_idx', 0)] +
           [('q_c', h) for h in range(4)] + [('k_cmp', 0), ('k_sel', 0), ('k_win', 0)])


def _col_index():
    cols = []
    for n, h in FM_PLAIN:
        c0 = OFF[n] + 64 * h
        cols += list(range(c0, c0 + 64))
    for n, h in FM_ROPE:
        c0 = OFF[n] + 64 * h
        cols += list(range(c0, c0 + 64))
        cols += [c0 + (d + 32) % 64 for d in range(64)]
    cols += [OFF['g_c'] + h * 3 + br for br in range(3) for h in range(4)]
    cols += list(range(OFF['g_merge'], OFF['g_merge'] + 3072))
    cols += list(range(OFF['v_a'], OFF['v_a'] + 256))
    cols += list(range(OFF['v_b'], OFF['v_b'] + 64))
    cols += list(range(OFF['v_sel'], OFF['v_sel'] + 64))
    cols += list(range(OFF['v_win'], OFF['v_win'] + 64))
    cols += list(range(OFF['w_idx'], OFF['w_idx'] + 4))
    assert len(cols) == NA
    return np.asarray(cols)


def make_consts(S):
    NB = S // 128
    c = {}
    c['c_ident4'] = np.tile(np.eye(128, dtype=np.float32), (1, 4))
    t = np.arange(128)[:, None]
    s = np.arange(128)[None, :]
    m = np.zeros((128, 5, 128), np.float32)
    m[:, 0, :] = np.where(s >= t, NEG, 0.0)
    m[:, 1, :] = np.where(s > t, NEG, 0.0)
    m[:, 2, :] = np.where(s <= t, NEG, 0.0)
    j = np.arange(128)[:, None]
    m[:, 3, :] = np.where(j >= s, -1.0, 0.0)
    m[:, 4, :] = -1.0
    c['c_masks'] = m
    c['c_tokcausal'] = np.where(s > t, -BIGF, 0.0).astype(np.float32)
    inv = 10000.0 ** (-np.arange(0, 64, 2, dtype=np.float32) / 64)
    ang = np.arange(S, dtype=np.float32)[None, :] * inv[:, None]
    cos = np.cos(ang).astype(np.float32)
    sin = np.sin(ang).astype(np.float32)
    c['c_rope'] = np.stack([np.concatenate([cos, cos], 0), np.concatenate([-sin, sin], 0)], 0)
    n_sel = S // 64
    ex = np.zeros((64, S), np.float32)
    for jj in range(min(n_sel, 64)):
        ex[jj, jj * 64:(jj + 1) * 64] = 1.0
    c['c_expand'] = ex
    n_cmp = (S - 32) // 16 + 1
    ncp = ((n_cmp + 1 + 127) // 128) * 128
    cs = np.arange(n_cmp) * 16
    ss = np.arange(n_sel) * 64
    ov = np.zeros((ncp, 64), np.float32)
    ov[:n_cmp, :n_sel] = ((cs[:, None] < ss[None, :] + 64) & (cs[:, None] + 32 > ss[None, :])).astype(np.float32)
    c['c_ov'] = ov
    tg = np.arange(S)
    n = np.arange(ncp)
    adm = (n[None, :] * 16 + 31 <= tg[:, None]) & (n[None, :] < n_cmp)
    c['c_cmpmask'] = np.where(adm, 0.0, NEG).astype(np.float32).reshape(NB, 128, ncp)
    jj = np.arange(64)
    cur = tg // 64
    forced0 = (jj[None, :] == 0)
    forced1 = (jj[None, :] == cur[:, None])
    forced2 = (jj[None, :] == cur[:, None] - 1)
    forced = forced0 | forced1 | forced2
    admis = (jj[None, :] * 64 <= tg[:, None]) & (jj[None, :] < n_sel)
    mul = np.where(forced | ~admis, 0.0, 1.0).astype(np.float32)
    add = np.zeros((S, 64), np.float32)
    add = np.where(forced0, 10000.0, add)
    add = np.where(forced2, 10001.0, add)
    add = np.where(forced1, 10002.0, add)
    add = np.where(admis, add, -BIGF).astype(np.float32)
    c['c_selmul'] = mul.reshape(NB, 128, 64)
    c['c_seladd'] = add.reshape(NB, 128, 64)
    return c


def build(S, layers, debug=False):
    NB = S // 128
    NG = S // 512
    L = len(layers)
    n_sel = S // 64
    topn = min(16, n_sel)
    topk = min(TOPK, S // 4)
    n_cmp = (S - 32) // 16 + 1
    NCP = ((n_cmp + 1 + 127) // 128) * 128
    NCC = NCP // 128
    nc = bass.Bass("TRN2", target_bir_lowering=False)

    def din(name, shape, dt=F32):
        return nc.dram_tensor(name, list(shape), dt, kind="ExternalInput")

    dbg_kind = "ExternalOutput" if debug else None

    def dscr(name, shape, dt):
        if debug:
            return nc.dram_tensor(name, list(shape), dt, kind="ExternalOutput")
        return nc.dram_tensor(name, list(shape), dt)

    x_d = din("x", [S, D])
    mem_d = din("mem", [NMEM, D])
    wA_d = din("wA", [L, D, NA])
    w1k_d = din("cmp_w1_k", [L, 2048, 128]); w2k_d = din("cmp_w2_k", [L, 128, 64]); posk_d = din("cmp_pos_k", [L, 32, 64])
    w1v_d = din("cmp_w1_v", [L, 2048, 128]); w2v_d = din("cmp_w2_v", [L, 128, 64]); posv_d = din("cmp_pos_v", [L, 32, 64])
    wup_d = din("w_up", [L, 3, 256, D]); wout_d = din("w_out", [L, D, D])
    wmq_d = din("w_mem_q", [L, D, 256]); wmkv_d = din("w_mem_kv", [L, D, 512]); wmo_d = din("w_mem_o", [L, 256, D])
    wfi_d = din("w_ffn_in", [L, D, 2 * DFF]); wfo_d = din("w_ffn_out", [L, DFF, D])
    norms_d = din("norms", [4 * L + 1, D])
    c_ident4 = din("c_ident4", [128, 512]); c_masks = din("c_masks", [128, 5, 128]); c_tokc = din("c_tokcausal", [128, 128])
    c_rope = din("c_rope", [2, 64, S]); c_expand = din("c_expand", [64, S]); c_ov = din("c_ov", [NCP, 64])
    c_cmpmask = din("c_cmpmask", [NB, 128, NCP]); c_selmul = din("c_selmul", [NB, 128, 64]); c_seladd = din("c_seladd", [NB, 128, 64])
    out_d = nc.dram_tensor("out", [S, D], F32, kind="ExternalOutput")

    QTA = dscr("QTA", [4, 64, S], BF16); KTA = dscr("KTA", [4, 64, S], BF16)
    QTB = dscr("QTB", [4, 64, S], BF16); KTB = dscr("KTB", [64, S], BF16)
    QTI = dscr("QTI", [4, 64, S], BF16); KTI = dscr("KTI", [64, S], BF16)
    QTC = dscr("QTC", [4, 64, S], BF16)
    KTCMP = dscr("KTCMP", [64, S], BF16); VTCMP = dscr("VTCMP", [64, S], BF16)
    KTSEL = dscr("KTSEL", [64, S], BF16); KTWIN = dscr("KTWIN", [64, S], BF16)
    VA = dscr("VA", [S, 256], BF16); V3 = dscr("V3", [S, 3, 64], BF16)
    WI = dscr("WI", [S, 4], F32); GC = dscr("GC", [12, S], F32); GT = dscr("GT", [3072, S], BF16)
    OT = dscr("OT", [12, 64, S], BF16)
    HM = dscr("HM", [S, D], F32); HO = dscr("HO", [S, D], F32)
    fm_dst = {}
    for n, h in FM_PLAIN + FM_ROPE:
        fm_dst[(n, h)] = {'q_a': QTA, 'k_a': KTA, 'q_b': QTB, 'q_idx': QTI, 'q_c': QTC}.get(n)
    single = {'v_cmp': VTCMP, 'k_b': KTB, 'k_idx': KTI, 'k_cmp': KTCMP, 'k_sel': KTSEL, 'k_win': KTWIN}

    def bcrow(handle, row, n=D, parts=128):
        return bass.AP(handle, row * n, [[0, parts], [1, n]])

    with ExitStack() as es:
        P = Prog(nc, es)
        sbg = lambda n, s, d: es.enter_context(nc.sbuf_tensor(n, list(s), d))
        psg = lambda n, s, d: es.enter_context(nc.psum_tensor(n, list(s), d))
        pb = [None] * 8
        ptrh = [None]

        def alloc_psum(stk, tag, nfp):
            for i_ in range(nfp):
                pb[i_] = stk.enter_context(nc.psum_tensor("pb%d_%s" % (i_, tag), [128, 512], F32))
            if nfp < 8:
                ptrh[0] = stk.enter_context(nc.psum_tensor("ptr_%s" % tag, [128, 1024], BF16))
        ident = sbg("ident", [128, 512], BF16)
        masks = sbg("masks", [128, 5, 128], BF16)
        ones32 = sbg("ones32", [128, 64], F32)
        stat = sbg("stat", [128, 16], F32)
        P.dma("pool", ident[:], c_ident4.ap(), writes=["ident"], key="c0")
        P.dma("pool", masks[:], c_masks.ap(), writes=["masks"], key="c1")
        P.op("pool", lambda e: e.memset(ones32[:], 1.0), writes=["ones32"])

        def rmsnorm_T(src, src_res, g_bc, g_res, dstT, dst_res, tmp):
            junk, ubf = tmp
            P.op("act", lambda e: e.activation(out=junk[:], in_=src, func=AF.Square, accum_out=stat[:, 0:1]),
                 reads=[src_res], writes=["junk", "stat0"])
            P.op("act", lambda e: e.activation(out=stat[:, 1:2], in_=stat[:, 0:1], func=AF.Sqrt, scale=1.0 / D, bias=1e-6),
                 reads=["stat0"], writes=["stat1"])
            P.op("dve", lambda e: e.reciprocal(out=stat[:, 2:3], in_=stat[:, 1:2]), reads=["stat1"], writes=["stat2"])
            P.op("dve", lambda e: e.scalar_tensor_tensor(out=ubf[:], in0=src, scalar=stat[:, 2:3], in1=g_bc[:],
                                                          op0=ALU.mult, op1=ALU.mult),
                 reads=[src_res, "stat2", g_res], writes=["ubf"])
            P.op("pe", [(lambda e, kc=kc: e.transpose(out=ptrh[0][:, kc * 128:(kc + 1) * 128], in_=ubf[:, kc * 128:(kc + 1) * 128],
                                                      identity=ident[:, 0:128])) for kc in range(8)],
                 reads=["ubf", "ident"], writes=["ptr"])
            P.op("act", lambda e: e.activation(out=dstT, in_=ptrh[0][:].rearrange("p (k t) -> p k t", k=8), func=AF.Copy),
                 reads=["ptr"], writes=[dst_res])

        def attn_finish(obank, obres, dst, dst_res, work, gate=None, guard=False, extra_reads=(), bcbank=6):
            rrow, bcs, accum = work
            P.op("act", lambda e: e.activation(out=rrow[64:65, :], in_=obank[64:65, :], func=AF.Ln), reads=[obres], writes=["rrow"])
            P.op("act", lambda e: e.activation(out=rrow[64:65, :], in_=rrow[64:65, :], func=AF.Exp, scale=-1.0), reads=["rrow"], writes=["rrow"])
            if gate is not None:
                gap, gres = gate
                P.op("dve", lambda e: e.tensor_tensor(out=rrow[64:65, :], in0=rrow[64:65, :], in1=gap, op=ALU.mult),
                     reads=["rrow", gres], writes=["rrow"])
            P.op("pe", lambda e: e.matmul(pb[bcbank][0:64, :], lhsT=ones32[64:65, 0:64], rhs=rrow[64:65, :], start=True, stop=True),
                 reads=["rrow", "ones32"], writes=["pb%d" % bcbank])
            P.op("act", lambda e: e.activation(out=bcs[0:64, :], in_=pb[bcbank][0:64, :], func=AF.Copy), reads=["pb%d" % bcbank], writes=["bcs"])
            if accum is None:
                P.op("dve", lambda e: e.tensor_tensor(out=dst, in0=obank[0:64, :], in1=bcs[0:64, :], op=ALU.mult),
                     reads=[obres, "bcs"] + list(extra_reads), writes=[dst_res])
            else:
                acc, first = accum
                if first:
                    P.op("dve", lambda e: e.tensor_tensor(out=acc[0:64, :], in0=obank[0:64, :], in1=bcs[0:64, :], op=ALU.mult),
                         reads=[obres, "bcs"], writes=["oacc"])
                else:
                    P.op("dve", lambda e: e.tensor_tensor(out=bcs[0:64, :], in0=obank[0:64, :], in1=bcs[0:64, :], op=ALU.mult),
                         reads=[obres, "bcs"], writes=["bcs"])
                    P.op("pool", lambda e: e.tensor_tensor(out=acc[0:64, :], in0=acc[0:64, :], in1=bcs[0:64, :], op=ALU.add),
                         reads=["bcs", "oacc"], writes=["oacc"])

        for li, l in enumerate(layers):
            h_in = x_d if li == 0 else HO
            last = (li == L - 1)
            with ExitStack() as ea:
                sb = lambda n, s, d: ea.enter_context(nc.sbuf_tensor(n + "_L%d" % li, list(s), d))
                alloc_psum(ea, "A%d" % li, 7)
                wA = sb("wA_sb", [128, 8, NA], BF16)
                gA = sb("gA", [128, D], F32)
                rope = sb("rope", [64, 2, S], F32)
                hb = [sb("hbA%d" % i, [128, D], F32) for i in range(2)]
                junk = sb("junkA", [128, D], F32)
                ubf = sb("ubfA", [128, D], BF16)
                uT = sb("uTA", [128, 8, 512], BF16)
                stg = [sb("stgA%d" % i, [128, 512], BF16) for i in range(4)]
                stgf = sb("stgfA", [128, 512], F32)
                t1 = sb("ropeT1", [64, 512], F32)
                t2 = sb("ropeT2", [64, 512], F32)
                for kc in range(8):
                    P.dma("pool", wA[:, kc, :], wA_d.ap()[l, kc * 128:(kc + 1) * 128, :], writes=["wA%d" % kc], key="wA%d" % kc)
                wA_res = ["wA%d" % kc for kc in range(8)]
                P.dma("sp", gA[:], bcrow(norms_d, 4 * l + 0), writes=["gA"], key="gA")
                P.dma("sp", rope[:], c_rope.ap().rearrange("c d s -> d c s"), writes=["rope"], key="rope")
                sctr = [0]

                def stage():
                    i = sctr[0] % 4
                    sctr[0] += 1
                    return stg[i], "stgA%d" % i

                def proj_fm(bank, c0, M):
                    P.op("pe", [(lambda e, kc=kc: e.matmul(pb[bank][0:M, :], lhsT=wA[:, kc, c0:c0 + M], rhs=uT[:, kc, :],
                                                            start=(kc == 0), stop=(kc == 7))) for kc in range(8)],
                         reads=["uT"] + wA_res, writes=["pb%d" % bank])

                LIM = int(os.environ.get("LIMA", "99"))
                for g in range(NG if LIM > 0 else 0):
                    gs = slice(g * 512, (g + 1) * 512)
                    for b4 in range(4):
                        blk = g * 4 + b4
                        hbt = hb[blk % 2]
                        P.dma("sp", hbt[:], h_in.ap()[blk * 128:(blk + 1) * 128, :], writes=["hb%d" % (blk % 2)], key="hb%d" % (blk % 2))
                        rmsnorm_T(hbt[:], "hb%d" % (blk % 2), gA, "gA", uT[:, :, b4 * 128:(b4 + 1) * 128], "uT", (junk, ubf))
                    col = 0
                    bank = 0
                    if LIM <= 1:
                        continue
                    for (n, h) in FM_PLAIN:
                        proj_fm(bank, col, 64)
                        st, sr = stage()
                        sc = 0.125 if n == 'q_a' else 1.0
                        P.op("act", lambda e, bank=bank, st=st, sc=sc: e.activation(out=st[0:64, :], in_=pb[bank][0:64, :], func=AF.Copy, scale=sc),
                             reads=["pb%d" % bank], writes=[sr])
                        dst = fm_dst[(n, h)].ap()[h, :, gs] if fm_dst[(n, h)] is not None else single[n].ap()[:, gs]
                        P.dma("sp", dst, st[0:64, :], reads=[sr], writes=[], key=sr)
                        col += 64
                        bank = (bank + 1) % 6
                    if LIM <= 2:
                        continue
                    for (n, h) in FM_ROPE:
                        b0 = bank
                        b1 = (bank + 1) % 6
                        proj_fm(b0, col, 64)
                        proj_fm(b1, col + 64, 64)
                        st, sr = stage()
                        P.op("dve", lambda e, b0=b0: e.tensor_tensor(out=t1[:], in0=pb[b0][0:64, :], in1=rope[:, 0, gs], op=ALU.mult),
                             reads=["pb%d" % b0, "rope"], writes=["t1"])
                        P.op("dve", lambda e, b1=b1: e.tensor_tensor(out=t2[:], in0=pb[b1][0:64, :], in1=rope[:, 1, gs], op=ALU.mult),
                             reads=["pb%d" % b1, "rope"], writes=["t2"])
                        P.op("pool", lambda e, st=st: e.tensor_tensor(out=st[0:64, :], in0=t1[:], in1=t2[:], op=ALU.add),
                             reads=["t1", "t2"], writes=[sr])
                        dst = fm_dst[(n, h)].ap()[h, :, gs] if fm_dst[(n, h)] is not None else single[n].ap()[:, gs]
                        P.dma("sp", dst, st[0:64, :], reads=[sr], writes=[], key=sr)
                        col += 128
                        bank = (bank + 2) % 6
                    if LIM <= 3:
                        continue
                    proj_fm(bank, col, 12)
                    P.op("act", lambda e, bank=bank: e.activation(out=stgf[0:12, :], in_=pb[bank][0:12, :], func=AF.Sigmoid),
                         reads=["pb%d" % bank], writes=["stgf"])
                    P.dma("sp", GC.ap()[:, gs], stgf[0:12, :], reads=["stgf"], writes=[], key="stgf")
                    col += 12
                    bank = (bank + 1) % 6
                    for gg in range(24):
                        proj_fm(bank, col, 128)
                        st, sr = stage()
                        P.op("act", lambda e, bank=bank, st=st: e.activation(out=st[:], in_=pb[bank][:], func=AF.Sigmoid),
                             reads=["pb%d" % bank], writes=[sr])
                        P.dma("sp", GT.ap()[gg * 128:(gg + 1) * 128, gs], st[:], reads=[sr], writes=[], key=sr)
                        col += 128
                        bank = (bank + 1) % 6
                    assert col == NA_FM
                    if LIM <= 4:
                        continue
                    for b4 in range(4):
                        blk = g * 4 + b4
                        P.op("pe", [(lambda e, kc=kc, b4=b4, bank=bank: e.matmul(pb[bank][:, 0:452], lhsT=uT[:, kc, b4 * 128:(b4 + 1) * 128],
                                                                                  rhs=wA[:, kc, NA_FM:NA], start=(kc == 0), stop=(kc == 7)))
                                    for kc in range(8)], reads=["uT"] + wA_res, writes=["pb%d" % bank])
                        st, sr = stage()
                        P.op("act", lambda e, bank=bank, st=st: e.activation(out=st[:, 0:448], in_=pb[bank][:, 0:448], func=AF.Copy),
                             reads=["pb%d" % bank], writes=[sr])
                        P.op("dve", lambda e, bank=bank: e.tensor_scalar(out=stgf[:, 0:4], in0=pb[bank][:, 448:452], scalar1=1.0 / 16, scalar2=None, op0=ALU.mult),
                             reads=["pb%d" % bank], writes=["stgf"])
                        P.dma("sp", VA.ap()[blk * 128:(blk + 1) * 128, :], st[:, 0:256], reads=[sr], writes=[], key=sr)
                        P.dma("sp", V3.ap()[blk * 128:(blk + 1) * 128, :, :], st[:, 256:448].rearrange("p (a d) -> p a d", a=3), reads=[sr], writes=[], key=sr)
                        P.dma("sp", WI.ap()[blk * 128:(blk + 1) * 128, :], stgf[:, 0:4], reads=["stgf"], writes=[], key="stgf")
                        bank = (bank + 1) % 6
                P.barrier()
                P.emit_all()
            if debug == "A":
                break
            with ExitStack() as eb:
                sb = lambda n, s, d: eb.enter_context(nc.sbuf_tensor(n + "_L%d" % li, list(s), d))
                alloc_psum(eb, "B%d" % li, 8)
                KTAs = sb("KTAs", [64, 4, S], BF16); VAs = sb("VAs", [128, NB, 256], BF16)
                KTBs = sb("KTBs", [64, S], BF16); KTIs = sb("KTIs", [64, S], BF16)
                KTSs = sb("KTSs", [64, S], BF16); KTWs = sb("KTWs", [64, S], BF16)
                V3s = [sb("V3s%d" % j, [128, NB, 65], BF16) for j in range(3)]
                EXs = sb("EXs", [64, S], BF16)
                KCT = sb("KCT", [64, NCP], BF16); VCs = sb("VCs", [128, NCC, 65], BF16); OVs = sb("OVs", [128, NCC, 64], BF16)
                tokc = sb("tokc", [128, 128], F32)
                score = sb("score", [128, S], F32); junkb = sb("junkb", [128, S], BF16); mneg = [sb("mneg%d" % i_, [128, S], BF16) for i_ in range(2)]
                QA = [sb("QA%d" % i, [64, 512], BF16) for i in range(2)]
                QB = [sb("QB%d" % i, [64, 512], BF16) for i in range(2)]
                QI = [sb("QI%d" % i, [64, 512], BF16) for i in range(2)]
                QC = [sb("QC%d" % i, [64, 512], BF16) for i in range(2)]
                wi = [sb("wi%d" % i, [128, 4], F32) for i in range(2)]
                grow = [sb("grow%d" % i_, [128, 3, 512], F32) for i_ in range(2)]
                cmk = [sb("cmk%d" % i, [128, NCP], BF16) for i in range(2)]
                smul = [sb("smul%d" % i, [128, 64], F32) for i in range(2)]
                sadd = [sb("sadd%d" % i, [128, 64], F32) for i in range(2)]
                e1 = [sb("e1_%d" % i, [128, 512], F32) for i in range(2)]
                sp_ = [sb("sp_%d" % i, [128, 512], BF16) for i in range(2)]
                asb = [sb("asb%d" % i, [128, 512], F32) for i in range(2)]
                Wt = [sb("Wt%d" % i, [128, 512], BF16) for i in range(2)]
                csum = sb("csum", [128, 512], F32)
                Eb = [sb("Eb%d" % i, [128, 512], BF16) for i in range(3)]
                tmpi = [sb("tmpi%d" % i_, [128, 512], F32) for i_ in range(2)]
                rrow = sb("rrow", [128, 512], F32); bcs = sb("bcs", [64, 512], F32); oacc = sb("oacc", [64, 512], F32)
                ost = [sb("ost%d" % i, [64, 3, 512], BF16) for i in range(2)]
                bis = sb("bis", [128, 8], F32)
                impT = sb("impT", [64, 128], F32); uimp = sb("uimp", [64, 512], F32)
                imp = sb("imp", [128, 64], F32); imp2 = sb("imp2", [128, 64], F32); m8 = sb("m8", [128, 16], F32)
                msel = sb("msel", [128, 64], F32); mselT = sb("mselT", [64, 512], BF16)
                identf = sb("identf", [128, 128], F32)
                w2 = sb("w2", [128, 64], BF16); posT = sb("posT", [64, 32], BF16)
                w1 = (mneg[0][0:64, 0:4096] if S >= 4096 else sb("w1x", [64, 4096], BF16)[:, :]).rearrange("d (a h) -> d a h", a=32)
                TCs = junkb[0:64, :]
                hid = sb("hid", [128, NCP], BF16); bcol = sb("bcol", [128, 1], F32)

                P.dma("sp", KTAs[:], KTA.ap().rearrange("h d s -> d h s"), reads=["scrA"], writes=["KTAs"], key="ldB0")
                P.dma("sp", VAs[:], VA.ap().rearrange("(b p) c -> p b c", p=128), reads=["scrA"], writes=["VAs"], key="ldB1")
                P.dma("sp", KTBs[:], KTB.ap(), reads=["scrA"], writes=["KTBs"], key="ldB2")
                P.dma("sp", KTIs[:], KTI.ap(), reads=["scrA"], writes=["KTIs"], key="ldB3")
                P.dma("sp", KTSs[:], KTSEL.ap(), reads=["scrA"], writes=["KTSs"], key="ldB4")
                P.dma("sp", KTWs[:], KTWIN.ap(), reads=["scrA"], writes=["KTWs"], key="ldB5")
                for j in range(3):
                    P.op("pool", lambda e, j=j: e.memset(V3s[j][:], 1.0), writes=["V3s%d" % j])
                    P.dma("sp", V3s[j][:, :, 0:64], V3.ap()[:, j, :].rearrange("(b p) d -> p b d", p=128), reads=["scrA"], writes=["V3s%d" % j], key="ldB6%d" % j)
                P.dma("pool", EXs[:], c_expand.ap(), writes=["EXs"], key="ldB7")
                P.dma("pool", OVs[:], c_ov.ap().rearrange("(c p) j -> p c j", p=128), writes=["OVs"], key="ldB8")
                P.dma("sp", tokc[:], c_tokc.ap(), writes=["tokc"], key="ldB9")
                P.dma("sp", identf[:], c_ident4.ap()[:, 0:128], writes=["identf"], key="ldB10")
                P.op("pool", lambda e: e.memset(VCs[:], 1.0), writes=["VCs"])
                for which in range(2 if int(os.environ.get("LIMB", "99")) > 0 else 0):
                    w1_d, w2_d, pos_d, src = ((w1k_d, w2k_d, posk_d, KTCMP), (w1v_d, w2v_d, posv_d, VTCMP))[which]
                    P.dma("pool", w1, w1_d.ap()[l].rearrange("(a d) h -> d a h", d=64), writes=["mneg0"], key="cw1")
                    P.dma("pool", w2[:], w2_d.ap()[l], writes=["w2"], key="cw2")
                    for a in range(32):
                        P.dma("pool", posT[:, a:a + 1], pos_d.ap()[l, a:a + 1, :].rearrange("a d -> d a"), writes=["posT"], key="cpos")
                    P.dma("sp", TCs, src.ap(), reads=["scrA"], writes=["junkb"], key="cTC")
                    P.op("pe", [(lambda e, a=a: e.matmul(pb[1][:, 0:1], lhsT=w1[:, a, :], rhs=posT[:, a:a + 1], start=(a == 0), stop=(a == 31)))
                                for a in range(32)], reads=["mneg0", "posT"], writes=["pb1"])
                    P.op("act", lambda e: e.activation(out=bcol[:], in_=pb[1][:, 0:1], func=AF.Copy), reads=["pb1"], writes=["bcol"])
                    P.op("pe", [(lambda e, a=a: e.matmul(pb[0][:, 0:n_cmp], lhsT=w1[:, a, :], rhs=TCs[:, a:a + 16 * (n_cmp - 1) + 1:16],
                                                         start=(a == 0), stop=(a == 31))) for a in range(32)],
                         reads=["mneg0", "junkb"], writes=["pb0"])
                    P.op("pool", lambda e: e.memset(hid[:], 0.0), writes=["hid"])
                    P.op("act", lambda e: e.activation(out=hid[:, 0:n_cmp], in_=pb[0][:, 0:n_cmp], func=AF.Gelu_apprx_tanh, bias=bcol[:, 0:1]),
                         reads=["pb0", "bcol"], writes=["hid"])
                    if which == 0:
                        P.op("pe", lambda e: e.matmul(pb[2][0:64, 0:NCP], lhsT=w2[:], rhs=hid[:], start=True, stop=True),
                             reads=["w2", "hid"], writes=["pb2"])
                        P.op("act", lambda e: e.activation(out=KCT[:], in_=pb[2][0:64, 0:NCP], func=AF.Copy), reads=["pb2"], writes=["KCT"])
                    else:
                        for c in range(NCC):
                            P.op("pe", lambda e, c=c: e.matmul(pb[2][:, 0:64], lhsT=hid[:, c * 128:(c + 1) * 128], rhs=w2[:], start=True, stop=True),
                                 reads=["w2", "hid"], writes=["pb2"])
                            P.op("act", lambda e, c=c: e.activation(out=VCs[:, c, 0:64], in_=pb[2][:, 0:64], func=AF.Copy), reads=["pb2"], writes=["VCs"])

                ectr = [0]
                tctr = [0]
                LIMB = int(os.environ.get("LIMB", "99"))

                def load_block(i):
                    pi = i % 2
                    ts = slice(i * 128, (i + 1) * 128)
                    for (qt, qn, src) in ((QI, "QI", QTI), (QA, "QA", QTA), (QB, "QB", QTB), (QC, "QC", QTC)):
                        P.dma("sp", qt[pi][:].rearrange("d (h t) -> d h t", h=4), src.ap()[:, :, ts].rearrange("h d t -> d h t"),
                              reads=[], writes=["%s%d" % (qn, pi)], key="%s%d" % (qn, pi))
                    P.dma("sp", wi[pi][:], WI.ap()[ts, :], writes=["wi%d" % pi], key="wi%d" % pi)
                    P.dma("sp", grow[pi][64:65, :, :].rearrange("o b (h t) -> o b h t", h=4),
                          GC.ap()[:, ts].rearrange("(o b h) t -> o b h t", o=1, b=3), writes=["grow%d" % pi], key="grow%d" % pi)
                    P.dma("pool", cmk[pi][:], c_cmpmask.ap()[i], writes=["cmk%d" % pi], key="cmk%d" % pi)
                    P.dma("sp", smul[pi][:], c_selmul.ap()[i], writes=["smul%d" % pi], key="smul%d" % pi)
                    P.dma("sp", sadd[pi][:], c_seladd.ap()[i], writes=["sadd%d" % pi], key="sadd%d" % pi)

                def chain_P(i):
                    U = []
                    pi = i % 2
                    Lk = (i + 1) * 128
                    qi = QI[pi]
                    qir = "QI%d" % pi
                    mn = mneg[pi]
                    mres = "mneg%d" % pi
                    wres = "wi%d" % pi
                    nch = (Lk + 511) // 512
                    for c in range(nch):
                        w_ = min(512, Lk - c * 512)
                        cs = slice(c * 512, c * 512 + w_)
                        for h in range(4):
                            def u(c=c, h=h, w_=w_, cs=cs):
                                P.op("pe", lambda e: e.matmul(pb[7][:, 0:w_], lhsT=qi[:, h * 128:(h + 1) * 128], rhs=KTIs[:, cs], start=True, stop=True),
                                     reads=[qir, "KTIs"], writes=["pb7"])
                                if h == 0:
                                    P.op("dve", lambda e: e.tensor_scalar(out=score[:, cs], in0=pb[7][:, 0:w_], scalar1=0.0, scalar2=wi[pi][:, 0:1], op0=ALU.max, op1=ALU.mult),
                                         reads=["pb7", wres], writes=["score"])
                                else:
                                    tb = tctr[0] % 2
                                    tctr[0] += 1
                                    P.op("dve", lambda e: e.tensor_scalar(out=tmpi[tb][:, 0:w_], in0=pb[7][:, 0:w_], scalar1=0.0, scalar2=wi[pi][:, h:h + 1], op0=ALU.max, op1=ALU.mult),
                                         reads=["pb7", wres], writes=["tmpi%d" % tb])
                                    P.op("pool", lambda e: e.tensor_tensor(out=score[:, cs], in0=score[:, cs], in1=tmpi[tb][:, 0:w_], op=ALU.add),
                                         reads=["tmpi%d" % tb, "score"], writes=["score"])
                            U.append(u)

                    def u_pre():
                        if Lk > topk:
                            P.op("dve", lambda e: e.tensor_reduce(out=bis[:, 0:1], in_=score[:, 0:Lk], axis=AX.X, op=ALU.min), reads=["score"], writes=["bis"])
                            P.op("dve", lambda e: e.tensor_reduce(out=bis[:, 1:2], in_=score[:, 0:Lk], axis=AX.X, op=ALU.max), reads=["score"], writes=["bis"])
                            P.op("dve", lambda e: e.tensor_tensor(out=bis[:, 1:2], in0=bis[:, 1:2], in1=bis[:, 0:1], op=ALU.subtract), reads=["bis"], writes=["bis"])
                        P.op("pool", lambda e: e.tensor_tensor(out=score[:, i * 128:(i + 1) * 128], in0=score[:, i * 128:(i + 1) * 128], in1=tokc[:], op=ALU.add),
                             reads=["score", "tokc"], writes=["score"])
                    U.append(u_pre)
                    if Lk > topk:
                        def u_it():
                            P.op("dve", lambda e: e.tensor_scalar(out=bis[:, 1:2], in0=bis[:, 1:2], scalar1=0.5, scalar2=None, op0=ALU.mult), reads=["bis"], writes=["bis"])
                            P.op("dve", lambda e: e.tensor_tensor(out=bis[:, 2:3], in0=bis[:, 0:1], in1=bis[:, 1:2], op=ALU.add), reads=["bis"], writes=["bis"])
                            P.op("dve", lambda e: e.tensor_scalar(out=junkb[:, 0:Lk], in0=score[:, 0:Lk], scalar1=bis[:, 2:3], scalar2=None, op0=ALU.is_ge, op1=ALU.add,
                                                                  accum_out=bis[:, 3:4]), reads=["score", "bis"], writes=["junkb", "bis"])
                            P.op("dve", lambda e: e.tensor_scalar(out=bis[:, 4:5], in0=bis[:, 3:4], scalar1=float(topk) - 0.5, scalar2=None, op0=ALU.is_ge), reads=["bis"], writes=["bis"])
                            P.op("dve", lambda e: e.scalar_tensor_tensor(out=bis[:, 0:1], in0=bis[:, 4:5], scalar=bis[:, 1:2], in1=bis[:, 0:1], op0=ALU.mult, op1=ALU.add),
                                 reads=["bis"], writes=["bis"])
                        for it in range(NBIS):
                            U.append(u_it)

                        def u_fin():
                            P.op("dve", lambda e: e.tensor_scalar(out=mn[:, 0:Lk], in0=score[:, 0:Lk], scalar1=bis[:, 0:1], scalar2=NEG, op0=ALU.is_lt, op1=ALU.mult),
                                 reads=["score", "bis"], writes=[mres])
                    else:
                        def u_fin():
                            P.op("dve", lambda e: e.tensor_scalar(out=mn[:, 0:Lk], in0=score[:, 0:Lk], scalar1=-1.0e29, scalar2=NEG, op0=ALU.is_lt, op1=ALU.mult),
                                 reads=["score"], writes=[mres])
                    U.append(u_fin)
                    return U

                def chain_SB(i):
                    U = []
                    pi = i % 2
                    qa = QA[pi]
                    qar = "QA%d" % pi
                    o_st = ost[pi]
                    ostr = "ost%d" % pi
                    kbs = list(range(i, -1, -1))
                    n = len(kbs)

                    def z_ops(kb, bank, start_first):
                        fns = []
                        first = start_first
                        if kb == i:
                            fns.append(lambda e, first=first: e.matmul(pb[bank][:], lhsT=masks[:, 0, :], rhs=ident[:], start=first, stop=False))
                            first = False
                        for h in range(4):
                            fns.append(lambda e, h=h, first=first: e.matmul(
                                pb[bank][:, h * 128:(h + 1) * 128], lhsT=KTAs[:, h, kb * 128:(kb + 1) * 128], rhs=qa[:, h * 128:(h + 1) * 128],
                                start=first, stop=(h == 3)))
                            first = False
                        return fns

                    def stage1(j):
                        kb = kbs[j]
                        bk = j % 2
                        P.op("pe", z_ops(kb, 0, True), reads=["KTAs", qar, "masks", "ident"], writes=["pb0"])
                        P.op("act", lambda e: e.activation(out=e1[bk][:], in_=pb[0][:], func=AF.Exp), reads=["pb0"], writes=["e1_%d" % bk])
                        P.op("act", lambda e: e.activation(out=sp_[bk][:], in_=e1[bk][:], func=AF.Ln, bias=1.0), reads=["e1_%d" % bk], writes=["sp_%d" % bk])

                    def stage2(j):
                        kb = kbs[j]
                        bk = j % 2
                        fns = [lambda e: e.matmul(pb[1][:], lhsT=masks[:, 3, :], rhs=sp_[bk][:], start=True, stop=False)] + z_ops(kb, 1, False)
                        rds = ["sp_%d" % bk, "masks", "KTAs", qar, "ident"]
                        if j > 0:
                            P.op("act", lambda e: e.activation(out=csum[:], in_=pb[2][:], func=AF.Copy), reads=["pb2"], writes=["csum"])
                            fns.append(lambda e: e.matmul(pb[1][:], lhsT=identf[:, :], rhs=csum[:], start=False, stop=True))
                            rds += ["identf", "csum"]
                        P.op("pe", fns, reads=rds, writes=["pb1"])
                        P.op("pe", lambda e: e.matmul(pb[2][:], lhsT=masks[:, 4, :], rhs=sp_[bk][:], start=(j == 0), stop=(j == n - 1)),
                             reads=["sp_%d" % bk, "masks"], writes=["pb2"])
                        P.op("act", lambda e: e.activation(out=Wt[bk][:], in_=pb[1][:], func=AF.Exp), reads=["pb1"], writes=["Wt%d" % bk])
                        P.op("pe", [(lambda e, h=h: e.matmul(pb[3][0:64, h * 128:(h + 1) * 128], lhsT=VAs[:, kb, h * 64:(h + 1) * 64],
                                                             rhs=Wt[bk][:, h * 128:(h + 1) * 128], start=(j == 0 and h == 0), stop=(j == n - 1)))
                                    for h in range(4)], reads=["Wt%d" % bk, "VAs"], writes=["pb3"])

                    def u0():
                        stage1(0)
                    U.append(u0)
                    for j in range(n):
                        def u(j=j):
                            if j + 1 < n:
                                stage1(j + 1)
                            stage2(j)
                        U.append(u)

                    def ufin():
                        P.op("act", lambda e: e.activation(out=o_st[:, 0, :], in_=pb[3][0:64, :], func=AF.Copy), reads=["pb3"], writes=[ostr])
                    U.append(ufin)
                    return U

                def attn_units(U, nsteps, s_ops, v_ap, v_res, scale=0.125):
                    def emitS(j):
                        bk = 4 + j % 2
                        fns, rd = s_ops(j, bk)
                        P.op("pe", fns, reads=rd, writes=["pb%d" % bk])

                    def unit(j):
                        if j == 0:
                            emitS(0)
                        if j + 1 < nsteps:
                            emitS(j + 1)
                        bk = 4 + j % 2
                        eb_i = ectr[0] % 3
                        ectr[0] += 1
                        E = Eb[eb_i]
                        P.op("act", lambda e: e.activation(out=E[:], in_=pb[bk][:], func=AF.Exp, scale=scale),
                             reads=["pb%d" % bk], writes=["Eb%d" % eb_i])
                        P.op("pe", lambda e: e.matmul(pb[6][0:65, :], lhsT=v_ap(j), rhs=E[:], start=(j == 0), stop=(j == nsteps - 1)),
                             reads=["Eb%d" % eb_i, v_res], writes=["pb6"])
                    for j in range(nsteps):
                        U.append((lambda j=j: unit(j)))

                def chain_ATT(i):
                    U = []
                    pi = i % 2
                    ts = slice(i * 128, (i + 1) * 128)
                    qb, qc = QB[pi], QC[pi]
                    qbr, qcr = "QB%d" % pi, "QC%d" % pi
                    o_st = ost[pi]
                    ostr = "ost%d" % pi
                    mn = mneg[pi]
                    mres = "mneg%d" % pi
                    gres = "grow%d" % pi

                    def dsa_s(j, bk):
                        return ([lambda e: e.matmul(pb[bk][:], lhsT=mn[:, j * 128:(j + 1) * 128], rhs=ident[:], start=True, stop=False),
                                 lambda e: e.matmul(pb[bk][:], lhsT=KTBs[:, j * 128:(j + 1) * 128], rhs=qb[:], start=False, stop=True)],
                                [mres, "ident", "KTBs", qbr])
                    attn_units(U, i + 1, dsa_s, lambda j: V3s[0][:, j, :], "V3s0")
                    U.append(lambda: attn_finish(pb[6], "pb6", o_st[:, 1, :], ostr, (rrow, bcs, None), bcbank=4))

                    def u_cmp():
                        for j in range(NCC):
                            P.op("pe", [lambda e: e.matmul(pb[4][:], lhsT=cmk[pi][:, j * 128:(j + 1) * 128], rhs=ident[:], start=True, stop=False),
                                        lambda e: e.matmul(pb[4][:], lhsT=KCT[:, j * 128:(j + 1) * 128], rhs=qc[:], start=False, stop=True)],
                                 reads=["cmk%d" % pi, "ident", "KCT", qcr], writes=["pb4"])
                            eb_i = ectr[0] % 3
                            ectr[0] += 1
                            E = Eb[eb_i]
                            P.op("act", lambda e: e.activation(out=E[:], in_=pb[4][:], func=AF.Exp, scale=0.125), reads=["pb4"], writes=["Eb%d" % eb_i])
                            P.op("pe", lambda e: e.matmul(pb[6][0:65, :], lhsT=VCs[:, j, :], rhs=E[:], start=(j == 0), stop=(j == NCC - 1)),
                                 reads=["Eb%d" % eb_i, "VCs"], writes=["pb6"])
                            P.op("pe", lambda e: e.matmul(pb[5][0:64, :], lhsT=OVs[:, j, :], rhs=E[:], start=(j == 0), stop=(j == NCC - 1)),
                                 reads=["Eb%d" % eb_i, "OVs"], writes=["pb5"])
                    U.append(u_cmp)

                    def u_imp():
                        P.op("dve", lambda e: e.tensor_scalar(out=rrow[64:65, :], in0=pb[6][64:65, :], scalar1=1e-30, scalar2=None, op0=ALU.max), reads=["pb6"], writes=["rrow"])
                        P.op("act", lambda e: e.activation(out=rrow[64:65, :], in_=rrow[64:65, :], func=AF.Ln), reads=["rrow"], writes=["rrow"])
                        P.op("act", lambda e: e.activation(out=rrow[64:65, :], in_=rrow[64:65, :], func=AF.Exp, scale=-1.0), reads=["rrow"], writes=["rrow"])
                        P.op("pe", lambda e: e.matmul(pb[4][0:64, :], lhsT=ones32[64:65, 0:64], rhs=rrow[64:65, :], start=True, stop=True), reads=["rrow", "ones32"], writes=["pb4"])
                        P.op("act", lambda e: e.activation(out=bcs[0:64, :], in_=pb[4][0:64, :], func=AF.Copy), reads=["pb4"], writes=["bcs"])
                        P.op("dve", lambda e: e.tensor_tensor(out=uimp[:], in0=pb[5][0:64, :], in1=bcs[0:64, :], op=ALU.mult), reads=["pb5", "bcs"], writes=["uimp"])
                        P.op("dve", lambda e: e.tensor_tensor(out=impT[:], in0=uimp[:, 0:128], in1=uimp[:, 128:256], op=ALU.add), reads=["uimp"], writes=["impT"])
                        P.op("dve", lambda e: e.tensor_tensor(out=impT[:], in0=impT[:], in1=uimp[:, 256:384], op=ALU.add), reads=["uimp", "impT"], writes=["impT"])
                        P.op("dve", lambda e: e.tensor_tensor(out=impT[:], in0=impT[:], in1=uimp[:, 384:512], op=ALU.add), reads=["uimp", "impT"], writes=["impT"])
                        P.op("dve", lambda e: e.tensor_tensor(out=rrow[64:65, :], in0=rrow[64:65, :], in1=grow[pi][64:65, 0, :], op=ALU.mult), reads=["rrow", gres], writes=["rrow"])
                        P.op("pe", lambda e: e.matmul(pb[4][0:64, :], lhsT=ones32[64:65, 0:64], rhs=rrow[64:65, :], start=True, stop=True), reads=["rrow", "ones32"], writes=["pb4"])
                        P.op("act", lambda e: e.activation(out=bcs[0:64, :], in_=pb[4][0:64, :], func=AF.Copy), reads=["pb4"], writes=["bcs"])
                        P.op("dve", lambda e: e.tensor_tensor(out=oacc[:], in0=pb[6][0:64, :], in1=bcs[0:64, :], op=ALU.mult), reads=["pb6", "bcs"], writes=["oacc"])
                    U.append(u_imp)

                    def u_top():
                        P.op("pe", lambda e: e.matmul(pb[4][:, 0:64], lhsT=impT[:], rhs=identf[0:64, 0:64], start=True, stop=True), reads=["impT", "identf"], writes=["pb4"])
                        P.op("dve", lambda e: e.tensor_tensor(out=imp[:], in0=pb[4][:, 0:64], in1=smul[pi][:], op=ALU.mult), reads=["pb4", "smul%d" % pi], writes=["imp"])
                        P.op("dve", lambda e: e.tensor_tensor(out=imp[:], in0=imp[:], in1=sadd[pi][:], op=ALU.add), reads=["imp", "sadd%d" % pi], writes=["imp"])
                        P.op("dve", lambda e: e.max(out=m8[:, 0:8], in_=imp[:]), reads=["imp"], writes=["m8"])
                        if topn > 8:
                            P.op("dve", lambda e: e.match_replace(out=imp2[:], in_to_replace=m8[:, 0:8], in_values=imp[:], imm_value=-BIGF), reads=["imp", "m8"], writes=["imp2"])
                            P.op("dve", lambda e: e.max(out=m8[:, 8:16], in_=imp2[:]), reads=["imp2"], writes=["m8"])
                            lo_, hi_ = 8, 8 + (topn - 8)
                        else:
                            lo_, hi_ = 0, topn
                        P.op("dve", lambda e: e.tensor_reduce(out=bis[:, 5:6], in_=m8[:, lo_:hi_], axis=AX.X, op=ALU.min), reads=["m8"], writes=["bis5"])
                        P.op("dve", lambda e: e.tensor_scalar(out=bis[:, 5:6], in0=bis[:, 5:6], scalar1=-1.0e29, scalar2=None, op0=ALU.max), reads=["bis5"], writes=["bis5"])
                        P.op("dve", lambda e: e.tensor_scalar(out=msel[:], in0=imp[:], scalar1=bis[:, 5:6], scalar2=NEG, op0=ALU.is_lt, op1=ALU.mult), reads=["imp", "bis5"], writes=["msel"])
                        P.op("pe", lambda e: e.matmul(pb[4][0:64, 0:128], lhsT=msel[:], rhs=identf[:, :], start=True, stop=True), reads=["msel", "identf"], writes=["pb4"])
                        P.op("act", [(lambda e, h=h: e.activation(out=mselT[:, h * 128:(h + 1) * 128], in_=pb[4][0:64, 0:128], func=AF.Copy)) for h in range(4)],
                             reads=["pb4"], writes=["mselT"])
                    U.append(u_top)

                    def sel_s(j, bk):
                        fns = [lambda e: e.matmul(pb[bk][:], lhsT=EXs[:, j * 128:(j + 1) * 128], rhs=mselT[:], start=True, stop=False)]
                        if j == i:
                            fns.append(lambda e: e.matmul(pb[bk][:], lhsT=masks[:, 1, :], rhs=ident[:], start=False, stop=False))
                        fns.append(lambda e: e.matmul(pb[bk][:], lhsT=KTSs[:, j * 128:(j + 1) * 128], rhs=qc[:], start=False, stop=True))
                        return fns, ["EXs", "mselT", "masks", "ident", "KTSs", qcr]
                    attn_units(U, i + 1, sel_s, lambda j: V3s[1][:, j, :], "V3s1")
                    U.append(lambda: attn_finish(pb[6], "pb6", None, None, (rrow, bcs, (oacc, False)), gate=(grow[pi][64:65, 1, :], gres), bcbank=4))
                    wk0 = max(0, i - 4)

                    def win_s(j, bk):
                        kb = wk0 + j
                        fns = []
                        first = True
                        if kb == i:
                            fns.append(lambda e: e.matmul(pb[bk][:], lhsT=masks[:, 1, :], rhs=ident[:], start=True, stop=False))
                            first = False
                        if kb == i - 4:
                            fns.append(lambda e, first=first: e.matmul(pb[bk][:], lhsT=masks[:, 2, :], rhs=ident[:], start=first, stop=False))
                            first = False
                        fns.append(lambda e, first=first: e.matmul(pb[bk][:], lhsT=KTWs[:, kb * 128:(kb + 1) * 128], rhs=qc[:], start=first, stop=True))
                        return fns, ["masks", "ident", "KTWs", qcr]
                    attn_units(U, i - wk0 + 1, win_s, lambda j: V3s[2][:, wk0 + j, :], "V3s2")

                    def u_end():
                        attn_finish(pb[6], "pb6", None, None, (rrow, bcs, (oacc, False)), gate=(grow[pi][64:65, 2, :], gres), bcbank=4)
                        P.op("act", lambda e: e.activation(out=o_st[:, 2, :], in_=oacc[:], func=AF.Copy), reads=["oacc"], writes=[ostr])
                    U.append(u_end)
                    return U

                def store_o(i):
                    pi = i % 2
                    ts = slice(i * 128, (i + 1) * 128)
                    P.dma("sp", OT.ap()[:, :, ts].rearrange("(b h) d t -> d b h t", b=3), ost[pi][:].rearrange("d b (h t) -> d b h t", h=4),
                          reads=["ost%d" % pi], writes=[], key="ost%d" % pi)

                def run_merged(chains):
                    items = []
                    for ci, U in enumerate(chains):
                        n = len(U)
                        for k, f in enumerate(U):
                            items.append(((k + 0.5) / n, ci, k, f))
                    items.sort(key=lambda t: (t[0], t[1], t[2]))
                    for _, _, _, f in items:
                        f()

                load_block(0)
                run_merged([chain_P(0)])
                for i in range(NB):
                    if i + 1 < NB:
                        load_block(i + 1)
                    chains = [chain_SB(i), chain_ATT(i)]
                    if i + 1 < NB:
                        chains.append(chain_P(i + 1))
                    run_merged(chains)
                    store_o(i)
                P.barrier()
                P.emit_all()
            if debug == "B":
                break
            with ExitStack() as ec:
                sb = lambda n, s, d: ec.enter_context(nc.sbuf_tensor(n + "_L%d" % li, list(s), d))
                alloc_psum(ec, "C%d" % li, 7)
                wup = sb("wup", [64, 12, D], BF16); wout = sb("wout", [128, 8, D], BF16)
                wmq = sb("wmq", [128, 8, 256], BF16); wmo = sb("wmo", [64, 4, D], BF16); wmkv = sb("wmkv", [128, 8, 512], BF16)
                gq = sb("gq", [128, D], F32); gkv = sb("gkv", [128, D], F32)
                KmT = sb("KmT", [64, 4, NMEM], BF16); Vm = sb("Vm", [128, 2, 4, 65], BF16)
                oT = sb("oT", [64, 12, 512], BF16); gT = sb("gT", [128, 24, 512], BF16)
                mT = sb("mT", [128, 8, 512], BF16)
                tm = [sb("tm%d" % i, [128, 512], F32) for i in range(3)]
                hin = [sb("hin%d" % i, [128, D], F32) for i in range(2)]
                h1 = sb("h1", [128, 4, D], F32)
                junk = sb("junkC", [128, D], F32); ubf = sb("ubfC", [128, D], BF16)
                uT2 = sb("uT2", [128, 8, 512], BF16)
                qmT = sb("qmT", [64, 4, 512], BF16)
                Eb = [sb("EbC%d" % i, [128, 512], BF16) for i in range(2)]
                omT = sb("omT", [64, 4, 512], BF16)
                rrow = sb("rrowC", [128, 512], F32); bcs = sb("bcsC", [64, 512], F32)
                hout = [sb("houtC%d" % i, [128, D], F32) for i in range(2)]
                memb = sb("memb", [128, D], F32); mTm = sb("mTm", [128, 8, NMEM], BF16)
                P.dma("pool", wup[:], wup_d.ap()[l].rearrange("b (h d) n -> d (b h) n", d=64), writes=["wup"], key="c1w0")
                P.dma("pool", wout[:], wout_d.ap()[l].rearrange("(k p) n -> p k n", p=128), writes=["wout"], key="c1w1")
                P.dma("pool", wmq[:], wmq_d.ap()[l].rearrange("(k p) n -> p k n", p=128), writes=["wmq"], key="c1w2")
                P.dma("pool", wmo[:], wmo_d.ap()[l].rearrange("(h d) n -> d h n", d=64), writes=["wmo"], key="c1w3")
                P.dma("pool", wmkv[:], wmkv_d.ap()[l].rearrange("(k p) n -> p k n", p=128), writes=["wmkv"], key="c1w4")
                P.dma("sp", gq[:], bcrow(norms_d, 4 * l + 1), writes=["gq"], key="c1g0")
                P.dma("sp", gkv[:], bcrow(norms_d, 4 * l + 2), writes=["gkv"], key="c1g1")
                P.op("pool", lambda e: e.memset(Vm[:], 1.0), writes=["Vm"])
                for mc in range(2):
                    P.dma("sp", memb[:], mem_d.ap()[mc * 128:(mc + 1) * 128, :], writes=["memb"], key="c1mem")
                    rmsnorm_T(memb[:], "memb", gkv, "gkv", mTm[:, :, mc * 128:(mc + 1) * 128], "mTm", (junk, ubf))
                for h in range(4):
                    P.op("pe", [(lambda e, kc=kc, h=h: e.matmul(pb[0][0:64, 0:NMEM], lhsT=wmkv[:, kc, h * 64:(h + 1) * 64], rhs=mTm[:, kc, :],
                                                                start=(kc == 0), stop=(kc == 7))) for kc in range(8)], reads=["wmkv", "mTm"], writes=["pb0"])
                    P.op("act", lambda e, h=h: e.activation(out=KmT[:, h, :], in_=pb[0][0:64, 0:NMEM], func=AF.Copy), reads=["pb0"], writes=["KmT"])
                for mc in range(2):
                    P.op("pe", [(lambda e, kc=kc, mc=mc: e.matmul(pb[1][:, 0:256], lhsT=mTm[:, kc, mc * 128:(mc + 1) * 128], rhs=wmkv[:, kc, 256:512],
                                                                  start=(kc == 0), stop=(kc == 7))) for kc in range(8)], reads=["wmkv", "mTm"], writes=["pb1"])
                    P.op("act", lambda e, mc=mc: e.activation(out=Vm[:, mc, :, 0:64], in_=pb[1][:, 0:256].rearrange("p (h d) -> p h d", h=4), func=AF.Copy),
                         reads=["pb1"], writes=["Vm"])
                for g in range(NG):
                    gs = slice(g * 512, (g + 1) * 512)
                    P.dma("sp", oT[:], OT.ap()[:, :, gs].rearrange("c d s -> d c s"), reads=["scrB"], writes=["oT"], key="c1oT")
                    P.dma("sp", gT[:], GT.ap()[:, gs].rearrange("(c p) s -> p c s", p=128), reads=["scrA"], writes=["gT"], key="c1gT")
                    for fc in range(8):
                        for br in range(3):
                            P.op("pe", [(lambda e, h=h, br=br, fc=fc: e.matmul(pb[br][:], lhsT=wup[:, br * 4 + h, fc * 128:(fc + 1) * 128], rhs=oT[:, br * 4 + h, :],
                                                                                start=(h == 0), stop=(h == 3))) for h in range(4)], reads=["wup", "oT"], writes=["pb%d" % br])
                            P.op("dve", lambda e, br=br, fc=fc: e.tensor_tensor(out=tm[br][:], in0=pb[br][:], in1=gT[:, br * 8 + fc, :], op=ALU.mult),
                                 reads=["pb%d" % br, "gT"], writes=["tm%d" % br])
                        P.op("pool", lambda e: e.tensor_tensor(out=tm[0][:], in0=tm[0][:], in1=tm[1][:], op=ALU.add), reads=["tm0", "tm1"], writes=["tm0"])
                        P.op("pool", lambda e, fc=fc: e.tensor_tensor(out=mT[:, fc, :], in0=tm[0][:], in1=tm[2][:], op=ALU.add), reads=["tm0", "tm2"], writes=["mT"])
                    for b4 in range(4):
                        blk = g * 4 + b4
                        hi_ = hin[blk % 2]
                        P.dma("sp", hi_[:], h_in.ap()[blk * 128:(blk + 1) * 128, :], writes=["hin%d" % (blk % 2)], key="hin%d" % (blk % 2))
                        for half in range(2):
                            bk = 3 + half
                            P.op("pe", [(lambda e, kc=kc, half=half, bk=bk, b4=b4: e.matmul(pb[bk][:], lhsT=mT[:, kc, b4 * 128:(b4 + 1) * 128], rhs=wout[:, kc, half * 512:(half + 1) * 512],
                                                                                          start=(kc == 0), stop=(kc == 7))) for kc in range(8)], reads=["mT", "wout"], writes=["pb%d" % bk])
                            P.op("dve", lambda e, half=half, bk=bk, b4=b4, hi_=hi_: e.tensor_tensor(out=h1[:, b4, half * 512:(half + 1) * 512], in0=pb[bk][:], in1=hi_[:, half * 512:(half + 1) * 512], op=ALU.add),
                                 reads=["pb%d" % bk, "hin%d" % (blk % 2)], writes=["h1_%d" % b4])
                        rmsnorm_T(h1[:, b4, :], "h1_%d" % b4, gq, "gq", uT2[:, :, b4 * 128:(b4 + 1) * 128], "uT2", (junk, ubf))
                    for h in range(4):
                        bk = h % 2
                        P.op("pe", [(lambda e, kc=kc, h=h, bk=bk: e.matmul(pb[bk][0:64, :], lhsT=wmq[:, kc, h * 64:(h + 1) * 64], rhs=uT2[:, kc, :], start=(kc == 0), stop=(kc == 7)))
                                    for kc in range(8)], reads=["wmq", "uT2"], writes=["pb%d" % bk])
                        P.op("act", lambda e, h=h, bk=bk: e.activation(out=qmT[:, h, :], in_=pb[bk][0:64, :], func=AF.Copy), reads=["pb%d" % bk], writes=["qmT"])
                    for h in range(4):
                        for mc in range(2):
                            bk = mc
                            P.op("pe", lambda e, h=h, mc=mc, bk=bk: e.matmul(pb[bk][:], lhsT=KmT[:, h, mc * 128:(mc + 1) * 128], rhs=qmT[:, h, :], start=True, stop=True),
                                 reads=["KmT", "qmT"], writes=["pb%d" % bk])
                            P.op("act", lambda e, mc=mc, bk=bk: e.activation(out=Eb[mc][:], in_=pb[bk][:], func=AF.Exp, scale=0.125), reads=["pb%d" % bk], writes=["EbC%d" % mc])
                            P.op("pe", lambda e, h=h, mc=mc: e.matmul(pb[5][0:65, :], lhsT=Vm[:, mc, h, :], rhs=Eb[mc][:], start=(mc == 0), stop=(mc == 1)),
                                 reads=["Vm", "EbC%d" % mc], writes=["pb5"])
                        attn_finish(pb[5], "pb5", omT[:, h, :], "omT", (rrow, bcs, None))
                    for b4 in range(4):
                        blk = g * 4 + b4
                        ho = hout[blk % 2]
                        for half in range(2):
                            bk = 3 + half
                            P.op("pe", [(lambda e, h=h, half=half, bk=bk, b4=b4: e.matmul(pb[bk][:], lhsT=omT[:, h, b4 * 128:(b4 + 1) * 128], rhs=wmo[:, h, half * 512:(half + 1) * 512],
                                                                                        start=(h == 0), stop=(h == 3))) for h in range(4)], reads=["omT", "wmo"], writes=["pb%d" % bk])
                            P.op("dve", lambda e, half=half, bk=bk, b4=b4, ho=ho: e.tensor_tensor(out=ho[:, half * 512:(half + 1) * 512], in0=pb[bk][:], in1=h1[:, b4, half * 512:(half + 1) * 512], op=ALU.add),
                                 reads=["pb%d" % bk, "h1_%d" % b4], writes=["hout%d" % (blk % 2)])
                        P.dma("sp", HM.ap()[blk * 128:(blk + 1) * 128, :], ho[:], reads=["hout%d" % (blk % 2)], writes=[], key="hout%d" % (blk % 2))
                P.barrier()
                P.emit_all()
            if debug == "C1":
                break
            with ExitStack() as ef:
                sb = lambda n, s, d: ef.enter_context(nc.sbuf_tensor(n + "_L%d" % li, list(s), d))
                alloc_psum(ef, "F%d" % li, 7)
                wfi = sb("wfi", [128, 8, 2 * DFF], BF16); wfo = sb("wfo", [128, 22, D], BF16)
                gf = sb("gf", [128, D], F32); gfin = sb("gfin", [128, D], F32)
                hmb = sb("hmb", [128, 2, D], F32)
                junk = sb("junkF", [128, D], F32); ubf = sb("ubfF", [128, D], BF16)
                uT3 = sb("uT3", [128, 8, 256], BF16)
                sil = [sb("sil%d" % i, [128, 256], F32) for i in range(2)]
                actT = sb("actT", [128, 22, 256], BF16)
                hf = [sb("hf%d" % i, [128, D], F32) for i in range(2)]
                for kc in range(8):
                    P.dma("pool", wfi[:, kc, :], wfi_d.ap()[l, kc * 128:(kc + 1) * 128, :], writes=["wfi"], key="c2w%d" % kc)
                P.dma("pool", wfo[:, 0:11, :], wfo_d.ap()[l, 0:11 * 128, :].rearrange("(c p) n -> p c n", p=128), writes=["wfo"], key="c2wo0")
                P.dma("pool", wfo[:, 11:22, :], wfo_d.ap()[l, 11 * 128:22 * 128, :].rearrange("(c p) n -> p c n", p=128), writes=["wfo"], key="c2wo1")
                P.dma("sp", gf[:], bcrow(norms_d, 4 * l + 3), writes=["gf"], key="c2g0")
                P.dma("sp", gfin[:], bcrow(norms_d, 4 * L), writes=["gfin"], key="c2g1")
                for g2 in range(S // 256):
                    for b2 in range(2):
                        blk = g2 * 2 + b2
                        P.dma("sp", hmb[:, b2, :], HM.ap()[blk * 128:(blk + 1) * 128, :], reads=["scrC"], writes=["hmb%d" % b2], key="hmb%d" % b2)
                        rmsnorm_T(hmb[:, b2, :], "hmb%d" % b2, gf, "gf", uT3[:, :, b2 * 128:(b2 + 1) * 128], "uT3", (junk, ubf))
                    for c in range(22):
                        bk = c % 2
                        P.op("pe", [(lambda e, kc=kc, c=c, bk=bk: e.matmul(pb[bk][:, 0:256], lhsT=wfi[:, kc, c * 128:(c + 1) * 128], rhs=uT3[:, kc, :], start=(kc == 0), stop=(kc == 7)))
                                    for kc in range(8)] +
                                   [(lambda e, kc=kc, c=c, bk=bk: e.matmul(pb[bk][:, 256:512], lhsT=wfi[:, kc, DFF + c * 128:DFF + (c + 1) * 128], rhs=uT3[:, kc, :], start=False, stop=(kc == 7)))
                                    for kc in range(8)], reads=["wfi", "uT3"], writes=["pb%d" % bk])
                        P.op("act", lambda e, bk=bk: e.activation(out=sil[bk][:], in_=pb[bk][:, 0:256], func=AF.Silu), reads=["pb%d" % bk], writes=["sil%d" % bk])
                        P.op("dve", lambda e, bk=bk, c=c: e.tensor_tensor(out=actT[:, c, :], in0=pb[bk][:, 256:512], in1=sil[bk][:], op=ALU.mult),
                             reads=["pb%d" % bk, "sil%d" % bk], writes=["actT%d" % c])
                        for b2 in range(2):
                            for half in range(2):
                                ob = 2 + b2 * 2 + half
                                P.op("pe", lambda e, c=c, b2=b2, half=half, ob=ob: e.matmul(pb[ob][:], lhsT=actT[:, c, b2 * 128:(b2 + 1) * 128], rhs=wfo[:, c, half * 512:(half + 1) * 512],
                                                                                           start=(c == 0), stop=(c == 21)), reads=["actT%d" % c, "wfo"], writes=["pb%d" % ob])
                    for b2 in range(2):
                        blk = g2 * 2 + b2
                        hft = hf[b2]
                        for half in range(2):
                            ob = 2 + b2 * 2 + half
                            P.op("dve", lambda e, ob=ob, half=half, b2=b2, hft=hft: e.tensor_tensor(out=hft[:, half * 512:(half + 1) * 512], in0=pb[ob][:], in1=hmb[:, b2, half * 512:(half + 1) * 512], op=ALU.add),
                                 reads=["pb%d" % ob, "hmb%d" % b2], writes=["hf%d" % b2])
                        if not last:
                            P.dma("sp", HO.ap()[blk * 128:(blk + 1) * 128, :], hft[:], reads=["hf%d" % b2], writes=[], key="hf%d" % b2)
                        else:
                            P.op("act", lambda e, hft=hft: e.activation(out=junk[:], in_=hft[:], func=AF.Square, accum_out=stat[:, 4:5]), reads=["hf%d" % b2], writes=["junk", "stat4"])
                            P.op("act", lambda e: e.activation(out=stat[:, 5:6], in_=stat[:, 4:5], func=AF.Sqrt, scale=1.0 / D, bias=1e-6), reads=["stat4"], writes=["stat5"])
                            P.op("dve", lambda e: e.reciprocal(out=stat[:, 6:7], in_=stat[:, 5:6]), reads=["stat5"], writes=["stat6"])
                            P.op("dve", lambda e, hft=hft: e.scalar_tensor_tensor(out=hft[:], in0=hft[:], scalar=stat[:, 6:7], in1=gfin[:], op0=ALU.mult, op1=ALU.mult),
                                 reads=["hf%d" % b2, "stat6", "gfin"], writes=["hf%d" % b2])
                            P.dma("sp", out_d.ap()[blk * 128:(blk + 1) * 128, :], hft[:], reads=["hf%d" % b2], writes=[], key="hf%d" % b2)
                P.barrier()
                P.emit_all()
    return nc


NBIS = 16

_CACHE = {}


def prep_shared(inputs, S, layers):
    cols = _col_index()
    sh = {}
    w_in = np.asarray(inputs['w_in'], np.float32)
    sh['wA'] = np.ascontiguousarray(np.stack([w_in[l][:, cols] for l in layers], 0))
    for k in ('cmp_w1_k', 'cmp_w2_k', 'cmp_pos_k', 'cmp_w1_v', 'cmp_w2_v', 'cmp_pos_v', 'w_up', 'w_out',
              'w_mem_q', 'w_mem_kv', 'w_mem_o', 'w_ffn_in', 'w_ffn_out'):
        a = np.asarray(inputs[k], np.float32)
        sh[k] = np.ascontiguousarray(np.stack([a[l] for l in layers], 0))
    rows = []
    for l in layers:
        rows += [np.asarray(inputs[k], np.float32)[l] for k in ('norm_mix', 'norm_mem_q', 'norm_mem_kv', 'norm_ffn')]
    rows.append(np.asarray(inputs['norm_final'], np.float32))
    sh['norms'] = np.ascontiguousarray(np.stack(rows, 0))
    sh.update(make_consts(S))
    return sh


def kernel(**inputs):
    x = np.asarray(inputs['x'], np.float32)
    mem = np.asarray(inputs['mem'], np.float32)
    B, S, _ = x.shape
    layers = [0, 1]
    key = (S, tuple(layers))
    if key not in _CACHE:
        _CACHE[key] = build(S, layers)
    nc = _CACHE[key]
    sh = prep_shared(inputs, S, layers)
    in_maps = []
    for b in range(B):
        m = dict(sh)
        m['x'] = np.ascontiguousarray(x[b])
        m['mem'] = np.ascontiguousarray(mem[b])
        in_maps.append(m)
    res = run_bass_kernel_spmd(nc, in_maps, core_ids=list(range(B)))
    return np.stack([np.asarray(r['out'], np.float32) for r in res.results], 0)
```

```python
import numpy as np
import concourse.bass as bass
import concourse.mybir as mybir
from concourse.bass_utils import run_bass_kernel_spmd
from contextlib import ExitStack
import os

F32 = mybir.dt.float32
BF16 = mybir.dt.bfloat16
AF = mybir.ActivationFunctionType
ALU = mybir.AluOpType
AX = mybir.AxisListType

EPOCH = 30000
D = 1024
DFF = 2816
NEG = -30000.0
BIGF = 1.0e30
NMEM = 256
TOPK = 256
NA_FM = 9 * 64 + 17 * 2 * 64 + 12 + 3072
NA = NA_FM + 452


class _Rec:
    def __init__(self):
        self.calls = []

    def __getattr__(self, name):
        def f(*a, **kw):
            self.calls.append((name, a, kw))
            return self
        return f


class Prog:
    ENGS = ("pe", "act", "dve", "pool", "sp")

    def __init__(self, nc, es):
        self.nc = nc
        self.es = es
        self.ops = {e: [] for e in self.ENGS}
        self.cnt = {e: 0 for e in self.ENGS}
        self.seen = {e: {} for e in self.ENGS}
        self.sems = {}
        self.latest = {}
        self.res_w = {}
        self.res_r = {}
        self.dcnt = {}

    def sem(self, key):
        if key not in self.sems:
            self.sems[key] = self.es.enter_context(
                self.nc.semaphore("s_" + key.replace("#", "_").replace(":", "_")))
        return self.sems[key]

    def _deps(self, eng, reads, writes):
        deps = {}
        for r in reads:
            ev = self.res_w.get(r)
            if ev is not None:
                deps[ev[0]] = max(deps.get(ev[0], 0), ev[1])
        for w in writes:
            ev = self.res_w.get(w)
            if ev is not None:
                deps[ev[0]] = max(deps.get(ev[0], 0), ev[1])
            for ev in self.res_r.get(w, ()):
                deps[ev[0]] = max(deps.get(ev[0], 0), ev[1])
        out = []
        for k, v in deps.items():
            if eng == "pe" and k.startswith("pe#"):
                continue
            if self.seen[eng].get(k, 0) >= v:
                continue
            self.seen[eng][k] = v
            out.append((k, v))
        return out

    def _commit(self, ev, reads, writes):
        self.latest[ev[0]] = max(self.latest.get(ev[0], 0), ev[1])
        for w in writes:
            self.res_w[w] = ev
            self.res_r[w] = []
        for r in reads:
            if r in writes:
                continue
            lst = self.res_r.setdefault(r, [])
            lst.append(ev)
            if len(lst) > 16:
                d = {}
                for k, v in lst:
                    d[k] = max(d.get(k, 0), v)
                self.res_r[r] = list(d.items())

    def op(self, eng, fns, reads=(), writes=()):
        if callable(fns):
            fns = [fns]
        rec = _Rec()
        for f in fns:
            f(rec)
        calls = rec.calls
        px = [r for r in reads if r.startswith("pb") or r == "ptr"]
        if px:
            reads = [r for r in reads if r not in px]
            writes = list(writes) + [r for r in px if r not in writes]
        waits = self._deps(eng, reads, writes)
        c = self.cnt[eng] + 1
        self.cnt[eng] = c
        key = "%s#%d" % (eng, (c - 1) // EPOCH)
        val = (c - 1) % EPOCH + 1
        semh = self.sem(key)
        wl = [(self.sem(k), v) for k, v in waits]

        def emit(e, calls=calls, wl=wl, semh=semh):
            for s, v in wl:
                e.wait_ge(s, v)
            ins = None
            for name, a, kw in calls:
                if name == "matmul":
                    kw = dict(kw)
                    kw["skip_group_check"] = True
                ins = getattr(e, name)(*a, **kw)
            ins.then_inc(semh, 1)

        self.ops[eng].append(emit)
        ev = (key, val)
        self._commit(ev, reads, writes)
        return ev

    def dma(self, q, out, in_, reads=(), writes=(), key=None):
        waits = self._deps(q, reads, writes)
        dk = "dma:" + key
        semh = self.sem(dk)
        self.dcnt[dk] = self.dcnt.get(dk, 0) + 16
        wl = [(self.sem(k), v) for k, v in waits]

        def emit(e, wl=wl, semh=semh, out=out, in_=in_):
            for s, v in wl:
                e.wait_ge(s, v)
            e.dma_start(out=out, in_=in_).then_inc(semh, 16)

        self.ops[q].append(emit)
        ev = (dk, self.dcnt[dk])
        self._commit(ev, reads, writes)
        return ev

    def barrier(self):
        for eng in self.ENGS:
            wl = []
            for k, v in self.latest.items():
                if self.seen[eng].get(k, 0) >= v:
                    continue
                self.seen[eng][k] = v
                wl.append((self.sem(k), v))

            def emit(e, wl=wl):
                for s, v in wl:
                    e.wait_ge(s, v)

            self.ops[eng].append(emit)

    def emit_all(self):
        nc = self.nc
        ops = self.ops
        with nc.Block() as block:
            @block.tensor
            def _(e):
                for f in ops["pe"]:
                    f(e)

            @block.scalar
            def _(e):
                for f in ops["act"]:
                    f(e)

            @block.vector
            def _(e):
                for f in ops["dve"]:
                    f(e)

            @block.gpsimd
            def _(e):
                for f in ops["pool"]:
                    f(e)

            @block.sync
            def _(e):
                for f in ops["sp"]:
                    f(e)
        self.ops = {e: [] for e in self.ENGS}


OFF = {}
_o = 0
for _n, _w in (('q_a', 256), ('k_a', 256), ('v_a', 256), ('q_b', 256), ('k_b', 64), ('v_b', 64),
               ('q_idx', 256), ('k_idx', 64), ('w_idx', 4), ('q_c', 256), ('k_cmp', 64), ('v_cmp', 64),
               ('k_sel', 64), ('v_sel', 64), ('k_win', 64), ('v_win', 64), ('g_c', 12), ('g_merge', 3072)):
    OFF[_n] = _o
    _o += _w

FM_PLAIN = [('q_a', h) for h in range(4)] + [('k_a', h) for h in range(4)] + [('v_cmp', 0)]
FM_ROPE = ([('q_b', h) for h in range(4)] + [('k_b', 0)] + [('q_idx', h) for h in range(4)] + [('k_idx', 0)] +
           [('q_c', h) for h in range(4)] + [('k_cmp', 0), ('k_sel', 0), ('k_win', 0)])


def _col_index():
    cols = []
    for n, h in FM_PLAIN:
        c0 = OFF[n] + 64 * h
        cols += list(range(c0, c0 + 64))
    for n, h in FM_ROPE:
        c0 = OFF[n] + 64 * h
        cols += list(range(c0, c0 + 64))
        cols += [c0 + (d + 32) % 64 for d in range(64)]
    cols += [OFF['g_c'] + h * 3 + br for br in range(3) for h in range(4)]
    cols += list(range(OFF['g_merge'], OFF['g_merge'] + 3072))
    cols += list(range(OFF['v_a'], OFF['v_a'] + 256))
    cols += list(range(OFF['v_b'], OFF['v_b'] + 64))
    cols += list(range(OFF['v_sel'], OFF['v_sel'] + 64))
    cols += list(range(OFF['v_win'], OFF['v_win'] + 64))
    cols += list(range(OFF['w_idx'], OFF['w_idx'] + 4))
    assert len(cols) == NA
    return np.asarray(cols)


def make_consts(S):
    NB = S // 128
    c = {}
    c['c_ident4'] = np.tile(np.eye(128, dtype=np.float32), (1, 4))
    t = np.arange(128)[:, None]
    s = np.arange(128)[None, :]
    m = np.zeros((128, 5, 128), np.float32)
    m[:, 0, :] = np.where(s >= t, NEG, 0.0)
    m[:, 1, :] = np.where(s > t, NEG, 0.0)
    m[:, 2, :] = np.where(s <= t, NEG, 0.0)
    j = np.arange(128)[:, None]
    m[:, 3, :] = np.where(j >= s, -1.0, 0.0)
    m[:, 4, :] = -1.0
    c['c_masks'] = m
    c['c_tokcausal'] = np.where(s > t, -BIGF, 0.0).astype(np.float32)
    inv = 10000.0 ** (-np.arange(0, 64, 2, dtype=np.float32) / 64)
    ang = np.arange(S, dtype=np.float32)[None, :] * inv[:, None]
    cos = np.cos(ang).astype(np.float32)
    sin = np.sin(ang).astype(np.float32)
    c['c_rope'] = np.stack([np.concatenate([cos, cos], 0), np.concatenate([-sin, sin], 0)], 0)
    n_sel = S // 64
    ex = np.zeros((64, S), np.float32)
    for jj in range(min(n_sel, 64)):
        ex[jj, jj * 64:(jj + 1) * 64] = 1.0
    c['c_expand'] = ex
    n_cmp = (S - 32) // 16 + 1
    ncp = ((n_cmp + 1 + 127) // 128) * 128
    cs = np.arange(n_cmp) * 16
    ss = np.arange(n_sel) * 64
    ov = np.zeros((ncp, 64), np.float32)
    ov[:n_cmp, :n_sel] = ((cs[:, None] < ss[None, :] + 64) & (cs[:, None] + 32 > ss[None, :])).astype(np.float32)
    c['c_ov'] = ov
    tg = np.arange(S)
    n = np.arange(ncp)
    adm = (n[None, :] * 16 + 31 <= tg[:, None]) & (n[None, :] < n_cmp)
    c['c_cmpmask'] = np.where(adm, 0.0, NEG).astype(np.float32).reshape(NB, 128, ncp)
    jj = np.arange(64)
    cur = tg // 64
    forced0 = (jj[None, :] == 0)
    forced1 = (jj[None, :] == cur[:, None])
    forced2 = (jj[None, :] == cur[:, None] - 1)
    forced = forced0 | forced1 | forced2
    admis = (jj[None, :] * 64 <= tg[:, None]) & (jj[None, :] < n_sel)
    mul = np.where(forced | ~admis, 0.0, 1.0).astype(np.float32)
    add = np.zeros((S, 64), np.float32)
    add = np.where(forced0, 10000.0, add)
    add = np.where(forced2, 10001.0, add)
    add = np.where(forced1, 10002.0, add)
    add = np.where(admis, add, -BIGF).astype(np.float32)
    c['c_selmul'] = mul.reshape(NB, 128, 64)
    c['c_seladd'] = add.reshape(NB, 128, 64)
    return c


def build(S, layers, debug=False):
    NB = S // 128
    NG = S // 512
    L = len(layers)
    n_sel = S // 64
    topn = min(16, n_sel)
    topk = min(TOPK, S // 4)
    n_cmp = (S - 32) // 16 + 1
    NCP = ((n_cmp + 1 + 127) // 128) * 128
    NCC = NCP // 128
    nc = bass.Bass("TRN2", target_bir_lowering=False)

    def din(name, shape, dt=F32):
        return nc.dram_tensor(name, list(shape), dt, kind="ExternalInput")

    dbg_kind = "ExternalOutput" if debug else None

    def dscr(name, shape, dt):
        if debug:
            return nc.dram_tensor(name, list(shape), dt, kind="ExternalOutput")
        return nc.dram_tensor(name, list(shape), dt)

    x_d = din("x", [S, D])
    mem_d = din("mem", [NMEM, D])
    wA_d = din("wA", [L, D, NA])
    w1k_d = din("cmp_w1_k", [L, 2048, 128]); w2k_d = din("cmp_w2_k", [L, 128, 64]); posk_d = din("cmp_pos_k", [L, 32, 64])
    w1v_d = din("cmp_w1_v", [L, 2048, 128]); w2v_d = din("cmp_w2_v", [L, 128, 64]); posv_d = din("cmp_pos_v", [L, 32, 64])
    wup_d = din("w_up", [L, 3, 256, D]); wout_d = din("w_out", [L, D, D])
    wmq_d = din("w_mem_q", [L, D, 256]); wmkv_d = din("w_mem_kv", [L, D, 512]); wmo_d = din("w_mem_o", [L, 256, D])
    wfi_d = din("w_ffn_in", [L, D, 2 * DFF]); wfo_d = din("w_ffn_out", [L, DFF, D])
    norms_d = din("norms", [4 * L + 1, D])
    c_ident4 = din("c_ident4", [128, 512]); c_masks = din("c_masks", [128, 5, 128]); c_tokc = din("c_tokcausal", [128, 128])
    c_rope = din("c_rope", [2, 64, S]); c_expand = din("c_expand", [64, S]); c_ov = din("c_ov", [NCP, 64])
    c_cmpmask = din("c_cmpmask", [NB, 128, NCP]); c_selmul = din("c_selmul", [NB, 128, 64]); c_seladd = din("c_seladd", [NB, 128, 64])
    out_d = nc.dram_tensor("out", [S, D], F32, kind="ExternalOutput")

    QTA = dscr("QTA", [4, 64, S], BF16); KTA = dscr("KTA", [4, 64, S], BF16)
    QTB = dscr("QTB", [4, 64, S], BF16); KTB = dscr("KTB", [64, S], BF16)
    QTI = dscr("QTI", [4, 64, S], BF16); KTI = dscr("KTI", [64, S], BF16)
    QTC = dscr("QTC", [4, 64, S], BF16)
    KTCMP = dscr("KTCMP", [64, S], BF16); VTCMP = dscr("VTCMP", [64, S], BF16)
    KTSEL = dscr("KTSEL", [64, S], BF16); KTWIN = dscr("KTWIN", [64, S], BF16)
    VA = dscr("VA", [S, 256], BF16); V3 = dscr("V3", [S, 3, 64], BF16)
    WI = dscr("WI", [S, 4], F32); GC = dscr("GC", [12, S], F32); GT = dscr("GT", [3072, S], BF16)
    OT = dscr("OT", [12, 64, S], BF16)
    HM = dscr("HM", [S, D], F32); HO = dscr("HO", [S, D], F32)
    fm_dst = {}
    for n, h in FM_PLAIN + FM_ROPE:
        fm_dst[(n, h)] = {'q_a': QTA, 'k_a': KTA, 'q_b': QTB, 'q_idx': QTI, 'q_c': QTC}.get(n)
    single = {'v_cmp': VTCMP, 'k_b': KTB, 'k_idx': KTI, 'k_cmp': KTCMP, 'k_sel': KTSEL, 'k_win': KTWIN}

    def bcrow(handle, row, n=D, parts=128):
        return bass.AP(handle, row * n, [[0, parts], [1, n]])

    with ExitStack() as es:
        P = Prog(nc, es)
        sbg = lambda n, s, d: es.enter_context(nc.sbuf_tensor(n, list(s), d))
        psg = lambda n, s, d: es.enter_context(nc.psum_tensor(n, list(s), d))
        pb = [None] * 8
        ptrh = [None]

        def alloc_psum(stk, tag, nfp):
            for i_ in range(nfp):
                pb[i_] = stk.enter_context(nc.psum_tensor("pb%d_%s" % (i_, tag), [128, 512], F32))
            if nfp < 8:
                ptrh[0] = stk.enter_context(nc.psum_tensor("ptr_%s" % tag, [128, 1024], BF16))
        ident = sbg("ident", [128, 512], BF16)
        masks = sbg("masks", [128, 5, 128], BF16)
        ones32 = sbg("ones32", [128, 64], F32)
        stat = sbg("stat", [128, 16], F32)
        P.dma("pool", ident[:], c_ident4.ap(), writes=["ident"], key="c0")
        P.dma("pool", masks[:], c_masks.ap(), writes=["masks"], key="c1")
        P.op("pool", lambda e: e.memset(ones32[:], 1.0), writes=["ones32"])

        def rmsnorm_T(src, src_res, g_bc, g_res, dstT, dst_res, tmp):
            junk, ubf = tmp
            P.op("act", lambda e: e.activation(out=junk[:], in_=src, func=AF.Square, accum_out=stat[:, 0:1]),
                 reads=[src_res], writes=["junk", "stat0"])
            P.op("act", lambda e: e.activation(out=stat[:, 1:2], in_=stat[:, 0:1], func=AF.Sqrt, scale=1.0 / D, bias=1e-6),
                 reads=["stat0"], writes=["stat1"])
            P.op("dve", lambda e: e.reciprocal(out=stat[:, 2:3], in_=stat[:, 1:2]), reads=["stat1"], writes=["stat2"])
            P.op("dve", lambda e: e.scalar_tensor_tensor(out=ubf[:], in0=src, scalar=stat[:, 2:3], in1=g_bc[:],
                                                          op0=ALU.mult, op1=ALU.mult),
                 reads=[src_res, "stat2", g_res], writes=["ubf"])
            P.op("pe", [(lambda e, kc=kc: e.transpose(out=ptrh[0][:, kc * 128:(kc + 1) * 128], in_=ubf[:, kc * 128:(kc + 1) * 128],
                                                      identity=ident[:, 0:128])) for kc in range(8)],
                 reads=["ubf", "ident"], writes=["ptr"])
            P.op("act", lambda e: e.activation(out=dstT, in_=ptrh[0][:].rearrange("p (k t) -> p k t", k=8), func=AF.Copy),
                 reads=["ptr"], writes=[dst_res])

        def attn_finish(obank, obres, dst, dst_res, work, gate=None, guard=False, extra_reads=(), bcbank=6):
            rrow, bcs, accum = work
            P.op("act", lambda e: e.activation(out=rrow[64:65, :], in_=obank[64:65, :], func=AF.Ln), reads=[obres], writes=["rrow"])
            P.op("act", lambda e: e.activation(out=rrow[64:65, :], in_=rrow[64:65, :], func=AF.Exp, scale=-1.0), reads=["rrow"], writes=["rrow"])
            if gate is not None:
                gap, gres = gate
                P.op("dve", lambda e: e.tensor_tensor(out=rrow[64:65, :], in0=rrow[64:65, :], in1=gap, op=ALU.mult),
                     reads=["rrow", gres], writes=["rrow"])
            P.op("pe", lambda e: e.matmul(pb[bcbank][0:64, :], lhsT=ones32[64:65, 0:64], rhs=rrow[64:65, :], start=True, stop=True),
                 reads=["rrow", "ones32"], writes=["pb%d" % bcbank])
            P.op("act", lambda e: e.activation(out=bcs[0:64, :], in_=pb[bcbank][0:64, :], func=AF.Copy), reads=["pb%d" % bcbank], writes=["bcs"])
            if accum is None:
                P.op("dve", lambda e: e.tensor_tensor(out=dst, in0=obank[0:64, :], in1=bcs[0:64, :], op=ALU.mult),
                     reads=[obres, "bcs"] + list(extra_reads), writes=[dst_res])
            else:
                acc, first = accum
                if first:
                    P.op("dve", lambda e: e.tensor_tensor(out=acc[0:64, :], in0=obank[0:64, :], in1=bcs[0:64, :], op=ALU.mult),
                         reads=[obres, "bcs"], writes=["oacc"])
                else:
                    P.op("dve", lambda e: e.tensor_tensor(out=bcs[0:64, :], in0=obank[0:64, :], in1=bcs[0:64, :], op=ALU.mult),
                         reads=[obres, "bcs"], writes=["bcs"])
                    P.op("pool", lambda e: e.tensor_tensor(out=acc[0:64, :], in0=acc[0:64, :], in1=bcs[0:64, :], op=ALU.add),
                         reads=["bcs", "oacc"], writes=["oacc"])

        for li, l in enumerate(layers):
            h_in = x_d if li == 0 else HO
            last = (li == L - 1)
            with ExitStack() as ea:
                sb = lambda n, s, d: ea.enter_context(nc.sbuf_tensor(n + "_L%d" % li, list(s), d))
                alloc_psum(ea, "A%d" % li, 7)
                wA = sb("wA_sb", [128, 8, NA], BF16)
                gA = sb("gA", [128, D], F32)
                rope = sb("rope", [64, 2, S], F32)
                hb = [sb("hbA%d" % i, [128, D], F32) for i in range(2)]
                junk = sb("junkA", [128, D], F32)
                ubf = sb("ubfA", [128, D], BF16)
                uT = sb("uTA", [128, 8, 512], BF16)
                stg = [sb("stgA%d" % i, [128, 512], BF16) for i in range(4)]
                stgf = sb("stgfA", [128, 512], F32)
                t1 = sb("ropeT1", [64, 512], F32)
                t2 = sb("ropeT2", [64, 512], F32)
                for kc in range(8):
                    P.dma("pool", wA[:, kc, :], wA_d.ap()[l, kc * 128:(kc + 1) * 128, :], writes=["wA%d" % kc], key="wA%d" % kc)
                wA_res = ["wA%d" % kc for kc in range(8)]
                P.dma("sp", gA[:], bcrow(norms_d, 4 * l + 0), writes=["gA"], key="gA")
                P.dma("sp", rope[:], c_rope.ap().rearrange("c d s -> d c s"), writes=["rope"], key="rope")
                sctr = [0]

                def stage():
                    i = sctr[0] % 4
                    sctr[0] += 1
                    return stg[i], "stgA%d" % i

                def proj_fm(bank, c0, M):
                    P.op("pe", [(lambda e, kc=kc: e.matmul(pb[bank][0:M, :], lhsT=wA[:, kc, c0:c0 + M], rhs=uT[:, kc, :],
                                                            start=(kc == 0), stop=(kc == 7))) for kc in range(8)],
                         reads=["uT"] + wA_res, writes=["pb%d" % bank])

                LIM = int(os.environ.get("LIMA", "99"))
                for g in range(NG if LIM > 0 else 0):
                    gs = slice(g * 512, (g + 1) * 512)
                    for b4 in range(4):
                        blk = g * 4 + b4
                        hbt = hb[blk % 2]
                        P.dma("sp", hbt[:], h_in.ap()[blk * 128:(blk + 1) * 128, :], writes=["hb%d" % (blk % 2)], key="hb%d" % (blk % 2))
                        rmsnorm_T(hbt[:], "hb%d" % (blk % 2), gA, "gA", uT[:, :, b4 * 128:(b4 + 1) * 128], "uT", (junk, ubf))
                    col = 0
                    bank = 0
                    if LIM <= 1:
                        continue
                    for (n, h) in FM_PLAIN:
                        proj_fm(bank, col, 64)
                        st, sr = stage()
                        sc = 0.125 if n == 'q_a' else 1.0
                        P.op("act", lambda e, bank=bank, st=st, sc=sc: e.activation(out=st[0:64, :], in_=pb[bank][0:64, :], func=AF.Copy, scale=sc),
                             reads=["pb%d" % bank], writes=[sr])
                        dst = fm_dst[(n, h)].ap()[h, :, gs] if fm_dst[(n, h)] is not None else single[n].ap()[:, gs]
                        P.dma("sp", dst, st[0:64, :], reads=[sr], writes=[], key=sr)
                        col += 64
                        bank = (bank + 1) % 6
                    if LIM <= 2:
                        continue
                    for (n, h) in FM_ROPE:
                        b0 = bank
                        b1 = (bank + 1) % 6
                        proj_fm(b0, col, 64)
                        proj_fm(b1, col + 64, 64)
                        st, sr = stage()
                        P.op("dve", lambda e, b0=b0: e.tensor_tensor(out=t1[:], in0=pb[b0][0:64, :], in1=rope[:, 0, gs], op=ALU.mult),
                             reads=["pb%d" % b0, "rope"], writes=["t1"])
                        P.op("dve", lambda e, b1=b1: e.tensor_tensor(out=t2[:], in0=pb[b1][0:64, :], in1=rope[:, 1, gs], op=ALU.mult),
                             reads=["pb%d" % b1, "rope"], writes=["t2"])
                        P.op("pool", lambda e, st=st: e.tensor_tensor(out=st[0:64, :], in0=t1[:], in1=t2[:], op=ALU.add),
                             reads=["t1", "t2"], writes=[sr])
                        dst = fm_dst[(n, h)].ap()[h, :, gs] if fm_dst[(n, h)] is not None else single[n].ap()[:, gs]
                        P.dma("sp", dst, st[0:64, :], reads=[sr], writes=[], key=sr)
                        col += 128
                        bank = (bank + 2) % 6
                    if LIM <= 3:
                        continue
                    proj_fm(bank, col, 12)
                    P.op("act", lambda e, bank=bank: e.activation(out=stgf[0:12, :], in_=pb[bank][0:12, :], func=AF.Sigmoid),
                         reads=["pb%d" % bank], writes=["stgf"])
                    P.dma("sp", GC.ap()[:, gs], stgf[0:12, :], reads=["stgf"], writes=[], key="stgf")
                    col += 12
                    bank = (bank + 1) % 6
                    for gg in range(24):
                        proj_fm(bank, col, 128)
                        st, sr = stage()
                        P.op("act", lambda e, bank=bank, st=st: e.activation(out=st[:], in_=pb[bank][:], func=AF.Sigmoid),
                             reads=["pb%d" % bank], writes=[sr])
                        P.dma("sp", GT.ap()[gg * 128:(gg + 1) * 128, gs], st[:], reads=[sr], writes=[], key=sr)
                        col += 128
                        bank = (bank + 1) % 6
                    assert col == NA_FM
                    if LIM <= 4:
                        continue
                    for b4 in range(4):
                        blk = g * 4 + b4
                        P.op("pe", [(lambda e, kc=kc, b4=b4, bank=bank: e.matmul(pb[bank][:, 0:452], lhsT=uT[:, kc, b4 * 128:(b4 + 1) * 128],
                                                                                  rhs=wA[:, kc, NA_FM:NA], start=(kc == 0), stop=(kc == 7)))
                                    for kc in range(8)], reads=["uT"] + wA_res, writes=["pb%d" % bank])
                        st, sr = stage()
                        P.op("act", lambda e, bank=bank, st=st: e.activation(out=st[:, 0:448], in_=pb[bank][:, 0:448], func=AF.Copy),
                             reads=["pb%d" % bank], writes=[sr])
                        P.op("dve", lambda e, bank=bank: e.tensor_scalar(out=stgf[:, 0:4], in0=pb[bank][:, 448:452], scalar1=1.0 / 16, scalar2=None, op0=ALU.mult),
                             reads=["pb%d" % bank], writes=["stgf"])
                        P.dma("sp", VA.ap()[blk * 128:(blk + 1) * 128, :], st[:, 0:256], reads=[sr], writes=[], key=sr)
                        P.dma("sp", V3.ap()[blk * 128:(blk + 1) * 128, :, :], st[:, 256:448].rearrange("p (a d) -> p a d", a=3), reads=[sr], writes=[], key=sr)
                        P.dma("sp", WI.ap()[blk * 128:(blk + 1) * 128, :], stgf[:, 0:4], reads=["stgf"], writes=[], key="stgf")
                        bank = (bank + 1) % 6
                P.barrier()
                P.emit_all()
            if debug == "A":
                break
            with ExitStack() as eb:
                sb = lambda n, s, d: eb.enter_context(nc.sbuf_tensor(n + "_L%d" % li, list(s), d))
                alloc_psum(eb, "B%d" % li, 8)
                KTAs = sb("KTAs", [64, 4, S], BF16); VAs = sb("VAs", [128, NB, 256], BF16)
                KTBs = sb("KTBs", [64, S], BF16); KTIs = sb("KTIs", [64, S], BF16)
                KTSs = sb("KTSs", [64, S], BF16); KTWs = sb("KTWs", [64, S], BF16)
                V3s = [sb("V3s%d" % j, [128, NB, 65], BF16) for j in range(3)]
                EXs = sb("EXs", [64, S], BF16)
                KCT = sb("KCT", [64, NCP], BF16); VCs = sb("VCs", [128, NCC, 65], BF16); OVs = sb("OVs", [128, NCC, 64], BF16)
                tokc = sb("tokc", [128, 128], F32)
                score = sb("score", [128, S], F32); junkb = sb("junkb", [128, S], BF16); mneg = [sb("mneg%d" % i_, [128, S], BF16) for i_ in range(2)]
                QA = [sb("QA%d" % i, [64, 512], BF16) for i in range(2)]
                QB = [sb("QB%d" % i, [64, 512], BF16) for i in range(2)]
                QI = [sb("QI%d" % i, [64, 512], BF16) for i in range(2)]
                QC = [sb("QC%d" % i, [64, 512], BF16) for i in range(2)]
                wi = [sb("wi%d" % i, [128, 4], F32) for i in range(2)]
                grow = [sb("grow%d" % i_, [128, 3, 512], F32) for i_ in range(2)]
                cmk = [sb("cmk%d" % i, [128, NCP], BF16) for i in range(2)]
                smul = [sb("smul%d" % i, [128, 64], F32) for i in range(2)]
                sadd = [sb("sadd%d" % i, [128, 64], F32) for i in range(2)]
                e1 = [sb("e1_%d" % i, [128, 512], F32) for i in range(2)]
                sp_ = [sb("sp_%d" % i, [128, 512], BF16) for i in range(2)]
                asb = [sb("asb%d" % i, [128, 512], F32) for i in range(2)]
                Wt = [sb("Wt%d" % i, [128, 512], BF16) for i in range(2)]
                chi = sb("chi", [2, 512], BF16); crow2 = sb("crow2", [2, 512], BF16); negm = sb("negm", [2, 1], F32); onesb = sb("onesb", [2, 128], BF16)
                P.op("pool", lambda e: e.memset(negm[0:2, :], -1.0), writes=["negm"])
                P.op("pool", lambda e: e.memset(negm[0:1, :], 0.0), writes=["negm"])
                P.op("pool", lambda e: e.memset(onesb[:], 1.0), writes=["onesb"])
                Eb = [sb("Eb%d" % i, [128, 512], BF16) for i in range(3)]
                tmpi = [sb("tmpi%d" % i_, [128, 512], F32) for i_ in range(2)]
                rrow = sb("rrow", [128, 512], F32); bcs = sb("bcs", [64, 512], F32); oacc = sb("oacc", [64, 512], F32)
                ost = [sb("ost%d" % i, [64, 3, 512], BF16) for i in range(2)]
                bis = sb("bis", [128, 8], F32)
                impT = sb("impT", [64, 128], F32); uimp = sb("uimp", [64, 512], F32)
                imp = sb("imp", [128, 64], F32); imp2 = sb("imp2", [128, 64], F32); m8 = sb("m8", [128, 16], F32)
                msel = sb("msel", [128, 64], F32); mselT = sb("mselT", [64, 512], BF16)
                identf = sb("identf", [128, 128], F32)
                w2 = sb("w2", [128, 64], BF16); posT = sb("posT", [64, 32], BF16)
                w1 = (mneg[0][0:64, 0:4096] if S >= 4096 else sb("w1x", [64, 4096], BF16)[:, :]).rearrange("d (a h) -> d a h", a=32)
                TCs = junkb[0:64, :]
                hid = sb("hid", [128, NCP], BF16); bcol = sb("bcol", [128, 1], F32)

                P.dma("sp", KTAs[:], KTA.ap().rearrange("h d s -> d h s"), reads=["scrA"], writes=["KTAs"], key="ldB0")
                P.dma("sp", VAs[:], VA.ap().rearrange("(b p) c -> p b c", p=128), reads=["scrA"], writes=["VAs"], key="ldB1")
                P.dma("sp", KTBs[:], KTB.ap(), reads=["scrA"], writes=["KTBs"], key="ldB2")
                P.dma("sp", KTIs[:], KTI.ap(), reads=["scrA"], writes=["KTIs"], key="ldB3")
                P.dma("sp", KTSs[:], KTSEL.ap(), reads=["scrA"], writes=["KTSs"], key="ldB4")
                P.dma("sp", KTWs[:], KTWIN.ap(), reads=["scrA"], writes=["KTWs"], key="ldB5")
                for j in range(3):
                    P.op("pool", lambda e, j=j: e.memset(V3s[j][:], 1.0), writes=["V3s%d" % j])
                    P.dma("sp", V3s[j][:, :, 0:64], V3.ap()[:, j, :].rearrange("(b p) d -> p b d", p=128), reads=["scrA"], writes=["V3s%d" % j], key="ldB6%d" % j)
                P.dma("pool", EXs[:], c_expand.ap(), writes=["EXs"], key="ldB7")
                P.dma("pool", OVs[:], c_ov.ap().rearrange("(c p) j -> p c j", p=128), writes=["OVs"], key="ldB8")
                P.dma("sp", tokc[:], c_tokc.ap(), writes=["tokc"], key="ldB9")
                P.dma("sp", identf[:], c_ident4.ap()[:, 0:128], writes=["identf"], key="ldB10")
                P.op("pool", lambda e: e.memset(VCs[:], 1.0), writes=["VCs"])
                for which in range(2 if int(os.environ.get("LIMB", "99")) > 0 else 0):
                    w1_d, w2_d, pos_d, src = ((w1k_d, w2k_d, posk_d, KTCMP), (w1v_d, w2v_d, posv_d, VTCMP))[which]
                    P.dma("pool", w1, w1_d.ap()[l].rearrange("(a d) h -> d a h", d=64), writes=["mneg0"], key="cw1")
                    P.dma("pool", w2[:], w2_d.ap()[l], writes=["w2"], key="cw2")
                    for a in range(32):
                        P.dma("pool", posT[:, a:a + 1], pos_d.ap()[l, a:a + 1, :].rearrange("a d -> d a"), writes=["posT"], key="cpos")
                    P.dma("sp", TCs, src.ap(), reads=["scrA"], writes=["junkb"], key="cTC")
                    P.op("pe", [(lambda e, a=a: e.matmul(pb[1][:, 0:1], lhsT=w1[:, a, :], rhs=posT[:, a:a + 1], start=(a == 0), stop=(a == 31)))
                                for a in range(32)], reads=["mneg0", "posT"], writes=["pb1"])
                    P.op("act", lambda e: e.activation(out=bcol[:], in_=pb[1][:, 0:1], func=AF.Copy), reads=["pb1"], writes=["bcol"])
                    P.op("pe", [(lambda e, a=a: e.matmul(pb[0][:, 0:n_cmp], lhsT=w1[:, a, :], rhs=TCs[:, a:a + 16 * (n_cmp - 1) + 1:16],
                                                         start=(a == 0), stop=(a == 31))) for a in range(32)],
                         reads=["mneg0", "junkb"], writes=["pb0"])
                    P.op("pool", lambda e: e.memset(hid[:], 0.0), writes=["hid"])
                    P.op("act", lambda e: e.activation(out=hid[:, 0:n_cmp], in_=pb[0][:, 0:n_cmp], func=AF.Gelu_apprx_tanh, bias=bcol[:, 0:1]),
                         reads=["pb0", "bcol"], writes=["hid"])
                    if which == 0:
                        P.op("pe", lambda e: e.matmul(pb[2][0:64, 0:NCP], lhsT=w2[:], rhs=hid[:], start=True, stop=True),
                             reads=["w2", "hid"], writes=["pb2"])
                        P.op("act", lambda e: e.activation(out=KCT[:], in_=pb[2][0:64, 0:NCP], func=AF.Copy), reads=["pb2"], writes=["KCT"])
                    else:
                        for c in range(NCC):
                            P.op("pe", lambda e, c=c: e.matmul(pb[2][:, 0:64], lhsT=hid[:, c * 128:(c + 1) * 128], rhs=w2[:], start=True, stop=True),
                                 reads=["w2", "hid"], writes=["pb2"])
                            P.op("act", lambda e, c=c: e.activation(out=VCs[:, c, 0:64], in_=pb[2][:, 0:64], func=AF.Copy), reads=["pb2"], writes=["VCs"])

                ectr = [0]
                tctr = [0]
                LIMB = int(os.environ.get("LIMB", "99"))

                def load_block(i):
                    pi = i % 2
                    ts = slice(i * 128, (i + 1) * 128)
                    for (qt, qn, src) in ((QI, "QI", QTI), (QA, "QA", QTA), (QB, "QB", QTB), (QC, "QC", QTC)):
                        P.dma("sp", qt[pi][:].rearrange("d (h t) -> d h t", h=4), src.ap()[:, :, ts].rearrange("h d t -> d h t"),
                              reads=[], writes=["%s%d" % (qn, pi)], key="%s%d" % (qn, pi))
                    P.dma("sp", wi[pi][:], WI.ap()[ts, :], writes=["wi%d" % pi], key="wi%d" % pi)
                    P.dma("sp", grow[pi][64:65, :, :].rearrange("o b (h t) -> o b h t", h=4),
                          GC.ap()[:, ts].rearrange("(o b h) t -> o b h t", o=1, b=3), writes=["grow%d" % pi], key="grow%d" % pi)
                    P.dma("pool", cmk[pi][:], c_cmpmask.ap()[i], writes=["cmk%d" % pi], key="cmk%d" % pi)
                    P.dma("sp", smul[pi][:], c_selmul.ap()[i], writes=["smul%d" % pi], key="smul%d" % pi)
                    P.dma("sp", sadd[pi][:], c_seladd.ap()[i], writes=["sadd%d" % pi], key="sadd%d" % pi)

                def chain_P(i):
                    U = []
                    pi = i % 2
                    Lk = (i + 1) * 128
                    qi = QI[pi]
                    qir = "QI%d" % pi
                    mn = mneg[pi]
                    mres = "mneg%d" % pi
                    wres = "wi%d" % pi
                    nch = (Lk + 511) // 512
                    for c in range(nch):
                        w_ = min(512, Lk - c * 512)
                        cs = slice(c * 512, c * 512 + w_)
                        for h in range(4):
                            def u(c=c, h=h, w_=w_, cs=cs):
                                P.op("pe", lambda e: e.matmul(pb[7][:, 0:w_], lhsT=qi[:, h * 128:(h + 1) * 128], rhs=KTIs[:, cs], start=True, stop=True),
                                     reads=[qir, "KTIs"], writes=["pb7"])
                                if h == 0:
                                    P.op("dve", lambda e: e.tensor_scalar(out=score[:, cs], in0=pb[7][:, 0:w_], scalar1=0.0, scalar2=wi[pi][:, 0:1], op0=ALU.max, op1=ALU.mult),
                                         reads=["pb7", wres], writes=["score"])
                                else:
                                    tb = tctr[0] % 2
                                    tctr[0] += 1
                                    P.op("dve", lambda e: e.tensor_scalar(out=tmpi[tb][:, 0:w_], in0=pb[7][:, 0:w_], scalar1=0.0, scalar2=wi[pi][:, h:h + 1], op0=ALU.max, op1=ALU.mult),
                                         reads=["pb7", wres], writes=["tmpi%d" % tb])
                                    P.op("pool", lambda e: e.tensor_tensor(out=score[:, cs], in0=score[:, cs], in1=tmpi[tb][:, 0:w_], op=ALU.add),
                                         reads=["tmpi%d" % tb, "score"], writes=["score"])
                            U.append(u)

                    def u_pre():
                        if Lk > topk:
                            P.op("dve", lambda e: e.tensor_reduce(out=bis[:, 0:1], in_=score[:, 0:Lk], axis=AX.X, op=ALU.min), reads=["score"], writes=["bis"])
                            P.op("dve", lambda e: e.tensor_reduce(out=bis[:, 1:2], in_=score[:, 0:Lk], axis=AX.X, op=ALU.max), reads=["score"], writes=["bis"])
                            P.op("dve", lambda e: e.tensor_tensor(out=bis[:, 1:2], in0=bis[:, 1:2], in1=bis[:, 0:1], op=ALU.subtract), reads=["bis"], writes=["bis"])
                        P.op("pool", lambda e: e.tensor_tensor(out=score[:, i * 128:(i + 1) * 128], in0=score[:, i * 128:(i + 1) * 128], in1=tokc[:], op=ALU.add),
                             reads=["score", "tokc"], writes=["score"])
                    U.append(u_pre)
                    if Lk > topk:
                        def u_it():
                            P.op("dve", lambda e: e.tensor_scalar(out=bis[:, 1:2], in0=bis[:, 1:2], scalar1=0.5, scalar2=None, op0=ALU.mult), reads=["bis"], writes=["bis"])
                            P.op("dve", lambda e: e.tensor_tensor(out=bis[:, 2:3], in0=bis[:, 0:1], in1=bis[:, 1:2], op=ALU.add), reads=["bis"], writes=["bis"])
                            P.op("dve", lambda e: e.tensor_scalar(out=junkb[:, 0:Lk], in0=score[:, 0:Lk], scalar1=bis[:, 2:3], scalar2=None, op0=ALU.is_ge, op1=ALU.add,
                                                                  accum_out=bis[:, 3:4]), reads=["score", "bis"], writes=["junkb", "bis"])
                            P.op("dve", lambda e: e.tensor_scalar(out=bis[:, 4:5], in0=bis[:, 3:4], scalar1=float(topk) - 0.5, scalar2=None, op0=ALU.is_ge), reads=["bis"], writes=["bis"])
                            P.op("dve", lambda e: e.scalar_tensor_tensor(out=bis[:, 0:1], in0=bis[:, 4:5], scalar=bis[:, 1:2], in1=bis[:, 0:1], op0=ALU.mult, op1=ALU.add),
                                 reads=["bis"], writes=["bis"])
                        for it in range(NBIS):
                            U.append(u_it)

                        def u_fin():
                            P.op("dve", lambda e: e.tensor_scalar(out=mn[:, 0:Lk], in0=score[:, 0:Lk], scalar1=bis[:, 0:1], scalar2=NEG, op0=ALU.is_lt, op1=ALU.mult),
                                 reads=["score", "bis"], writes=[mres])
                    else:
                        def u_fin():
                            P.op("dve", lambda e: e.tensor_scalar(out=mn[:, 0:Lk], in0=score[:, 0:Lk], scalar1=-1.0e29, scalar2=NEG, op0=ALU.is_lt, op1=ALU.mult),
                                 reads=["score"], writes=[mres])
                    U.append(u_fin)
                    return U

                def chain_SB(i):
                    U = []
                    pi = i % 2
                    qa = QA[pi]
                    qar = "QA%d" % pi
                    o_st = ost[pi]
                    ostr = "ost%d" % pi
                    kbs = list(range(i, -1, -1))
                    n = len(kbs)

                    def z_ops(kb, bank, start_first):
                        fns = []
                        first = start_first
                        if kb == i:
                            fns.append(lambda e, first=first: e.matmul(pb[bank][:], lhsT=masks[:, 0, :], rhs=ident[:], start=first, stop=False))
                            first = False
                        for h in range(4):
                            fns.append(lambda e, h=h, first=first: e.matmul(
                                pb[bank][:, h * 128:(h + 1) * 128], lhsT=KTAs[:, h, kb * 128:(kb + 1) * 128], rhs=qa[:, h * 128:(h + 1) * 128],
                                start=first, stop=(h == 3)))
                            first = False
                        return fns

                    def stage1(j):
                        kb = kbs[j]
                        bk = j % 2
                        P.op("pe", z_ops(kb, bk, True), reads=["KTAs", qar, "masks", "ident"], writes=["pb%d" % bk])
                        P.op("act", lambda e: e.activation(out=e1[bk][:], in_=pb[bk][:], func=AF.Exp), reads=["pb%d" % bk], writes=["e1_%d" % bk])
                        P.op("act", lambda e: e.activation(out=sp_[bk][:], in_=e1[bk][:], func=AF.Ln, bias=1.0), reads=["e1_%d" % bk], writes=["sp_%d" % bk])

                    def stage2(j):
                        kb = kbs[j]
                        bk = j % 2
                        fns = [lambda e: e.matmul(pb[bk][:], lhsT=masks[:, 3, :], rhs=sp_[bk][:], start=False, stop=(j == 0))]
                        rds = ["sp_%d" % bk, "masks"]
                        if j > 0:
                            P.op("act", lambda e: e.activation(out=chi[0:2, :], in_=pb[2][0:2, :], func=AF.Copy), reads=["pb2"], writes=["chi"])
                            P.op("dve", lambda e: e.scalar_tensor_tensor(out=crow2[0:2, :], in0=chi[0:2, :], scalar=negm[0:2, 0:1], in1=pb[2][0:2, :],
                                                                          op0=ALU.mult, op1=ALU.add), reads=["chi", "negm", "pb2"], writes=["crow2"])
                            fns.append(lambda e: e.matmul(pb[bk][:], lhsT=onesb[0:2, :], rhs=crow2[0:2, :], start=False, stop=True))
                            rds += ["onesb", "crow2"]
                        P.op("pe", fns, reads=rds, writes=["pb%d" % bk])
                        P.op("pe", lambda e: e.matmul(pb[2][:], lhsT=masks[:, 4, :], rhs=sp_[bk][:], start=(j == 0), stop=(j == n - 1)),
                             reads=["sp_%d" % bk, "masks"], writes=["pb2"])
                        P.op("act", lambda e: e.activation(out=Wt[bk][:], in_=pb[bk][:], func=AF.Exp), reads=["pb%d" % bk], writes=["Wt%d" % bk])
                        P.op("pe", [(lambda e, h=h: e.matmul(pb[3][0:64, h * 128:(h + 1) * 128], lhsT=VAs[:, kb, h * 64:(h + 1) * 64],
                                                             rhs=Wt[bk][:, h * 128:(h + 1) * 128], start=(j == 0 and h == 0), stop=(j == n - 1)))
                                    for h in range(4)], reads=["Wt%d" % bk, "VAs"], writes=["pb3"])

                    def u0():
                        stage1(0)
                    U.append(u0)
                    for j in range(n):
                        def u(j=j):
                            if j + 1 < n:
                                stage1(j + 1)
                            stage2(j)
                        U.append(u)

                    def ufin():
                        P.op("act", lambda e: e.activation(out=o_st[:, 0, :], in_=pb[3][0:64, :], func=AF.Copy), reads=["pb3"], writes=[ostr])
                    U.append(ufin)
                    return U

                def attn_units(U, nsteps, s_ops, v_ap, v_res, scale=0.125):
                    def emitS(j):
                        bk = 4 + j % 2
                        fns, rd = s_ops(j, bk)
                        P.op("pe", fns, reads=rd, writes=["pb%d" % bk])

                    def unit(j):
                        if j == 0:
                            emitS(0)
                        if j + 1 < nsteps:
                            emitS(j + 1)
                        bk = 4 + j % 2
                        eb_i = ectr[0] % 3
                        ectr[0] += 1
                        E = Eb[eb_i]
                        P.op("act", lambda e: e.activation(out=E[:], in_=pb[bk][:], func=AF.Exp, scale=scale),
                             reads=["pb%d" % bk], writes=["Eb%d" % eb_i])
                        P.op("pe", lambda e: e.matmul(pb[6][0:65, :], lhsT=v_ap(j), rhs=E[:], start=(j == 0), stop=(j == nsteps - 1)),
                             reads=["Eb%d" % eb_i, v_res], writes=["pb6"])
                    for j in range(nsteps):
                        U.append((lambda j=j: unit(j)))

                def chain_ATT(i):
                    U = []
                    pi = i % 2
                    ts = slice(i * 128, (i + 1) * 128)
                    qb, qc = QB[pi], QC[pi]
                    qbr, qcr = "QB%d" % pi, "QC%d" % pi
                    o_st = ost[pi]
                    ostr = "ost%d" % pi
                    mn = mneg[pi]
                    mres = "mneg%d" % pi
                    gres = "grow%d" % pi

                    def dsa_s(j, bk):
                        return ([lambda e: e.matmul(pb[bk][:], lhsT=mn[:, j * 128:(j + 1) * 128], rhs=ident[:], start=True, stop=False),
                                 lambda e: e.matmul(pb[bk][:], lhsT=KTBs[:, j * 128:(j + 1) * 128], rhs=qb[:], start=False, stop=True)],
                                [mres, "ident", "KTBs", qbr])
                    attn_units(U, i + 1, dsa_s, lambda j: V3s[0][:, j, :], "V3s0")
                    U.append(lambda: attn_finish(pb[6], "pb6", o_st[:, 1, :], ostr, (rrow, bcs, None), bcbank=4))

                    def u_cmp():
                        for j in range(NCC):
                            P.op("pe", [lambda e: e.matmul(pb[4][:], lhsT=cmk[pi][:, j * 128:(j + 1) * 128], rhs=ident[:], start=True, stop=False),
                                        lambda e: e.matmul(pb[4][:], lhsT=KCT[:, j * 128:(j + 1) * 128], rhs=qc[:], start=False, stop=True)],
                                 reads=["cmk%d" % pi, "ident", "KCT", qcr], writes=["pb4"])
                            eb_i = ectr[0] % 3
                            ectr[0] += 1
                            E = Eb[eb_i]
                            P.op("act", lambda e: e.activation(out=E[:], in_=pb[4][:], func=AF.Exp, scale=0.125), reads=["pb4"], writes=["Eb%d" % eb_i])
                            P.op("pe", lambda e: e.matmul(pb[6][0:65, :], lhsT=VCs[:, j, :], rhs=E[:], start=(j == 0), stop=(j == NCC - 1)),
                                 reads=["Eb%d" % eb_i, "VCs"], writes=["pb6"])
                            P.op("pe", lambda e: e.matmul(pb[5][0:64, :], lhsT=OVs[:, j, :], rhs=E[:], start=(j == 0), stop=(j == NCC - 1)),
                                 reads=["Eb%d" % eb_i, "OVs"], writes=["pb5"])
                    U.append(u_cmp)

                    def u_imp():
                        P.op("dve", lambda e: e.tensor_scalar(out=rrow[64:65, :], in0=pb[6][64:65, :], scalar1=1e-30, scalar2=None, op0=ALU.max), reads=["pb6"], writes=["rrow"])
                        P.op("act", lambda e: e.activation(out=rrow[64:65, :], in_=rrow[64:65, :], func=AF.Ln), reads=["rrow"], writes=["rrow"])
                        P.op("act", lambda e: e.activation(out=rrow[64:65, :], in_=rrow[64:65, :], func=AF.Exp, scale=-1.0), reads=["rrow"], writes=["rrow"])
                        P.op("pe", lambda e: e.matmul(pb[4][0:64, :], lhsT=ones32[64:65, 0:64], rhs=rrow[64:65, :], start=True, stop=True), reads=["rrow", "ones32"], writes=["pb4"])
                        P.op("act", lambda e: e.activation(out=bcs[0:64, :], in_=pb[4][0:64, :], func=AF.Copy), reads=["pb4"], writes=["bcs"])
                        P.op("dve", lambda e: e.tensor_tensor(out=uimp[:], in0=pb[5][0:64, :], in1=bcs[0:64, :], op=ALU.mult), reads=["pb5", "bcs"], writes=["uimp"])
                        P.op("dve", lambda e: e.tensor_tensor(out=impT[:], in0=uimp[:, 0:128], in1=uimp[:, 128:256], op=ALU.add), reads=["uimp"], writes=["impT"])
                        P.op("dve", lambda e: e.tensor_tensor(out=impT[:], in0=impT[:], in1=uimp[:, 256:384], op=ALU.add), reads=["uimp", "impT"], writes=["impT"])
                        P.op("dve", lambda e: e.tensor_tensor(out=impT[:], in0=impT[:], in1=uimp[:, 384:512], op=ALU.add), reads=["uimp", "impT"], writes=["impT"])
                        P.op("dve", lambda e: e.tensor_tensor(out=rrow[64:65, :], in0=rrow[64:65, :], in1=grow[pi][64:65, 0, :], op=ALU.mult), reads=["rrow", gres], writes=["rrow"])
                        P.op("pe", lambda e: e.matmul(pb[4][0:64, :], lhsT=ones32[64:65, 0:64], rhs=rrow[64:65, :], start=True, stop=True), reads=["rrow", "ones32"], writes=["pb4"])
                        P.op("act", lambda e: e.activation(out=bcs[0:64, :], in_=pb[4][0:64, :], func=AF.Copy), reads=["pb4"], writes=["bcs"])
                        P.op("dve", lambda e: e.tensor_tensor(out=oacc[:], in0=pb[6][0:64, :], in1=bcs[0:64, :], op=ALU.mult), reads=["pb6", "bcs"], writes=["oacc"])
                    U.append(u_imp)

                    def u_top():
                        P.op("pe", lambda e: e.matmul(pb[4][:, 0:64], lhsT=impT[:], rhs=identf[0:64, 0:64], start=True, stop=True), reads=["impT", "identf"], writes=["pb4"])
                        P.op("dve", lambda e: e.tensor_tensor(out=imp[:], in0=pb[4][:, 0:64], in1=smul[pi][:], op=ALU.mult), reads=["pb4", "smul%d" % pi], writes=["imp"])
                        P.op("dve", lambda e: e.tensor_tensor(out=imp[:], in0=imp[:], in1=sadd[pi][:], op=ALU.add), reads=["imp", "sadd%d" % pi], writes=["imp"])
                        P.op("dve", lambda e: e.max(out=m8[:, 0:8], in_=imp[:]), reads=["imp"], writes=["m8"])
                        if topn > 8:
                            P.op("dve", lambda e: e.match_replace(out=imp2[:], in_to_replace=m8[:, 0:8], in_values=imp[:], imm_value=-BIGF), reads=["imp", "m8"], writes=["imp2"])
                            P.op("dve", lambda e: e.max(out=m8[:, 8:16], in_=imp2[:]), reads=["imp2"], writes=["m8"])
                            lo_, hi_ = 8, 8 + (topn - 8)
                        else:
                            lo_, hi_ = 0, topn
                        P.op("dve", lambda e: e.tensor_reduce(out=bis[:, 5:6], in_=m8[:, lo_:hi_], axis=AX.X, op=ALU.min), reads=["m8"], writes=["bis5"])
                        P.op("dve", lambda e: e.tensor_scalar(out=bis[:, 5:6], in0=bis[:, 5:6], scalar1=-1.0e29, scalar2=None, op0=ALU.max), reads=["bis5"], writes=["bis5"])
                        P.op("dve", lambda e: e.tensor_scalar(out=msel[:], in0=imp[:], scalar1=bis[:, 5:6], scalar2=NEG, op0=ALU.is_lt, op1=ALU.mult), reads=["imp", "bis5"], writes=["msel"])
                        P.op("pe", lambda e: e.matmul(pb[4][0:64, 0:128], lhsT=msel[:], rhs=identf[:, :], start=True, stop=True), reads=["msel", "identf"], writes=["pb4"])
                        P.op("act", [(lambda e, h=h: e.activation(out=mselT[:, h * 128:(h + 1) * 128], in_=pb[4][0:64, 0:128], func=AF.Copy)) for h in range(4)],
                             reads=["pb4"], writes=["mselT"])
                    U.append(u_top)

                    def sel_s(j, bk):
                        fns = [lambda e: e.matmul(pb[bk][:], lhsT=EXs[:, j * 128:(j + 1) * 128], rhs=mselT[:], start=True, stop=False)]
                        if j == i:
                            fns.append(lambda e: e.matmul(pb[bk][:], lhsT=masks[:, 1, :], rhs=ident[:], start=False, stop=False))
                        fns.append(lambda e: e.matmul(pb[bk][:], lhsT=KTSs[:, j * 128:(j + 1) * 128], rhs=qc[:], start=False, stop=True))
                        return fns, ["EXs", "mselT", "masks", "ident", "KTSs", qcr]
                    attn_units(U, i + 1, sel_s, lambda j: V3s[1][:, j, :], "V3s1")
                    U.append(lambda: attn_finish(pb[6], "pb6", None, None, (rrow, bcs, (oacc, False)), gate=(grow[pi][64:65, 1, :], gres), bcbank=4))
                    wk0 = max(0, i - 4)

                    def win_s(j, bk):
                        kb = wk0 + j
                        fns = []
                        first = True
                        if kb == i:
                            fns.append(lambda e: e.matmul(pb[bk][:], lhsT=masks[:, 1, :], rhs=ident[:], start=True, stop=False))
                            first = False
                        if kb == i - 4:
                            fns.append(lambda e, first=first: e.matmul(pb[bk][:], lhsT=masks[:, 2, :], rhs=ident[:], start=first, stop=False))
                            first = False
                        fns.append(lambda e, first=first: e.matmul(pb[bk][:], lhsT=KTWs[:, kb * 128:(kb + 1) * 128], rhs=qc[:], start=first, stop=True))
                        return fns, ["masks", "ident", "KTWs", qcr]
                    attn_units(U, i - wk0 + 1, win_s, lambda j: V3s[2][:, wk0 + j, :], "V3s2")

                    def u_end():
                        attn_finish(pb[6], "pb6", None, None, (rrow, bcs, (oacc, False)), gate=(grow[pi][64:65, 2, :], gres), bcbank=4)
                        P.op("act", lambda e: e.activation(out=o_st[:, 2, :], in_=oacc[:], func=AF.Copy), reads=["oacc"], writes=[ostr])
                    U.append(u_end)
                    return U

                def store_o(i):
                    pi = i % 2
                    ts = slice(i * 128, (i + 1) * 128)
                    P.dma("sp", OT.ap()[:, :, ts].rearrange("(b h) d t -> d b h t", b=3), ost[pi][:].rearrange("d b (h t) -> d b h t", h=4),
                          reads=["ost%d" % pi], writes=[], key="ost%d" % pi)

                def run_merged(chains):
                    items = []
                    for ci, U in enumerate(chains):
                        n = len(U)
                        for k, f in enumerate(U):
                            items.append(((k + 0.5) / n, ci, k, f))
                    items.sort(key=lambda t: (t[0], t[1], t[2]))
                    for _, _, _, f in items:
                        f()

                load_block(0)
                run_merged([chain_P(0)])
                for i in range(NB):
                    if i + 1 < NB:
                        load_block(i + 1)
                    chains = [chain_SB(i), chain_ATT(i)]
                    if i + 1 < NB:
                        chains.append(chain_P(i + 1))
                    run_merged(chains)
                    store_o(i)
                P.barrier()
                P.emit_all()
            if debug == "B":
                break
            with ExitStack() as ec:
                sb = lambda n, s, d: ec.enter_context(nc.sbuf_tensor(n + "_L%d" % li, list(s), d))
                alloc_psum(ec, "C%d" % li, 7)
                wup = sb("wup", [64, 12, D], BF16); wout = sb("wout", [128, 8, D], BF16)
                wmq = sb("wmq", [128, 8, 256], BF16); wmo = sb("wmo", [64, 4, D], BF16); wmkv = sb("wmkv", [128, 8, 512], BF16)
                gq = sb("gq", [128, D], F32); gkv = sb("gkv", [128, D], F32)
                KmT = sb("KmT", [64, 4, NMEM], BF16); Vm = sb("Vm", [128, 2, 4, 65], BF16)
                oT = sb("oT", [64, 12, 512], BF16); gT = sb("gT", [128, 24, 512], BF16)
                mT = sb("mT", [128, 8, 512], BF16)
                tm = [sb("tm%d" % i, [128, 512], F32) for i in range(3)]
                hin = [sb("hin%d" % i, [128, D], F32) for i in range(2)]
                h1 = sb("h1", [128, 4, D], F32)
                junk = sb("junkC", [128, D], F32); ubf = sb("ubfC", [128, D], BF16)
                uT2 = sb("uT2", [128, 8, 512], BF16)
                qmT = sb("qmT", [64, 4, 512], BF16)
                Eb = [sb("EbC%d" % i, [128, 512], BF16) for i in range(2)]
                omT = sb("omT", [64, 4, 512], BF16)
                rrow = sb("rrowC", [128, 512], F32); bcs = sb("bcsC", [64, 512], F32)
                hout = [sb("houtC%d" % i, [128, D], F32) for i in range(2)]
                memb = sb("memb", [128, D], F32); mTm = sb("mTm", [128, 8, NMEM], BF16)
                P.dma("pool", wup[:], wup_d.ap()[l].rearrange("b (h d) n -> d (b h) n", d=64), writes=["wup"], key="c1w0")
                P.dma("pool", wout[:], wout_d.ap()[l].rearrange("(k p) n -> p k n", p=128), writes=["wout"], key="c1w1")
                P.dma("pool", wmq[:], wmq_d.ap()[l].rearrange("(k p) n -> p k n", p=128), writes=["wmq"], key="c1w2")
                P.dma("pool", wmo[:], wmo_d.ap()[l].rearrange("(h d) n -> d h n", d=64), writes=["wmo"], key="c1w3")
                P.dma("pool", wmkv[:], wmkv_d.ap()[l].rearrange("(k p) n -> p k n", p=128), writes=["wmkv"], key="c1w4")
                P.dma("sp", gq[:], bcrow(norms_d, 4 * l + 1), writes=["gq"], key="c1g0")
                P.dma("sp", gkv[:], bcrow(norms_d, 4 * l + 2), writes=["gkv"], key="c1g1")
                P.op("pool", lambda e: e.memset(Vm[:], 1.0), writes=["Vm"])
                for mc in range(2):
                    P.dma("sp", memb[:], mem_d.ap()[mc * 128:(mc + 1) * 128, :], writes=["memb"], key="c1mem")
                    rmsnorm_T(memb[:], "memb", gkv, "gkv", mTm[:, :, mc * 128:(mc + 1) * 128], "mTm", (junk, ubf))
                for h in range(4):
                    P.op("pe", [(lambda e, kc=kc, h=h: e.matmul(pb[0][0:64, 0:NMEM], lhsT=wmkv[:, kc, h * 64:(h + 1) * 64], rhs=mTm[:, kc, :],
                                                                start=(kc == 0), stop=(kc == 7))) for kc in range(8)], reads=["wmkv", "mTm"], writes=["pb0"])
                    P.op("act", lambda e, h=h: e.activation(out=KmT[:, h, :], in_=pb[0][0:64, 0:NMEM], func=AF.Copy), reads=["pb0"], writes=["KmT"])
                for mc in range(2):
                    P.op("pe", [(lambda e, kc=kc, mc=mc: e.matmul(pb[1][:, 0:256], lhsT=mTm[:, kc, mc * 128:(mc + 1) * 128], rhs=wmkv[:, kc, 256:512],
                                                                  start=(kc == 0), stop=(kc == 7))) for kc in range(8)], reads=["wmkv", "mTm"], writes=["pb1"])
                    P.op("act", lambda e, mc=mc: e.activation(out=Vm[:, mc, :, 0:64], in_=pb[1][:, 0:256].rearrange("p (h d) -> p h d", h=4), func=AF.Copy),
                         reads=["pb1"], writes=["Vm"])
                for g in range(NG):
                    gs = slice(g * 512, (g + 1) * 512)
                    P.dma("sp", oT[:], OT.ap()[:, :, gs].rearrange("c d s -> d c s"), reads=["scrB"], writes=["oT"], key="c1oT")
                    P.dma("sp", gT[:], GT.ap()[:, gs].rearrange("(c p) s -> p c s", p=128), reads=["scrA"], writes=["gT"], key="c1gT")
                    for fc in range(8):
                        for br in range(3):
                            P.op("pe", [(lambda e, h=h, br=br, fc=fc: e.matmul(pb[br][:], lhsT=wup[:, br * 4 + h, fc * 128:(fc + 1) * 128], rhs=oT[:, br * 4 + h, :],
                                                                                start=(h == 0), stop=(h == 3))) for h in range(4)], reads=["wup", "oT"], writes=["pb%d" % br])
                            P.op("dve", lambda e, br=br, fc=fc: e.tensor_tensor(out=tm[br][:], in0=pb[br][:], in1=gT[:, br * 8 + fc, :], op=ALU.mult),
                                 reads=["pb%d" % br, "gT"], writes=["tm%d" % br])
                        P.op("pool", lambda e: e.tensor_tensor(out=tm[0][:], in0=tm[0][:], in1=tm[1][:], op=ALU.add), reads=["tm0", "tm1"], writes=["tm0"])
                        P.op("pool", lambda e, fc=fc: e.tensor_tensor(out=mT[:, fc, :], in0=tm[0][:], in1=tm[2][:], op=ALU.add), reads=["tm0", "tm2"], writes=["mT"])
                    for b4 in range(4):
                        blk = g * 4 + b4
                        hi_ = hin[blk % 2]
                        P.dma("sp", hi_[:], h_in.ap()[blk * 128:(blk + 1) * 128, :], writes=["hin%d" % (blk % 2)], key="hin%d" % (blk % 2))
                        for half in range(2):
                            bk = 3 + half
                            P.op("pe", [(lambda e, kc=kc, half=half, bk=bk, b4=b4: e.matmul(pb[bk][:], lhsT=mT[:, kc, b4 * 128:(b4 + 1) * 128], rhs=wout[:, kc, half * 512:(half + 1) * 512],
                                                                                          start=(kc == 0), stop=(kc == 7))) for kc in range(8)], reads=["mT", "wout"], writes=["pb%d" % bk])
                            P.op("dve", lambda e, half=half, bk=bk, b4=b4, hi_=hi_: e.tensor_tensor(out=h1[:, b4, half * 512:(half + 1) * 512], in0=pb[bk][:], in1=hi_[:, half * 512:(half + 1) * 512], op=ALU.add),
                                 reads=["pb%d" % bk, "hin%d" % (blk % 2)], writes=["h1_%d" % b4])
                        rmsnorm_T(h1[:, b4, :], "h1_%d" % b4, gq, "gq", uT2[:, :, b4 * 128:(b4 + 1) * 128], "uT2", (junk, ubf))
                    for h in range(4):
                        bk = h % 2
                        P.op("pe", [(lambda e, kc=kc, h=h, bk=bk: e.matmul(pb[bk][0:64, :], lhsT=wmq[:, kc, h * 64:(h + 1) * 64], rhs=uT2[:, kc, :], start=(kc == 0), stop=(kc == 7)))
                                    for kc in range(8)], reads=["wmq", "uT2"], writes=["pb%d" % bk])
                        P.op("act", lambda e, h=h, bk=bk: e.activation(out=qmT[:, h, :], in_=pb[bk][0:64, :], func=AF.Copy), reads=["pb%d" % bk], writes=["qmT"])
                    for h in range(4):
                        for mc in range(2):
                            bk = mc
                            P.op("pe", lambda e, h=h, mc=mc, bk=bk: e.matmul(pb[bk][:], lhsT=KmT[:, h, mc * 128:(mc + 1) * 128], rhs=qmT[:, h, :], start=True, stop=True),
                                 reads=["KmT", "qmT"], writes=["pb%d" % bk])
                            P.op("act", lambda e, mc=mc, bk=bk: e.activation(out=Eb[mc][:], in_=pb[bk][:], func=AF.Exp, scale=0.125), reads=["pb%d" % bk], writes=["EbC%d" % mc])
                            P.op("pe", lambda e, h=h, mc=mc: e.matmul(pb[5][0:65, :], lhsT=Vm[:, mc, h, :], rhs=Eb[mc][:], start=(mc == 0), stop=(mc == 1)),
                                 reads=["Vm", "EbC%d" % mc], writes=["pb5"])
                        attn_finish(pb[5], "pb5", omT[:, h, :], "omT", (rrow, bcs, None))
                    for b4 in range(4):
                        blk = g * 4 + b4
                        ho = hout[blk % 2]
                        for half in range(2):
                            bk = 3 + half
                            P.op("pe", [(lambda e, h=h, half=half, bk=bk, b4=b4: e.matmul(pb[bk][:], lhsT=omT[:, h, b4 * 128:(b4 + 1) * 128], rhs=wmo[:, h, half * 512:(half + 1) * 512],
                                                                                        start=(h == 0), stop=(h == 3))) for h in range(4)], reads=["omT", "wmo"], writes=["pb%d" % bk])
                            P.op("dve", lambda e, half=half, bk=bk, b4=b4, ho=ho: e.tensor_tensor(out=ho[:, half * 512:(half + 1) * 512], in0=pb[bk][:], in1=h1[:, b4, half * 512:(half + 1) * 512], op=ALU.add),
                                 reads=["pb%d" % bk, "h1_%d" % b4], writes=["hout%d" % (blk % 2)])
                        P.dma("sp", HM.ap()[blk * 128:(blk + 1) * 128, :], ho[:], reads=["hout%d" % (blk % 2)], writes=[], key="hout%d" % (blk % 2))
                P.barrier()
                P.emit_all()
            if debug == "C1":
                break
            with ExitStack() as ef:
                sb = lambda n, s, d: ef.enter_context(nc.sbuf_tensor(n + "_L%d" % li, list(s), d))
                alloc_psum(ef, "F%d" % li, 7)
                wfi = sb("wfi", [128, 8, 2 * DFF], BF16); wfo = sb("wfo", [128, 22, D], BF16)
                gf = sb("gf", [128, D], F32); gfin = sb("gfin", [128, D], F32)
                hmb = sb("hmb", [128, 2, D], F32)
                junk = sb("junkF", [128, D], F32); ubf = sb("ubfF", [128, D], BF16)
                uT3 = sb("uT3", [128, 8, 256], BF16)
                sil = [sb("sil%d" % i, [128, 256], F32) for i in range(2)]
                actT = sb("actT", [128, 22, 256], BF16)
                hf = [sb("hf%d" % i, [128, D], F32) for i in range(2)]
                for kc in range(8):
                    P.dma("pool", wfi[:, kc, :], wfi_d.ap()[l, kc * 128:(kc + 1) * 128, :], writes=["wfi"], key="c2w%d" % kc)
                P.dma("pool", wfo[:, 0:11, :], wfo_d.ap()[l, 0:11 * 128, :].rearrange("(c p) n -> p c n", p=128), writes=["wfo"], key="c2wo0")
                P.dma("pool", wfo[:, 11:22, :], wfo_d.ap()[l, 11 * 128:22 * 128, :].rearrange("(c p) n -> p c n", p=128), writes=["wfo"], key="c2wo1")
                P.dma("sp", gf[:], bcrow(norms_d, 4 * l + 3), writes=["gf"], key="c2g0")
                P.dma("sp", gfin[:], bcrow(norms_d, 4 * L), writes=["gfin"], key="c2g1")
                for g2 in range(S // 256):
                    for b2 in range(2):
                        blk = g2 * 2 + b2
                        P.dma("sp", hmb[:, b2, :], HM.ap()[blk * 128:(blk + 1) * 128, :], reads=["scrC"], writes=["hmb%d" % b2], key="hmb%d" % b2)
                        rmsnorm_T(hmb[:, b2, :], "hmb%d" % b2, gf, "gf", uT3[:, :, b2 * 128:(b2 + 1) * 128], "uT3", (junk, ubf))
                    for c in range(22):
                        bk = c % 2
                        P.op("pe", [(lambda e, kc=kc, c=c, bk=bk: e.matmul(pb[bk][:, 0:256], lhsT=wfi[:, kc, c * 128:(c + 1) * 128], rhs=uT3[:, kc, :], start=(kc == 0), stop=(kc == 7)))
                                    for kc in range(8)] +
                                   [(lambda e, kc=kc, c=c, bk=bk: e.matmul(pb[bk][:, 256:512], lhsT=wfi[:, kc, DFF + c * 128:DFF + (c + 1) * 128], rhs=uT3[:, kc, :], start=False, stop=(kc == 7)))
                                    for kc in range(8)], reads=["wfi", "uT3"], writes=["pb%d" % bk])
                        P.op("act", lambda e, bk=bk: e.activation(out=sil[bk][:], in_=pb[bk][:, 0:256], func=AF.Silu), reads=["pb%d" % bk], writes=["sil%d" % bk])
                        P.op("dve", lambda e, bk=bk, c=c: e.tensor_tensor(out=actT[:, c, :], in0=pb[bk][:, 256:512], in1=sil[bk][:], op=ALU.mult),
                             reads=["pb%d" % bk, "sil%d" % bk], writes=["actT%d" % c])
                        for b2 in range(2):
                            for half in range(2):
                                ob = 2 + b2 * 2 + half
                                P.op("pe", lambda e, c=c, b2=b2, half=half, ob=ob: e.matmul(pb[ob][:], lhsT=actT[:, c, b2 * 128:(b2 + 1) * 128], rhs=wfo[:, c, half * 512:(half + 1) * 512],
                                                                                           start=(c == 0), stop=(c == 21)), reads=["actT%d" % c, "wfo"], writes=["pb%d" % ob])
                    for b2 in range(2):
                        blk = g2 * 2 + b2
                        hft = hf[b2]
                        for half in range(2):
                            ob = 2 + b2 * 2 + half
                            P.op("dve", lambda e, ob=ob, half=half, b2=b2, hft=hft: e.tensor_tensor(out=hft[:, half * 512:(half + 1) * 512], in0=pb[ob][:], in1=hmb[:, b2, half * 512:(half + 1) * 512], op=ALU.add),
                                 reads=["pb%d" % ob, "hmb%d" % b2], writes=["hf%d" % b2])
                        if not last:
                            P.dma("sp", HO.ap()[blk * 128:(blk + 1) * 128, :], hft[:], reads=["hf%d" % b2], writes=[], key="hf%d" % b2)
                        else:
                            P.op("act", lambda e, hft=hft: e.activation(out=junk[:], in_=hft[:], func=AF.Square, accum_out=stat[:, 4:5]), reads=["hf%d" % b2], writes=["junk", "stat4"])
                            P.op("act", lambda e: e.activation(out=stat[:, 5:6], in_=stat[:, 4:5], func=AF.Sqrt, scale=1.0 / D, bias=1e-6), reads=["stat4"], writes=["stat5"])
                            P.op("dve", lambda e: e.reciprocal(out=stat[:, 6:7], in_=stat[:, 5:6]), reads=["stat5"], writes=["stat6"])
                            P.op("dve", lambda e, hft=hft: e.scalar_tensor_tensor(out=hft[:], in0=hft[:], scalar=stat[:, 6:7], in1=gfin[:], op0=ALU.mult, op1=ALU.mult),
                                 reads=["hf%d" % b2, "stat6", "gfin"], writes=["hf%d" % b2])
                            P.dma("sp", out_d.ap()[blk * 128:(blk + 1) * 128, :], hft[:], reads=["hf%d" % b2], writes=[], key="hf%d" % b2)
                P.barrier()
                P.emit_all()
    return nc


NBIS = 16

_CACHE = {}


def prep_shared(inputs, S, layers):
    cols = _col_index()
    sh = {}
    w_in = np.asarray(inputs['w_in'], np.float32)
    sh['wA'] = np.ascontiguousarray(np.stack([w_in[l][:, cols] for l in layers], 0))
    for k in ('cmp_w1_k', 'cmp_w2_k', 'cmp_pos_k', 'cmp_w1_v', 'cmp_w2_v', 'cmp_pos_v', 'w_up', 'w_out',
              'w_mem_q', 'w_mem_kv', 'w_mem_o', 'w_ffn_in', 'w_ffn_out'):
        a = np.asarray(inputs[k], np.float32)
        sh[k] = np.ascontiguousarray(np.stack([a[l] for l in layers], 0))
    rows = []
    for l in layers:
        rows += [np.asarray(inputs[k], np.float32)[l] for k in ('norm_mix', 'norm_mem_q', 'norm_mem_kv', 'norm_ffn')]
    rows.append(np.asarray(inputs['norm_final'], np.float32))
    sh['norms'] = np.ascontiguousarray(np.stack(rows, 0))
    sh.update(make_consts(S))
    return sh


def kernel(**inputs):
    x = np.asarray(inputs['x'], np.float32)
    mem = np.asarray(inputs['mem'], np.float32)
    B, S, _ = x.shape
    layers = [0, 1]
    key = (S, tuple(layers))
    if key not in _CACHE:
        _CACHE[key] = build(S, layers)
    nc = _CACHE[key]
    sh = prep_shared(inputs, S, layers)
    in_maps = []
    for b in range(B):
        m = dict(sh)
        m['x'] = np.ascontiguousarray(x[b])
        m['mem'] = np.ascontiguousarray(mem[b])
        in_maps.append(m)
    res = run_bass_kernel_spmd(nc, in_maps, core_ids=list(range(B)))
    return np.stack([np.asarray(r['out'], np.float32) for r in res.results], 0)
```

```python
import numpy as np
import concourse.bass as bass
import concourse.mybir as mybir
from concourse.bass_utils import run_bass_kernel_spmd
from contextlib import ExitStack
import os

F32 = mybir.dt.float32
BF16 = mybir.dt.bfloat16
AF = mybir.ActivationFunctionType
ALU = mybir.AluOpType
AX = mybir.AxisListType

EPOCH = 30000
D = 1024
DFF = 2816
NEG = -30000.0
BIGF = 1.0e30
NMEM = 256
TOPK = 256
NA_FM = 9 * 64 + 17 * 2 * 64 + 12 + 3072
NA = NA_FM + 452


class _Rec:
    def __init__(self):
        self.calls = []

    def __getattr__(self, name):
        def f(*a, **kw):
            self.calls.append((name, a, kw))
            return self
        return f


class Prog:
    ENGS = ("pe", "act", "dve", "pool", "sp")

    def __init__(self, nc, es):
        self.nc = nc
        self.es = es
        self.ops = {e: [] for e in self.ENGS}
        self.cnt = {e: 0 for e in self.ENGS}
        self.seen = {e: {} for e in self.ENGS}
        self.sems = {}
        self.latest = {}
        self.res_w = {}
        self.res_r = {}
        self.dcnt = {}
        self.capture = None

    def sem(self, key):
        if key not in self.sems:
            self.sems[key] = self.es.enter_context(
                self.nc.semaphore("s_" + key.replace("#", "_").replace(":", "_")))
        return self.sems[key]

    def _deps(self, eng, reads, writes):
        deps = {}
        for r in reads:
            ev = self.res_w.get(r)
            if ev is not None:
                deps[ev[0]] = max(deps.get(ev[0], 0), ev[1])
        for w in writes:
            ev = self.res_w.get(w)
            if ev is not None:
                deps[ev[0]] = max(deps.get(ev[0], 0), ev[1])
            for ev in self.res_r.get(w, ()):
                deps[ev[0]] = max(deps.get(ev[0], 0), ev[1])
        out = []
        for k, v in deps.items():
            if eng == "pe" and k.startswith("pe#"):
                continue
            if self.seen[eng].get(k, 0) >= v:
                continue
            self.seen[eng][k] = v
            out.append((k, v))
        return out

    def _commit(self, ev, reads, writes):
        self.latest[ev[0]] = max(self.latest.get(ev[0], 0), ev[1])
        for w in writes:
            self.res_w[w] = ev
            self.res_r[w] = []
        for r in reads:
            if r in writes:
                continue
            lst = self.res_r.setdefault(r, [])
            lst.append(ev)
            if len(lst) > 16:
                d = {}
                for k, v in lst:
                    d[k] = max(d.get(k, 0), v)
                self.res_r[r] = list(d.items())

    def op(self, eng, fns, reads=(), writes=()):
        if callable(fns):
            fns = [fns]
        rec = _Rec()
        for f in fns:
            f(rec)
        calls = rec.calls
        if self.capture is not None:
            self.capture.append(("op", eng, calls, list(reads), list(writes)))
            return None
        return self._op_commit(eng, calls, reads, writes)

    def _op_commit(self, eng, calls, reads, writes):
        px = [r for r in reads if r.startswith("pb") or r == "ptr"]
        if px:
            reads = [r for r in reads if r not in px]
            writes = list(writes) + [r for r in px if r not in writes]
        waits = self._deps(eng, reads, writes)
        c = self.cnt[eng] + 1
        self.cnt[eng] = c
        key = "%s#%d" % (eng, (c - 1) // EPOCH)
        val = (c - 1) % EPOCH + 1
        semh = self.sem(key)
        wl = [(self.sem(k), v) for k, v in waits]

        def emit(e, calls=calls, wl=wl, semh=semh):
            for s, v in wl:
                e.wait_ge(s, v)
            ins = None
            for name, a, kw in calls:
                if name == "matmul":
                    kw = dict(kw)
                    kw["skip_group_check"] = True
                ins = getattr(e, name)(*a, **kw)
            ins.then_inc(semh, 1)

        self.ops[eng].append(emit)
        ev = (key, val)
        self._commit(ev, reads, writes)
        return ev

    def dma(self, q, out, in_, reads=(), writes=(), key=None):
        if self.capture is not None:
            self.capture.append(("dma", q, out, in_, list(reads), list(writes), key))
            return None
        return self._dma_commit(q, out, in_, reads, writes, key)

    def commit(self, item):
        if item[0] == "op":
            return self._op_commit(*item[1:])
        return self._dma_commit(*item[1:])

    def _dma_commit(self, q, out, in_, reads=(), writes=(), key=None):
        waits = self._deps(q, reads, writes)
        dk = "dma:" + key
        semh = self.sem(dk)
        self.dcnt[dk] = self.dcnt.get(dk, 0) + 16
        wl = [(self.sem(k), v) for k, v in waits]

        def emit(e, wl=wl, semh=semh, out=out, in_=in_):
            for s, v in wl:
                e.wait_ge(s, v)
            e.dma_start(out=out, in_=in_).then_inc(semh, 16)

        self.ops[q].append(emit)
        ev = (dk, self.dcnt[dk])
        self._commit(ev, reads, writes)
        return ev

    def barrier(self):
        for eng in self.ENGS:
            wl = []
            for k, v in self.latest.items():
                if self.seen[eng].get(k, 0) >= v:
                    continue
                self.seen[eng][k] = v
                wl.append((self.sem(k), v))

            def emit(e, wl=wl):
                for s, v in wl:
                    e.wait_ge(s, v)

            self.ops[eng].append(emit)

    def emit_all(self):
        nc = self.nc
        ops = self.ops
        with nc.Block() as block:
            @block.tensor
            def _(e):
                for f in ops["pe"]:
                    f(e)

            @block.scalar
            def _(e):
                for f in ops["act"]:
                    f(e)

            @block.vector
            def _(e):
                for f in ops["dve"]:
                    f(e)

            @block.gpsimd
            def _(e):
                for f in ops["pool"]:
                    f(e)

            @block.sync
            def _(e):
                for f in ops["sp"]:
                    f(e)
        self.ops = {e: [] for e in self.ENGS}


OFF = {}
_o = 0
for _n, _w in (('q_a', 256), ('k_a', 256), ('v_a', 256), ('q_b', 256), ('k_b', 64), ('v_b', 64),
               ('q_idx', 256), ('k_idx', 64), ('w_idx', 4), ('q_c', 256), ('k_cmp', 64), ('v_cmp', 64),
               ('k_sel', 64), ('v_sel', 64), ('k_win', 64), ('v_win', 64), ('g_c', 12), ('g_merge', 3072)):
    OFF[_n] = _o
    _o += _w

FM_PLAIN = [('q_a', h) for h in range(4)] + [('k_a', h) for h in range(4)] + [('v_cmp', 0)]
FM_ROPE = ([('q_b', h) for h in range(4)] + [('k_b', 0)] + [('q_idx', h) for h in range(4)] + [('k_idx', 0)] +
           [('q_c', h) for h in range(4)] + [('k_cmp', 0), ('k_sel', 0), ('k_win', 0)])


def _col_index():
    cols = []
    for n, h in FM_PLAIN:
        c0 = OFF[n] + 64 * h
        cols += list(range(c0, c0 + 64))
    for n, h in FM_ROPE:
        c0 = OFF[n] + 64 * h
        cols += list(range(c0, c0 + 64))
        cols += [c0 + (d + 32) % 64 for d in range(64)]
    cols += [OFF['g_c'] + h * 3 + br for br in range(3) for h in range(4)]
    cols += list(range(OFF['g_merge'], OFF['g_merge'] + 3072))
    cols += list(range(OFF['v_a'], OFF['v_a'] + 256))
    cols += list(range(OFF['v_b'], OFF['v_b'] + 64))
    cols += list(range(OFF['v_sel'], OFF['v_sel'] + 64))
    cols += list(range(OFF['v_win'], OFF['v_win'] + 64))
    cols += list(range(OFF['w_idx'], OFF['w_idx'] + 4))
    assert len(cols) == NA
    return np.asarray(cols)


def make_consts(S):
    NB = S // 128
    c = {}
    c['c_ident4'] = np.tile(np.eye(128, dtype=np.float32), (1, 4))
    t = np.arange(128)[:, None]
    s = np.arange(128)[None, :]
    m = np.zeros((128, 5, 128), np.float32)
    m[:, 0, :] = np.where(s >= t, NEG, 0.0)
    m[:, 1, :] = np.where(s > t, NEG, 0.0)
    m[:, 2, :] = np.where(s <= t, NEG, 0.0)
    j = np.arange(128)[:, None]
    m[:, 3, :] = np.where(j >= s, -1.0, 0.0)
    m[:, 4, :] = -1.0
    c['c_masks'] = m
    c['c_tokcausal'] = np.where(s > t, -BIGF, 0.0).astype(np.float32)
    inv = 10000.0 ** (-np.arange(0, 64, 2, dtype=np.float32) / 64)
    ang = np.arange(S, dtype=np.float32)[None, :] * inv[:, None]
    cos = np.cos(ang).astype(np.float32)
    sin = np.sin(ang).astype(np.float32)
    c['c_rope'] = np.stack([np.concatenate([cos, cos], 0), np.concatenate([-sin, sin], 0)], 0)
    n_sel = S // 64
    ex = np.zeros((64, S), np.float32)
    for jj in range(min(n_sel, 64)):
        ex[jj, jj * 64:(jj + 1) * 64] = 1.0
    c['c_expand'] = ex
    n_cmp = (S - 32) // 16 + 1
    ncp = ((n_cmp + 1 + 127) // 128) * 128
    cs = np.arange(n_cmp) * 16
    ss = np.arange(n_sel) * 64
    ov = np.zeros((ncp, 64), np.float32)
    ov[:n_cmp, :n_sel] = ((cs[:, None] < ss[None, :] + 64) & (cs[:, None] + 32 > ss[None, :])).astype(np.float32)
    c['c_ov'] = ov
    tg = np.arange(S)
    n = np.arange(ncp)
    adm = (n[None, :] * 16 + 31 <= tg[:, None]) & (n[None, :] < n_cmp)
    c['c_cmpmask'] = np.where(adm, 0.0, NEG).astype(np.float32).reshape(NB, 128, ncp)
    jj = np.arange(64)
    cur = tg // 64
    forced0 = (jj[None, :] == 0)
    forced1 = (jj[None, :] == cur[:, None])
    forced2 = (jj[None, :] == cur[:, None] - 1)
    forced = forced0 | forced1 | forced2
    admis = (jj[None, :] * 64 <= tg[:, None]) & (jj[None, :] < n_sel)
    mul = np.where(forced | ~admis, 0.0, 1.0).astype(np.float32)
    add = np.zeros((S, 64), np.float32)
    add = np.where(forced0, 10000.0, add)
    add = np.where(forced2, 10001.0, add)
    add = np.where(forced1, 10002.0, add)
    add = np.where(admis, add, -BIGF).astype(np.float32)
    c['c_selmul'] = mul.reshape(NB, 128, 64)
    c['c_seladd'] = add.reshape(NB, 128, 64)
    return c


def build(S, layers, debug=False):
    NB = S // 128
    NG = S // 512
    L = len(layers)
    n_sel = S // 64
    topn = min(16, n_sel)
    topk = min(TOPK, S // 4)
    n_cmp = (S - 32) // 16 + 1
    NCP = ((n_cmp + 1 + 127) // 128) * 128
    NCC = NCP // 128
    nc = bass.Bass("TRN2", target_bir_lowering=False)

    def din(name, shape, dt=F32):
        return nc.dram_tensor(name, list(shape), dt, kind="ExternalInput")

    dbg_kind = "ExternalOutput" if debug else None

    def dscr(name, shape, dt):
        if debug:
            return nc.dram_tensor(name, list(shape), dt, kind="ExternalOutput")
        return nc.dram_tensor(name, list(shape), dt)

    x_d = din("x", [S, D])
    mem_d = din("mem", [NMEM, D])
    wA_d = din("wA", [L, D, NA])
    w1k_d = din("cmp_w1_k", [L, 2048, 128]); w2k_d = din("cmp_w2_k", [L, 128, 64]); posk_d = din("cmp_pos_k", [L, 32, 64])
    w1v_d = din("cmp_w1_v", [L, 2048, 128]); w2v_d = din("cmp_w2_v", [L, 128, 64]); posv_d = din("cmp_pos_v", [L, 32, 64])
    wup_d = din("w_up", [L, 3, 256, D]); wout_d = din("w_out", [L, D, D])
    wmq_d = din("w_mem_q", [L, D, 256]); wmkv_d = din("w_mem_kv", [L, D, 512]); wmo_d = din("w_mem_o", [L, 256, D])
    wfi_d = din("w_ffn_in", [L, D, 2 * DFF]); wfo_d = din("w_ffn_out", [L, DFF, D])
    norms_d = din("norms", [4 * L + 1, D])
    c_ident4 = din("c_ident4", [128, 512]); c_masks = din("c_masks", [128, 5, 128]); c_tokc = din("c_tokcausal", [128, 128])
    c_rope = din("c_rope", [2, 64, S]); c_expand = din("c_expand", [64, S]); c_ov = din("c_ov", [NCP, 64])
    c_cmpmask = din("c_cmpmask", [NB, 128, NCP]); c_selmul = din("c_selmul", [NB, 128, 64]); c_seladd = din("c_seladd", [NB, 128, 64])
    out_d = nc.dram_tensor("out", [S, D], F32, kind="ExternalOutput")

    QTA = dscr("QTA", [4, 64, S], BF16); KTA = dscr("KTA", [4, 64, S], BF16)
    QTB = dscr("QTB", [4, 64, S], BF16); KTB = dscr("KTB", [64, S], BF16)
    QTI = dscr("QTI", [4, 64, S], BF16); KTI = dscr("KTI", [64, S], BF16)
    QTC = dscr("QTC", [4, 64, S], BF16)
    KTCMP = dscr("KTCMP", [64, S], BF16); VTCMP = dscr("VTCMP", [64, S], BF16)
    KTSEL = dscr("KTSEL", [64, S], BF16); KTWIN = dscr("KTWIN", [64, S], BF16)
    VA = dscr("VA", [S, 256], BF16); V3 = dscr("V3", [S, 3, 64], BF16)
    WI = dscr("WI", [S, 4], F32); GC = dscr("GC", [12, S], F32); GT = dscr("GT", [3072, S], BF16)
    OT = dscr("OT", [12, 64, S], BF16)
    HM = dscr("HM", [S, D], F32); HO = dscr("HO", [S, D], F32)
    fm_dst = {}
    for n, h in FM_PLAIN + FM_ROPE:
        fm_dst[(n, h)] = {'q_a': QTA, 'k_a': KTA, 'q_b': QTB, 'q_idx': QTI, 'q_c': QTC}.get(n)
    single = {'v_cmp': VTCMP, 'k_b': KTB, 'k_idx': KTI, 'k_cmp': KTCMP, 'k_sel': KTSEL, 'k_win': KTWIN}

    def bcrow(handle, row, n=D, parts=128):
        return bass.AP(handle, row * n, [[0, parts], [1, n]])

    with ExitStack() as es:
        P = Prog(nc, es)
        sbg = lambda n, s, d: es.enter_context(nc.sbuf_tensor(n, list(s), d))
        psg = lambda n, s, d: es.enter_context(nc.psum_tensor(n, list(s), d))
        pb = [None] * 8
        ptrh = [None]

        def alloc_psum(stk, tag, nfp):
            for i_ in range(nfp):
                pb[i_] = stk.enter_context(nc.psum_tensor("pb%d_%s" % (i_, tag), [128, 512], F32))
            if nfp < 8:
                ptrh[0] = stk.enter_context(nc.psum_tensor("ptr_%s" % tag, [128, 1024], BF16))
        ident = sbg("ident", [128, 512], BF16)
        masks = sbg("masks", [128, 5, 128], BF16)
        ones32 = sbg("ones32", [128, 64], F32)
        stat = sbg("stat", [128, 16], F32)
        P.dma("pool", ident[:], c_ident4.ap(), writes=["ident"], key="c0")
        P.dma("pool", masks[:], c_masks.ap(), writes=["masks"], key="c1")
        P.op("pool", lambda e: e.memset(ones32[:], 1.0), writes=["ones32"])

        def rmsnorm_T(src, src_res, g_bc, g_res, dstT, dst_res, tmp):
            junk, ubf = tmp
            P.op("act", lambda e: e.activation(out=junk[:], in_=src, func=AF.Square, accum_out=stat[:, 0:1]),
                 reads=[src_res], writes=["junk", "stat0"])
            P.op("act", lambda e: e.activation(out=stat[:, 1:2], in_=stat[:, 0:1], func=AF.Sqrt, scale=1.0 / D, bias=1e-6),
                 reads=["stat0"], writes=["stat1"])
            P.op("dve", lambda e: e.reciprocal(out=stat[:, 2:3], in_=stat[:, 1:2]), reads=["stat1"], writes=["stat2"])
            P.op("dve", lambda e: e.scalar_tensor_tensor(out=ubf[:], in0=src, scalar=stat[:, 2:3], in1=g_bc[:],
                                                          op0=ALU.mult, op1=ALU.mult),
                 reads=[src_res, "stat2", g_res], writes=["ubf"])
            P.op("pe", [(lambda e, kc=kc: e.transpose(out=ptrh[0][:, kc * 128:(kc + 1) * 128], in_=ubf[:, kc * 128:(kc + 1) * 128],
                                                      identity=ident[:, 0:128])) for kc in range(8)],
                 reads=["ubf", "ident"], writes=["ptr"])
            P.op("act", lambda e: e.activation(out=dstT, in_=ptrh[0][:].rearrange("p (k t) -> p k t", k=8), func=AF.Copy),
                 reads=["ptr"], writes=[dst_res])

        def attn_finish(obank, obres, dst, dst_res, work, gate=None, guard=False, extra_reads=(), bcbank=6):
            rrow, bcs, accum = work
            P.op("act", lambda e: e.activation(out=rrow[64:65, :], in_=obank[64:65, :], func=AF.Ln), reads=[obres], writes=["rrow"])
            P.op("act", lambda e: e.activation(out=rrow[64:65, :], in_=rrow[64:65, :], func=AF.Exp, scale=-1.0), reads=["rrow"], writes=["rrow"])
            if gate is not None:
                gap, gres = gate
                P.op("dve", lambda e: e.tensor_tensor(out=rrow[64:65, :], in0=rrow[64:65, :], in1=gap, op=ALU.mult),
                     reads=["rrow", gres], writes=["rrow"])
            P.op("pe", lambda e: e.matmul(pb[bcbank][0:64, :], lhsT=ones32[64:65, 0:64], rhs=rrow[64:65, :], start=True, stop=True),
                 reads=["rrow", "ones32"], writes=["pb%d" % bcbank])
            P.op("act", lambda e: e.activation(out=bcs[0:64, :], in_=pb[bcbank][0:64, :], func=AF.Copy), reads=["pb%d" % bcbank], writes=["bcs"])
            if accum is None:
                P.op("dve", lambda e: e.tensor_tensor(out=dst, in0=obank[0:64, :], in1=bcs[0:64, :], op=ALU.mult),
                     reads=[obres, "bcs"] + list(extra_reads), writes=[dst_res])
            else:
                acc, first = accum
                if first:
                    P.op("dve", lambda e: e.tensor_tensor(out=acc[0:64, :], in0=obank[0:64, :], in1=bcs[0:64, :], op=ALU.mult),
                         reads=[obres, "bcs"], writes=["oacc"])
                else:
                    P.op("dve", lambda e: e.tensor_tensor(out=bcs[0:64, :], in0=obank[0:64, :], in1=bcs[0:64, :], op=ALU.mult),
                         reads=[obres, "bcs"], writes=["bcs"])
                    P.op("pool", lambda e: e.tensor_tensor(out=acc[0:64, :], in0=acc[0:64, :], in1=bcs[0:64, :], op=ALU.add),
                         reads=["bcs", "oacc"], writes=["oacc"])

        for li, l in enumerate(layers):
            h_in = x_d if li == 0 else HO
            last = (li == L - 1)
            with ExitStack() as ea:
                sb = lambda n, s, d: ea.enter_context(nc.sbuf_tensor(n + "_L%d" % li, list(s), d))
                alloc_psum(ea, "A%d" % li, 7)
                wA = sb("wA_sb", [128, 8, NA], BF16)
                gA = sb("gA", [128, D], F32)
                rope = sb("rope", [64, 2, S], F32)
                hb = [sb("hbA%d" % i, [128, D], F32) for i in range(2)]
                junk = sb("junkA", [128, D], F32)
                ubf = sb("ubfA", [128, D], BF16)
                uT = sb("uTA", [128, 8, 512], BF16)
                stg = [sb("stgA%d" % i, [128, 512], BF16) for i in range(4)]
                stgf = sb("stgfA", [128, 512], F32)
                t1 = sb("ropeT1", [64, 512], F32)
                t2 = sb("ropeT2", [64, 512], F32)
                for kc in range(8):
                    P.dma("pool", wA[:, kc, :], wA_d.ap()[l, kc * 128:(kc + 1) * 128, :], writes=["wA%d" % kc], key="wA%d" % kc)
                wA_res = ["wA%d" % kc for kc in range(8)]
                P.dma("sp", gA[:], bcrow(norms_d, 4 * l + 0), writes=["gA"], key="gA")
                P.dma("sp", rope[:], c_rope.ap().rearrange("c d s -> d c s"), writes=["rope"], key="rope")
                sctr = [0]

                def stage():
                    i = sctr[0] % 4
                    sctr[0] += 1
                    return stg[i], "stgA%d" % i

                def proj_fm(bank, c0, M):
                    P.op("pe", [(lambda e, kc=kc: e.matmul(pb[bank][0:M, :], lhsT=wA[:, kc, c0:c0 + M], rhs=uT[:, kc, :],
                                                            start=(kc == 0), stop=(kc == 7))) for kc in range(8)],
                         reads=["uT"] + wA_res, writes=["pb%d" % bank])

                LIM = int(os.environ.get("LIMA", "99"))
                for g in range(NG if LIM > 0 else 0):
                    gs = slice(g * 512, (g + 1) * 512)
                    for b4 in range(4):
                        blk = g * 4 + b4
                        hbt = hb[blk % 2]
                        P.dma("sp", hbt[:], h_in.ap()[blk * 128:(blk + 1) * 128, :], writes=["hb%d" % (blk % 2)], key="hb%d" % (blk % 2))
                        rmsnorm_T(hbt[:], "hb%d" % (blk % 2), gA, "gA", uT[:, :, b4 * 128:(b4 + 1) * 128], "uT", (junk, ubf))
                    col = 0
                    bank = 0
                    if LIM <= 1:
                        continue
                    for (n, h) in FM_PLAIN:
                        proj_fm(bank, col, 64)
                        st, sr = stage()
                        sc = 0.125 if n == 'q_a' else 1.0
                        P.op("act", lambda e, bank=bank, st=st, sc=sc: e.activation(out=st[0:64, :], in_=pb[bank][0:64, :], func=AF.Copy, scale=sc),
                             reads=["pb%d" % bank], writes=[sr])
                        dst = fm_dst[(n, h)].ap()[h, :, gs] if fm_dst[(n, h)] is not None else single[n].ap()[:, gs]
                        P.dma("sp", dst, st[0:64, :], reads=[sr], writes=[], key=sr)
                        col += 64
                        bank = (bank + 1) % 6
                    if LIM <= 2:
                        continue
                    for (n, h) in FM_ROPE:
                        b0 = bank
                        b1 = (bank + 1) % 6
                        proj_fm(b0, col, 64)
                        proj_fm(b1, col + 64, 64)
                        st, sr = stage()
                        P.op("dve", lambda e, b0=b0: e.tensor_tensor(out=t1[:], in0=pb[b0][0:64, :], in1=rope[:, 0, gs], op=ALU.mult),
                             reads=["pb%d" % b0, "rope"], writes=["t1"])
                        P.op("dve", lambda e, b1=b1: e.tensor_tensor(out=t2[:], in0=pb[b1][0:64, :], in1=rope[:, 1, gs], op=ALU.mult),
                             reads=["pb%d" % b1, "rope"], writes=["t2"])
                        P.op("pool", lambda e, st=st: e.tensor_tensor(out=st[0:64, :], in0=t1[:], in1=t2[:], op=ALU.add),
                             reads=["t1", "t2"], writes=[sr])
                        dst = fm_dst[(n, h)].ap()[h, :, gs] if fm_dst[(n, h)] is not None else single[n].ap()[:, gs]
                        P.dma("sp", dst, st[0:64, :], reads=[sr], writes=[], key=sr)
                        col += 128
                        bank = (bank + 2) % 6
                    if LIM <= 3:
                        continue
                    proj_fm(bank, col, 12)
                    P.op("act", lambda e, bank=bank: e.activation(out=stgf[0:12, :], in_=pb[bank][0:12, :], func=AF.Sigmoid),
                         reads=["pb%d" % bank], writes=["stgf"])
                    P.dma("sp", GC.ap()[:, gs], stgf[0:12, :], reads=["stgf"], writes=[], key="stgf")
                    col += 12
                    bank = (bank + 1) % 6
                    for gg in range(24):
                        proj_fm(bank, col, 128)
                        st, sr = stage()
                        P.op("act", lambda e, bank=bank, st=st: e.activation(out=st[:], in_=pb[bank][:], func=AF.Sigmoid),
                             reads=["pb%d" % bank], writes=[sr])
                        P.dma("sp", GT.ap()[gg * 128:(gg + 1) * 128, gs], st[:], reads=[sr], writes=[], key=sr)
                        col += 128
                        bank = (bank + 1) % 6
                    assert col == NA_FM
                    if LIM <= 4:
                        continue
                    for b4 in range(4):
                        blk = g * 4 + b4
                        P.op("pe", [(lambda e, kc=kc, b4=b4, bank=bank: e.matmul(pb[bank][:, 0:452], lhsT=uT[:, kc, b4 * 128:(b4 + 1) * 128],
                                                                                  rhs=wA[:, kc, NA_FM:NA], start=(kc == 0), stop=(kc == 7)))
                                    for kc in range(8)], reads=["uT"] + wA_res, writes=["pb%d" % bank])
                        st, sr = stage()
                        P.op("act", lambda e, bank=bank, st=st: e.activation(out=st[:, 0:448], in_=pb[bank][:, 0:448], func=AF.Copy),
                             reads=["pb%d" % bank], writes=[sr])
                        P.op("dve", lambda e, bank=bank: e.tensor_scalar(out=stgf[:, 0:4], in0=pb[bank][:, 448:452], scalar1=1.0 / 16, scalar2=None, op0=ALU.mult),
                             reads=["pb%d" % bank], writes=["stgf"])
                        P.dma("sp", VA.ap()[blk * 128:(blk + 1) * 128, :], st[:, 0:256], reads=[sr], writes=[], key=sr)
                        P.dma("sp", V3.ap()[blk * 128:(blk + 1) * 128, :, :], st[:, 256:448].rearrange("p (a d) -> p a d", a=3), reads=[sr], writes=[], key=sr)
                        P.dma("sp", WI.ap()[blk * 128:(blk + 1) * 128, :], stgf[:, 0:4], reads=["stgf"], writes=[], key="stgf")
                        bank = (bank + 1) % 6
                P.barrier()
                P.emit_all()
            if debug == "A":
                break
            with ExitStack() as eb:
                sb = lambda n, s, d: eb.enter_context(nc.sbuf_tensor(n + "_L%d" % li, list(s), d))
                alloc_psum(eb, "B%d" % li, 8)
                KTAs = sb("KTAs", [64, 4, S], BF16); VAs = sb("VAs", [128, NB, 256], BF16)
                KTBs = sb("KTBs", [64, S], BF16); KTIs = sb("KTIs", [64, S], BF16)
                KTSs = sb("KTSs", [64, S], BF16); KTWs = sb("KTWs", [64, S], BF16)
                V3s = [sb("V3s%d" % j, [128, NB, 65], BF16) for j in range(3)]
                EXs = sb("EXs", [64, S], BF16)
                KCT = sb("KCT", [64, NCP], BF16); VCs = sb("VCs", [128, NCC, 65], BF16); OVs = sb("OVs", [128, NCC, 64], BF16)
                tokc = sb("tokc", [128, 128], F32)
                score = sb("score", [128, S], F32); junkb = sb("junkb", [128, S], BF16); mneg = [sb("mneg%d" % i_, [128, S], BF16) for i_ in range(2)]
                QA = [sb("QA%d" % i, [64, 512], BF16) for i in range(2)]
                QB = [sb("QB%d" % i, [64, 512], BF16) for i in range(2)]
                QI = [sb("QI%d" % i, [64, 512], BF16) for i in range(2)]
                QC = [sb("QC%d" % i, [64, 512], BF16) for i in range(2)]
                wi = [sb("wi%d" % i, [128, 4], F32) for i in range(2)]
                grow = [sb("grow%d" % i_, [128, 3, 512], F32) for i_ in range(2)]
                cmk = [sb("cmk%d" % i, [128, NCP], BF16) for i in range(2)]
                smul = [sb("smul%d" % i, [128, 64], F32) for i in range(2)]
                sadd = [sb("sadd%d" % i, [128, 64], F32) for i in range(2)]
                e1 = [sb("e1_%d" % i, [128, 512], F32) for i in range(2)]
                sp_ = [sb("sp_%d" % i, [128, 512], BF16) for i in range(2)]
                asb = [sb("asb%d" % i, [128, 512], F32) for i in range(2)]
                Wt = [sb("Wt%d" % i, [128, 512], BF16) for i in range(2)]
                chi = sb("chi", [2, 512], BF16); crow2 = sb("crow2", [2, 512], BF16); negm = sb("negm", [2, 1], F32); onesb = sb("onesb", [2, 128], BF16)
                P.op("pool", lambda e: e.memset(negm[0:2, :], -1.0), writes=["negm"])
                P.op("pool", lambda e: e.memset(negm[0:1, :], 0.0), writes=["negm"])
                P.op("pool", lambda e: e.memset(onesb[:], 1.0), writes=["onesb"])
                Eb = [sb("Eb%d" % i, [128, 512], BF16) for i in range(3)]
                tmpi = [sb("tmpi%d" % i_, [128, 512], F32) for i_ in range(2)]
                rrow = sb("rrow", [128, 512], F32); bcs = sb("bcs", [64, 512], F32); oacc = sb("oacc", [64, 512], F32)
                ost = [sb("ost%d" % i, [64, 3, 512], BF16) for i in range(2)]
                bis = sb("bis", [128, 8], F32)
                impT = sb("impT", [64, 128], F32); uimp = sb("uimp", [64, 512], F32)
                imp = sb("imp", [128, 64], F32); imp2 = sb("imp2", [128, 64], F32); m8 = sb("m8", [128, 16], F32)
                msel = sb("msel", [128, 64], F32); mselT = sb("mselT", [64, 512], BF16)
                identf = sb("identf", [128, 128], F32)
                w2 = sb("w2", [128, 64], BF16); posT = sb("posT", [64, 32], BF16)
                w1 = (mneg[0][0:64, 0:4096] if S >= 4096 else sb("w1x", [64, 4096], BF16)[:, :]).rearrange("d (a h) -> d a h", a=32)
                TCs = junkb[0:64, :]
                hid = sb("hid", [128, NCP], BF16); bcol = sb("bcol", [128, 1], F32)

                P.dma("sp", KTAs[:], KTA.ap().rearrange("h d s -> d h s"), reads=["scrA"], writes=["KTAs"], key="ldB0")
                P.dma("sp", VAs[:], VA.ap().rearrange("(b p) c -> p b c", p=128), reads=["scrA"], writes=["VAs"], key="ldB1")
                P.dma("sp", KTBs[:], KTB.ap(), reads=["scrA"], writes=["KTBs"], key="ldB2")
                P.dma("sp", KTIs[:], KTI.ap(), reads=["scrA"], writes=["KTIs"], key="ldB3")
                P.dma("sp", KTSs[:], KTSEL.ap(), reads=["scrA"], writes=["KTSs"], key="ldB4")
                P.dma("sp", KTWs[:], KTWIN.ap(), reads=["scrA"], writes=["KTWs"], key="ldB5")
                for j in range(3):
                    P.op("pool", lambda e, j=j: e.memset(V3s[j][:], 1.0), writes=["V3s%d" % j])
                    P.dma("sp", V3s[j][:, :, 0:64], V3.ap()[:, j, :].rearrange("(b p) d -> p b d", p=128), reads=["scrA"], writes=["V3s%d" % j], key="ldB6%d" % j)
                P.dma("pool", EXs[:], c_expand.ap(), writes=["EXs"], key="ldB7")
                P.dma("pool", OVs[:], c_ov.ap().rearrange("(c p) j -> p c j", p=128), writes=["OVs"], key="ldB8")
                P.dma("sp", tokc[:], c_tokc.ap(), writes=["tokc"], key="ldB9")
                P.dma("sp", identf[:], c_ident4.ap()[:, 0:128], writes=["identf"], key="ldB10")
                P.op("pool", lambda e: e.memset(VCs[:], 1.0), writes=["VCs"])
                for which in range(2 if int(os.environ.get("LIMB", "99")) > 0 else 0):
                    w1_d, w2_d, pos_d, src = ((w1k_d, w2k_d, posk_d, KTCMP), (w1v_d, w2v_d, posv_d, VTCMP))[which]
                    P.dma("pool", w1, w1_d.ap()[l].rearrange("(a d) h -> d a h", d=64), writes=["mneg0"], key="cw1")
                    P.dma("pool", w2[:], w2_d.ap()[l], writes=["w2"], key="cw2")
                    for a in range(32):
                        P.dma("pool", posT[:, a:a + 1], pos_d.ap()[l, a:a + 1, :].rearrange("a d -> d a"), writes=["posT"], key="cpos")
                    P.dma("sp", TCs, src.ap(), reads=["scrA"], writes=["junkb"], key="cTC")
                    P.op("pe", [(lambda e, a=a: e.matmul(pb[1][:, 0:1], lhsT=w1[:, a, :], rhs=posT[:, a:a + 1], start=(a == 0), stop=(a == 31)))
                                for a in range(32)], reads=["mneg0", "posT"], writes=["pb1"])
                    P.op("act", lambda e: e.activation(out=bcol[:], in_=pb[1][:, 0:1], func=AF.Copy), reads=["pb1"], writes=["bcol"])
                    P.op("pe", [(lambda e, a=a: e.matmul(pb[0][:, 0:n_cmp], lhsT=w1[:, a, :], rhs=TCs[:, a:a + 16 * (n_cmp - 1) + 1:16],
                                                         start=(a == 0), stop=(a == 31))) for a in range(32)],
                         reads=["mneg0", "junkb"], writes=["pb0"])
                    P.op("pool", lambda e: e.memset(hid[:], 0.0), writes=["hid"])
                    P.op("act", lambda e: e.activation(out=hid[:, 0:n_cmp], in_=pb[0][:, 0:n_cmp], func=AF.Gelu_apprx_tanh, bias=bcol[:, 0:1]),
                         reads=["pb0", "bcol"], writes=["hid"])
                    if which == 0:
                        P.op("pe", lambda e: e.matmul(pb[2][0:64, 0:NCP], lhsT=w2[:], rhs=hid[:], start=True, stop=True),
                             reads=["w2", "hid"], writes=["pb2"])
                        P.op("act", lambda e: e.activation(out=KCT[:], in_=pb[2][0:64, 0:NCP], func=AF.Copy), reads=["pb2"], writes=["KCT"])
                    else:
                        for c in range(NCC):
                            P.op("pe", lambda e, c=c: e.matmul(pb[2][:, 0:64], lhsT=hid[:, c * 128:(c + 1) * 128], rhs=w2[:], start=True, stop=True),
                                 reads=["w2", "hid"], writes=["pb2"])
                            P.op("act", lambda e, c=c: e.activation(out=VCs[:, c, 0:64], in_=pb[2][:, 0:64], func=AF.Copy), reads=["pb2"], writes=["VCs"])

                ectr = [0]
                tctr = [0]
                LIMB = int(os.environ.get("LIMB", "99"))

                def load_block(i):
                    pi = i % 2
                    ts = slice(i * 128, (i + 1) * 128)
                    for (qt, qn, src) in ((QI, "QI", QTI), (QA, "QA", QTA), (QB, "QB", QTB), (QC, "QC", QTC)):
                        P.dma("sp", qt[pi][:].rearrange("d (h t) -> d h t", h=4), src.ap()[:, :, ts].rearrange("h d t -> d h t"),
                              reads=[], writes=["%s%d" % (qn, pi)], key="%s%d" % (qn, pi))
                    P.dma("sp", wi[pi][:], WI.ap()[ts, :], writes=["wi%d" % pi], key="wi%d" % pi)
                    P.dma("sp", grow[pi][64:65, :, :].rearrange("o b (h t) -> o b h t", h=4),
                          GC.ap()[:, ts].rearrange("(o b h) t -> o b h t", o=1, b=3), writes=["grow%d" % pi], key="grow%d" % pi)
                    P.dma("pool", cmk[pi][:], c_cmpmask.ap()[i], writes=["cmk%d" % pi], key="cmk%d" % pi)
                    P.dma("sp", smul[pi][:], c_selmul.ap()[i], writes=["smul%d" % pi], key="smul%d" % pi)
                    P.dma("sp", sadd[pi][:], c_seladd.ap()[i], writes=["sadd%d" % pi], key="sadd%d" % pi)

                def chain_P(i):
                    U = []
                    pi = i % 2
                    Lk = (i + 1) * 128
                    qi = QI[pi]
                    qir = "QI%d" % pi
                    mn = mneg[pi]
                    mres = "mneg%d" % pi
                    wres = "wi%d" % pi
                    nch = (Lk + 511) // 512
                    for c in range(nch):
                        w_ = min(512, Lk - c * 512)
                        cs = slice(c * 512, c * 512 + w_)
                        for h in range(4):
                            def u(c=c, h=h, w_=w_, cs=cs):
                                P.op("pe", lambda e: e.matmul(pb[7][:, 0:w_], lhsT=qi[:, h * 128:(h + 1) * 128], rhs=KTIs[:, cs], start=True, stop=True),
                                     reads=[qir, "KTIs"], writes=["pb7"])
                                if h == 0:
                                    P.op("dve", lambda e: e.tensor_scalar(out=score[:, cs], in0=pb[7][:, 0:w_], scalar1=0.0, scalar2=wi[pi][:, 0:1], op0=ALU.max, op1=ALU.mult),
                                         reads=["pb7", wres], writes=["score"])
                                else:
                                    tb = tctr[0] % 2
                                    tctr[0] += 1
                                    P.op("dve", lambda e: e.tensor_scalar(out=tmpi[tb][:, 0:w_], in0=pb[7][:, 0:w_], scalar1=0.0, scalar2=wi[pi][:, h:h + 1], op0=ALU.max, op1=ALU.mult),
                                         reads=["pb7", wres], writes=["tmpi%d" % tb])
                                    P.op("pool", lambda e: e.tensor_tensor(out=score[:, cs], in0=score[:, cs], in1=tmpi[tb][:, 0:w_], op=ALU.add),
                                         reads=["tmpi%d" % tb, "score"], writes=["score"])
                            U.append(u)

                    def u_pre():
                        if Lk > topk:
                            P.op("dve", lambda e: e.tensor_reduce(out=bis[:, 0:1], in_=score[:, 0:Lk], axis=AX.X, op=ALU.min), reads=["score"], writes=["bis"])
                            P.op("dve", lambda e: e.tensor_reduce(out=bis[:, 1:2], in_=score[:, 0:Lk], axis=AX.X, op=ALU.max), reads=["score"], writes=["bis"])
                            P.op("dve", lambda e: e.tensor_tensor(out=bis[:, 1:2], in0=bis[:, 1:2], in1=bis[:, 0:1], op=ALU.subtract), reads=["bis"], writes=["bis"])
                        P.op("pool", lambda e: e.tensor_tensor(out=score[:, i * 128:(i + 1) * 128], in0=score[:, i * 128:(i + 1) * 128], in1=tokc[:], op=ALU.add),
                             reads=["score", "tokc"], writes=["score"])
                    U.append(u_pre)
                    if Lk > topk:
                        def u_it():
                            P.op("dve", lambda e: e.tensor_scalar(out=bis[:, 1:2], in0=bis[:, 1:2], scalar1=0.5, scalar2=None, op0=ALU.mult), reads=["bis"], writes=["bis"])
                            P.op("dve", lambda e: e.tensor_tensor(out=bis[:, 2:3], in0=bis[:, 0:1], in1=bis[:, 1:2], op=ALU.add), reads=["bis"], writes=["bis"])
                            P.op("dve", lambda e: e.tensor_scalar(out=junkb[:, 0:Lk], in0=score[:, 0:Lk], scalar1=bis[:, 2:3], scalar2=None, op0=ALU.is_ge, op1=ALU.add,
                                                                  accum_out=bis[:, 3:4]), reads=["score", "bis"], writes=["junkb", "bis"])
                            P.op("dve", lambda e: e.tensor_scalar(out=bis[:, 4:5], in0=bis[:, 3:4], scalar1=float(topk) - 0.5, scalar2=None, op0=ALU.is_ge), reads=["bis"], writes=["bis"])
                            P.op("dve", lambda e: e.scalar_tensor_tensor(out=bis[:, 0:1], in0=bis[:, 4:5], scalar=bis[:, 1:2], in1=bis[:, 0:1], op0=ALU.mult, op1=ALU.add),
                                 reads=["bis"], writes=["bis"])
                        for it in range(NBIS):
                            U.append(u_it)

                        def u_fin():
                            P.op("dve", lambda e: e.tensor_scalar(out=mn[:, 0:Lk], in0=score[:, 0:Lk], scalar1=bis[:, 0:1], scalar2=NEG, op0=ALU.is_lt, op1=ALU.mult),
                                 reads=["score", "bis"], writes=[mres])
                    else:
                        def u_fin():
                            P.op("dve", lambda e: e.tensor_scalar(out=mn[:, 0:Lk], in0=score[:, 0:Lk], scalar1=-1.0e29, scalar2=NEG, op0=ALU.is_lt, op1=ALU.mult),
                                 reads=["score"], writes=[mres])
                    U.append(u_fin)
                    return U

                def chain_SB(i):
                    U = []
                    pi = i % 2
                    qa = QA[pi]
                    qar = "QA%d" % pi
                    o_st = ost[pi]
                    ostr = "ost%d" % pi
                    kbs = list(range(i, -1, -1))
                    n = len(kbs)

                    def z_ops(kb, bank, start_first):
                        fns = []
                        first = start_first
                        if kb == i:
                            fns.append(lambda e, first=first: e.matmul(pb[bank][:], lhsT=masks[:, 0, :], rhs=ident[:], start=first, stop=False))
                            first = False
                        for h in range(4):
                            fns.append(lambda e, h=h, first=first: e.matmul(
                                pb[bank][:, h * 128:(h + 1) * 128], lhsT=KTAs[:, h, kb * 128:(kb + 1) * 128], rhs=qa[:, h * 128:(h + 1) * 128],
                                start=first, stop=(h == 3)))
                            first = False
                        return fns

                    def stage1(j):
                        kb = kbs[j]
                        bk = j % 2
                        P.op("pe", z_ops(kb, bk, True), reads=["KTAs", qar, "masks", "ident"], writes=["pb%d" % bk])
                        P.op("act", lambda e: e.activation(out=e1[bk][:], in_=pb[bk][:], func=AF.Exp), reads=["pb%d" % bk], writes=["e1_%d" % bk])
                        P.op("act", lambda e: e.activation(out=sp_[bk][:], in_=e1[bk][:], func=AF.Ln, bias=1.0), reads=["e1_%d" % bk], writes=["sp_%d" % bk])

                    def stage2(j):
                        kb = kbs[j]
                        bk = j % 2
                        fns = [lambda e: e.matmul(pb[bk][:], lhsT=masks[:, 3, :], rhs=sp_[bk][:], start=False, stop=(j == 0))]
                        rds = ["sp_%d" % bk, "masks"]
                        if j > 0:
                            P.op("act", lambda e: e.activation(out=chi[0:2, :], in_=pb[2][0:2, :], func=AF.Copy), reads=["pb2"], writes=["chi"])
                            P.op("dve", lambda e: e.scalar_tensor_tensor(out=crow2[0:2, :], in0=chi[0:2, :], scalar=negm[0:2, 0:1], in1=pb[2][0:2, :],
                                                                          op0=ALU.mult, op1=ALU.add), reads=["chi", "negm", "pb2"], writes=["crow2"])
                            fns.append(lambda e: e.matmul(pb[bk][:], lhsT=onesb[0:2, :], rhs=crow2[0:2, :], start=False, stop=True))
                            rds += ["onesb", "crow2"]
                        P.op("pe", fns, reads=rds, writes=["pb%d" % bk])
                        P.op("pe", lambda e: e.matmul(pb[2][:], lhsT=masks[:, 4, :], rhs=sp_[bk][:], start=(j == 0), stop=(j == n - 1)),
                             reads=["sp_%d" % bk, "masks"], writes=["pb2"])
                        P.op("act", lambda e: e.activation(out=Wt[bk][:], in_=pb[bk][:], func=AF.Exp), reads=["pb%d" % bk], writes=["Wt%d" % bk])
                        P.op("pe", [(lambda e, h=h: e.matmul(pb[3][0:64, h * 128:(h + 1) * 128], lhsT=VAs[:, kb, h * 64:(h + 1) * 64],
                                                             rhs=Wt[bk][:, h * 128:(h + 1) * 128], start=(j == 0 and h == 0), stop=(j == n - 1)))
                                    for h in range(4)], reads=["Wt%d" % bk, "VAs"], writes=["pb3"])

                    def u0():
                        stage1(0)
                    U.append(u0)
                    for j in range(n):
                        def u(j=j):
                            if j + 1 < n:
                                stage1(j + 1)
                            stage2(j)
                        U.append(u)

                    def ufin():
                        P.op("act", lambda e: e.activation(out=o_st[:, 0, :], in_=pb[3][0:64, :], func=AF.Copy), reads=["pb3"], writes=[ostr])
                    U.append(ufin)
                    return U

                def attn_units(U, nsteps, s_ops, v_ap, v_res, scale=0.125):
                    def emitS(j):
                        bk = 4 + j % 2
                        fns, rd = s_ops(j, bk)
                        P.op("pe", fns, reads=rd, writes=["pb%d" % bk])

                    def unit(j):
                        if j == 0:
                            emitS(0)
                        if j + 1 < nsteps:
                            emitS(j + 1)
                        bk = 4 + j % 2
                        eb_i = ectr[0] % 3
                        ectr[0] += 1
                        E = Eb[eb_i]
                        P.op("act", lambda e: e.activation(out=E[:], in_=pb[bk][:], func=AF.Exp, scale=scale),
                             reads=["pb%d" % bk], writes=["Eb%d" % eb_i])
                        P.op("pe", lambda e: e.matmul(pb[6][0:65, :], lhsT=v_ap(j), rhs=E[:], start=(j == 0), stop=(j == nsteps - 1)),
                             reads=["Eb%d" % eb_i, v_res], writes=["pb6"])
                    for j in range(nsteps):
                        U.append((lambda j=j: unit(j)))

                def chain_ATT(i):
                    U = []
                    pi = i % 2
                    ts = slice(i * 128, (i + 1) * 128)
                    qb, qc = QB[pi], QC[pi]
                    qbr, qcr = "QB%d" % pi, "QC%d" % pi
                    o_st = ost[pi]
                    ostr = "ost%d" % pi
                    mn = mneg[pi]
                    mres = "mneg%d" % pi
                    gres = "grow%d" % pi

                    def dsa_s(j, bk):
                        return ([lambda e: e.matmul(pb[bk][:], lhsT=mn[:, j * 128:(j + 1) * 128], rhs=ident[:], start=True, stop=False),
                                 lambda e: e.matmul(pb[bk][:], lhsT=KTBs[:, j * 128:(j + 1) * 128], rhs=qb[:], start=False, stop=True)],
                                [mres, "ident", "KTBs", qbr])
                    attn_units(U, i + 1, dsa_s, lambda j: V3s[0][:, j, :], "V3s0")
                    U.append(lambda: attn_finish(pb[6], "pb6", o_st[:, 1, :], ostr, (rrow, bcs, None), bcbank=4))

                    def u_cmp():
                        for j in range(NCC):
                            P.op("pe", [lambda e: e.matmul(pb[4][:], lhsT=cmk[pi][:, j * 128:(j + 1) * 128], rhs=ident[:], start=True, stop=False),
                                        lambda e: e.matmul(pb[4][:], lhsT=KCT[:, j * 128:(j + 1) * 128], rhs=qc[:], start=False, stop=True)],
                                 reads=["cmk%d" % pi, "ident", "KCT", qcr], writes=["pb4"])
                            eb_i = ectr[0] % 3
                            ectr[0] += 1
                            E = Eb[eb_i]
                            P.op("act", lambda e: e.activation(out=E[:], in_=pb[4][:], func=AF.Exp, scale=0.125), reads=["pb4"], writes=["Eb%d" % eb_i])
                            P.op("pe", lambda e: e.matmul(pb[6][0:65, :], lhsT=VCs[:, j, :], rhs=E[:], start=(j == 0), stop=(j == NCC - 1)),
                                 reads=["Eb%d" % eb_i, "VCs"], writes=["pb6"])
                            P.op("pe", lambda e: e.matmul(pb[5][0:64, :], lhsT=OVs[:, j, :], rhs=E[:], start=(j == 0), stop=(j == NCC - 1)),
                                 reads=["Eb%d" % eb_i, "OVs"], writes=["pb5"])
                    U.append(u_cmp)

                    def u_imp():
                        P.op("dve", lambda e: e.tensor_scalar(out=rrow[64:65, :], in0=pb[6][64:65, :], scalar1=1e-30, scalar2=None, op0=ALU.max), reads=["pb6"], writes=["rrow"])
                        P.op("act", lambda e: e.activation(out=rrow[64:65, :], in_=rrow[64:65, :], func=AF.Ln), reads=["rrow"], writes=["rrow"])
                        P.op("act", lambda e: e.activation(out=rrow[64:65, :], in_=rrow[64:65, :], func=AF.Exp, scale=-1.0), reads=["rrow"], writes=["rrow"])
                        P.op("pe", lambda e: e.matmul(pb[4][0:64, :], lhsT=ones32[64:65, 0:64], rhs=rrow[64:65, :], start=True, stop=True), reads=["rrow", "ones32"], writes=["pb4"])
                        P.op("act", lambda e: e.activation(out=bcs[0:64, :], in_=pb[4][0:64, :], func=AF.Copy), reads=["pb4"], writes=["bcs"])
                        P.op("dve", lambda e: e.tensor_tensor(out=uimp[:], in0=pb[5][0:64, :], in1=bcs[0:64, :], op=ALU.mult), reads=["pb5", "bcs"], writes=["uimp"])
                        P.op("dve", lambda e: e.tensor_tensor(out=impT[:], in0=uimp[:, 0:128], in1=uimp[:, 128:256], op=ALU.add), reads=["uimp"], writes=["impT"])
                        P.op("dve", lambda e: e.tensor_tensor(out=impT[:], in0=impT[:], in1=uimp[:, 256:384], op=ALU.add), reads=["uimp", "impT"], writes=["impT"])
                        P.op("dve", lambda e: e.tensor_tensor(out=impT[:], in0=impT[:], in1=uimp[:, 384:512], op=ALU.add), reads=["uimp", "impT"], writes=["impT"])
                        P.op("dve", lambda e: e.tensor_tensor(out=rrow[64:65, :], in0=rrow[64:65, :], in1=grow[pi][64:65, 0, :], op=ALU.mult), reads=["rrow", gres], writes=["rrow"])
                        P.op("pe", lambda e: e.matmul(pb[4][0:64, :], lhsT=ones32[64:65, 0:64], rhs=rrow[64:65, :], start=True, stop=True), reads=["rrow", "ones32"], writes=["pb4"])
                        P.op("act", lambda e: e.activation(out=bcs[0:64, :], in_=pb[4][0:64, :], func=AF.Copy), reads=["pb4"], writes=["bcs"])
                        P.op("dve", lambda e: e.tensor_tensor(out=oacc[:], in0=pb[6][0:64, :], in1=bcs[0:64, :], op=ALU.mult), reads=["pb6", "bcs"], writes=["oacc"])
                    U.append(u_imp)

                    def u_top():
                        P.op("pe", lambda e: e.matmul(pb[4][:, 0:64], lhsT=impT[:], rhs=identf[0:64, 0:64], start=True, stop=True), reads=["impT", "identf"], writes=["pb4"])
                        P.op("dve", lambda e: e.tensor_tensor(out=imp[:], in0=pb[4][:, 0:64], in1=smul[pi][:], op=ALU.mult), reads=["pb4", "smul%d" % pi], writes=["imp"])
                        P.op("dve", lambda e: e.tensor_tensor(out=imp[:], in0=imp[:], in1=sadd[pi][:], op=ALU.add), reads=["imp", "sadd%d" % pi], writes=["imp"])
                        P.op("dve", lambda e: e.max(out=m8[:, 0:8], in_=imp[:]), reads=["imp"], writes=["m8"])
                        if topn > 8:
                            P.op("dve", lambda e: e.match_replace(out=imp2[:], in_to_replace=m8[:, 0:8], in_values=imp[:], imm_value=-BIGF), reads=["imp", "m8"], writes=["imp2"])
                            P.op("dve", lambda e: e.max(out=m8[:, 8:16], in_=imp2[:]), reads=["imp2"], writes=["m8"])
                            lo_, hi_ = 8, 8 + (topn - 8)
                        else:
                            lo_, hi_ = 0, topn
                        P.op("dve", lambda e: e.tensor_reduce(out=bis[:, 5:6], in_=m8[:, lo_:hi_], axis=AX.X, op=ALU.min), reads=["m8"], writes=["bis5"])
                        P.op("dve", lambda e: e.tensor_scalar(out=bis[:, 5:6], in0=bis[:, 5:6], scalar1=-1.0e29, scalar2=None, op0=ALU.max), reads=["bis5"], writes=["bis5"])
                        P.op("dve", lambda e: e.tensor_scalar(out=msel[:], in0=imp[:], scalar1=bis[:, 5:6], scalar2=NEG, op0=ALU.is_lt, op1=ALU.mult), reads=["imp", "bis5"], writes=["msel"])
                        P.op("pe", lambda e: e.matmul(pb[4][0:64, 0:128], lhsT=msel[:], rhs=identf[:, :], start=True, stop=True), reads=["msel", "identf"], writes=["pb4"])
                        P.op("act", [(lambda e, h=h: e.activation(out=mselT[:, h * 128:(h + 1) * 128], in_=pb[4][0:64, 0:128], func=AF.Copy)) for h in range(4)],
                             reads=["pb4"], writes=["mselT"])
                    U.append(u_top)

                    def sel_s(j, bk):
                        fns = [lambda e: e.matmul(pb[bk][:], lhsT=EXs[:, j * 128:(j + 1) * 128], rhs=mselT[:], start=True, stop=False)]
                        if j == i:
                            fns.append(lambda e: e.matmul(pb[bk][:], lhsT=masks[:, 1, :], rhs=ident[:], start=False, stop=False))
                        fns.append(lambda e: e.matmul(pb[bk][:], lhsT=KTSs[:, j * 128:(j + 1) * 128], rhs=qc[:], start=False, stop=True))
                        return fns, ["EXs", "mselT", "masks", "ident", "KTSs", qcr]
                    attn_units(U, i + 1, sel_s, lambda j: V3s[1][:, j, :], "V3s1")
                    U.append(lambda: attn_finish(pb[6], "pb6", None, None, (rrow, bcs, (oacc, False)), gate=(grow[pi][64:65, 1, :], gres), bcbank=4))
                    wk0 = max(0, i - 4)

                    def win_s(j, bk):
                        kb = wk0 + j
                        fns = []
                        first = True
                        if kb == i:
                            fns.append(lambda e: e.matmul(pb[bk][:], lhsT=masks[:, 1, :], rhs=ident[:], start=True, stop=False))
                            first = False
                        if kb == i - 4:
                            fns.append(lambda e, first=first: e.matmul(pb[bk][:], lhsT=masks[:, 2, :], rhs=ident[:], start=first, stop=False))
                            first = False
                        fns.append(lambda e, first=first: e.matmul(pb[bk][:], lhsT=KTWs[:, kb * 128:(kb + 1) * 128], rhs=qc[:], start=first, stop=True))
                        return fns, ["masks", "ident", "KTWs", qcr]
                    attn_units(U, i - wk0 + 1, win_s, lambda j: V3s[2][:, wk0 + j, :], "V3s2")

                    def u_end():
                        attn_finish(pb[6], "pb6", None, None, (rrow, bcs, (oacc, False)), gate=(grow[pi][64:65, 2, :], gres), bcbank=4)
                        P.op("act", lambda e: e.activation(out=o_st[:, 2, :], in_=oacc[:], func=AF.Copy), reads=["oacc"], writes=[ostr])
                    U.append(u_end)
                    return U

                def store_o(i):
                    pi = i % 2
                    ts = slice(i * 128, (i + 1) * 128)
                    P.dma("sp", OT.ap()[:, :, ts].rearrange("(b h) d t -> d b h t", b=3), ost[pi][:].rearrange("d b (h t) -> d b h t", h=4),
                          reads=["ost%d" % pi], writes=[], key="ost%d" % pi)

                def run_merged(chains):
                    items = []
                    for ci, U in enumerate(chains):
                        P.capture = []
                        for f in U:
                            f()
                        ops_ = P.capture
                        P.capture = None
                        n = len(ops_)
                        for k, it_ in enumerate(ops_):
                            items.append(((k + 0.5) / n, ci, k, it_))
                    items.sort(key=lambda t: (t[0], t[1], t[2]))
                    for _, _, _, it_ in items:
                        P.commit(it_)

                load_block(0)
                run_merged([chain_P(0)])
                for i in range(NB):
                    if i + 1 < NB:
                        load_block(i + 1)
                    chains = [chain_SB(i), chain_ATT(i)]
                    if i + 1 < NB:
                        chains.append(chain_P(i + 1))
                    run_merged(chains)
                    store_o(i)
                P.barrier()
                P.emit_all()
            if debug == "B":
                break
            with ExitStack() as ec:
                sb = lambda n, s, d: ec.enter_context(nc.sbuf_tensor(n + "_L%d" % li, list(s), d))
                alloc_psum(ec, "C%d" % li, 7)
                wup = sb("wup", [64, 12, D], BF16); wout = sb("wout", [128, 8, D], BF16)
                wmq = sb("wmq", [128, 8, 256], BF16); wmo = sb("wmo", [64, 4, D], BF16); wmkv = sb("wmkv", [128, 8, 512], BF16)
                gq = sb("gq", [128, D], F32); gkv = sb("gkv", [128, D], F32)
                KmT = sb("KmT", [64, 4, NMEM], BF16); Vm = sb("Vm", [128, 2, 4, 65], BF16)
                oT = sb("oT", [64, 12, 512], BF16); gT = sb("gT", [128, 24, 512], BF16)
                mT = sb("mT", [128, 8, 512], BF16)
                tm = [sb("tm%d" % i, [128, 512], F32) for i in range(3)]
                hin = [sb("hin%d" % i, [128, D], F32) for i in range(2)]
                h1 = sb("h1", [128, 4, D], F32)
                junk = sb("junkC", [128, D], F32); ubf = sb("ubfC", [128, D], BF16)
                uT2 = sb("uT2", [128, 8, 512], BF16)
                qmT = sb("qmT", [64, 4, 512], BF16)
                Eb = [sb("EbC%d" % i, [128, 512], BF16) for i in range(2)]
                omT = sb("omT", [64, 4, 512], BF16)
                rrow = sb("rrowC", [128, 512], F32); bcs = sb("bcsC", [64, 512], F32)
                hout = [sb("houtC%d" % i, [128, D], F32) for i in range(2)]
                memb = sb("memb", [128, D], F32); mTm = sb("mTm", [128, 8, NMEM], BF16)
                P.dma("pool", wup[:], wup_d.ap()[l].rearrange("b (h d) n -> d (b h) n", d=64), writes=["wup"], key="c1w0")
                P.dma("pool", wout[:], wout_d.ap()[l].rearrange("(k p) n -> p k n", p=128), writes=["wout"], key="c1w1")
                P.dma("pool", wmq[:], wmq_d.ap()[l].rearrange("(k p) n -> p k n", p=128), writes=["wmq"], key="c1w2")
                P.dma("pool", wmo[:], wmo_d.ap()[l].rearrange("(h d) n -> d h n", d=64), writes=["wmo"], key="c1w3")
                P.dma("pool", wmkv[:], wmkv_d.ap()[l].rearrange("(k p) n -> p k n", p=128), writes=["wmkv"], key="c1w4")
                P.dma("sp", gq[:], bcrow(norms_d, 4 * l + 1), writes=["gq"], key="c1g0")
                P.dma("sp", gkv[:], bcrow(norms_d, 4 * l + 2), writes=["gkv"], key="c1g1")
                P.op("pool", lambda e: e.memset(Vm[:], 1.0), writes=["Vm"])
                for mc in range(2):
                    P.dma("sp", memb[:], mem_d.ap()[mc * 128:(mc + 1) * 128, :], writes=["memb"], key="c1mem")
                    rmsnorm_T(memb[:], "memb", gkv, "gkv", mTm[:, :, mc * 128:(mc + 1) * 128], "mTm", (junk, ubf))
                for h in range(4):
                    P.op("pe", [(lambda e, kc=kc, h=h: e.matmul(pb[0][0:64, 0:NMEM], lhsT=wmkv[:, kc, h * 64:(h + 1) * 64], rhs=mTm[:, kc, :],
                                                                start=(kc == 0), stop=(kc == 7))) for kc in range(8)], reads=["wmkv", "mTm"], writes=["pb0"])
                    P.op("act", lambda e, h=h: e.activation(out=KmT[:, h, :], in_=pb[0][0:64, 0:NMEM], func=AF.Copy), reads=["pb0"], writes=["KmT"])
                for mc in range(2):
                    P.op("pe", [(lambda e, kc=kc, mc=mc: e.matmul(pb[1][:, 0:256], lhsT=mTm[:, kc, mc * 128:(mc + 1) * 128], rhs=wmkv[:, kc, 256:512],
                                                                  start=(kc == 0), stop=(kc == 7))) for kc in range(8)], reads=["wmkv", "mTm"], writes=["pb1"])
                    P.op("act", lambda e, mc=mc: e.activation(out=Vm[:, mc, :, 0:64], in_=pb[1][:, 0:256].rearrange("p (h d) -> p h d", h=4), func=AF.Copy),
                         reads=["pb1"], writes=["Vm"])
                for g in range(NG):
                    gs = slice(g * 512, (g + 1) * 512)
                    P.dma("sp", oT[:], OT.ap()[:, :, gs].rearrange("c d s -> d c s"), reads=["scrB"], writes=["oT"], key="c1oT")
                    P.dma("sp", gT[:], GT.ap()[:, gs].rearrange("(c p) s -> p c s", p=128), reads=["scrA"], writes=["gT"], key="c1gT")
                    for fc in range(8):
                        for br in range(3):
                            P.op("pe", [(lambda e, h=h, br=br, fc=fc: e.matmul(pb[br][:], lhsT=wup[:, br * 4 + h, fc * 128:(fc + 1) * 128], rhs=oT[:, br * 4 + h, :],
                                                                                start=(h == 0), stop=(h == 3))) for h in range(4)], reads=["wup", "oT"], writes=["pb%d" % br])
                            P.op("dve", lambda e, br=br, fc=fc: e.tensor_tensor(out=tm[br][:], in0=pb[br][:], in1=gT[:, br * 8 + fc, :], op=ALU.mult),
                                 reads=["pb%d" % br, "gT"], writes=["tm%d" % br])
                        P.op("pool", lambda e: e.tensor_tensor(out=tm[0][:], in0=tm[0][:], in1=tm[1][:], op=ALU.add), reads=["tm0", "tm1"], writes=["tm0"])
                        P.op("pool", lambda e, fc=fc: e.tensor_tensor(out=mT[:, fc, :], in0=tm[0][:], in1=tm[2][:], op=ALU.add), reads=["tm0", "tm2"], writes=["mT"])
                    for b4 in range(4):
                        blk = g * 4 + b4
                        hi_ = hin[blk % 2]
                        P.dma("sp", hi_[:], h_in.ap()[blk * 128:(blk + 1) * 128, :], writes=["hin%d" % (blk % 2)], key="hin%d" % (blk % 2))
                        for half in range(2):
                            bk = 3 + half
                            P.op("pe", [(lambda e, kc=kc, half=half, bk=bk, b4=b4: e.matmul(pb[bk][:], lhsT=mT[:, kc, b4 * 128:(b4 + 1) * 128], rhs=wout[:, kc, half * 512:(half + 1) * 512],
                                                                                          start=(kc == 0), stop=(kc == 7))) for kc in range(8)], reads=["mT", "wout"], writes=["pb%d" % bk])
                            P.op("dve", lambda e, half=half, bk=bk, b4=b4, hi_=hi_: e.tensor_tensor(out=h1[:, b4, half * 512:(half + 1) * 512], in0=pb[bk][:], in1=hi_[:, half * 512:(half + 1) * 512], op=ALU.add),
                                 reads=["pb%d" % bk, "hin%d" % (blk % 2)], writes=["h1_%d" % b4])
                        rmsnorm_T(h1[:, b4, :], "h1_%d" % b4, gq, "gq", uT2[:, :, b4 * 128:(b4 + 1) * 128], "uT2", (junk, ubf))
                    for h in range(4):
                        bk = h % 2
                        P.op("pe", [(lambda e, kc=kc, h=h, bk=bk: e.matmul(pb[bk][0:64, :], lhsT=wmq[:, kc, h * 64:(h + 1) * 64], rhs=uT2[:, kc, :], start=(kc == 0), stop=(kc == 7)))
                                    for kc in range(8)], reads=["wmq", "uT2"], writes=["pb%d" % bk])
                        P.op("act", lambda e, h=h, bk=bk: e.activation(out=qmT[:, h, :], in_=pb[bk][0:64, :], func=AF.Copy), reads=["pb%d" % bk], writes=["qmT"])
                    for h in range(4):
                        for mc in range(2):
                            bk = mc
                            P.op("pe", lambda e, h=h, mc=mc, bk=bk: e.matmul(pb[bk][:], lhsT=KmT[:, h, mc * 128:(mc + 1) * 128], rhs=qmT[:, h, :], start=True, stop=True),
                                 reads=["KmT", "qmT"], writes=["pb%d" % bk])
                            P.op("act", lambda e, mc=mc, bk=bk: e.activation(out=Eb[mc][:], in_=pb[bk][:], func=AF.Exp, scale=0.125), reads=["pb%d" % bk], writes=["EbC%d" % mc])
                            P.op("pe", lambda e, h=h, mc=mc: e.matmul(pb[5][0:65, :], lhsT=Vm[:, mc, h, :], rhs=Eb[mc][:], start=(mc == 0), stop=(mc == 1)),
                                 reads=["Vm", "EbC%d" % mc], writes=["pb5"])
                        attn_finish(pb[5], "pb5", omT[:, h, :], "omT", (rrow, bcs, None))
                    for b4 in range(4):
                        blk = g * 4 + b4
                        ho = hout[blk % 2]
                        for half in range(2):
                            bk = 3 + half
                            P.op("pe", [(lambda e, h=h, half=half, bk=bk, b4=b4: e.matmul(pb[bk][:], lhsT=omT[:, h, b4 * 128:(b4 + 1) * 128], rhs=wmo[:, h, half * 512:(half + 1) * 512],
                                                                                        start=(h == 0), stop=(h == 3))) for h in range(4)], reads=["omT", "wmo"], writes=["pb%d" % bk])
                            P.op("dve", lambda e, half=half, bk=bk, b4=b4, ho=ho: e.tensor_tensor(out=ho[:, half * 512:(half + 1) * 512], in0=pb[bk][:], in1=h1[:, b4, half * 512:(half + 1) * 512], op=ALU.add),
                                 reads=["pb%d" % bk, "h1_%d" % b4], writes=["hout%d" % (blk % 2)])
                        P.dma("sp", HM.ap()[blk * 128:(blk + 1) * 128, :], ho[:], reads=["hout%d" % (blk % 2)], writes=[], key="hout%d" % (blk % 2))
                P.barrier()
                P.emit_all()
            if debug == "C1":
                break
            with ExitStack() as ef:
                sb = lambda n, s, d: ef.enter_context(nc.sbuf_tensor(n + "_L%d" % li, list(s), d))
                alloc_psum(ef, "F%d" % li, 7)
                wfi = sb("wfi", [128, 8, 2 * DFF], BF16); wfo = sb("wfo", [128, 22, D], BF16)
                gf = sb("gf", [128, D], F32); gfin = sb("gfin", [128, D], F32)
                hmb = sb("hmb", [128, 2, D], F32)
                junk = sb("junkF", [128, D], F32); ubf = sb("ubfF", [128, D], BF16)
                uT3 = sb("uT3", [128, 8, 256], BF16)
                sil = [sb("sil%d" % i, [128, 256], F32) for i in range(2)]
                actT = sb("actT", [128, 22, 256], BF16)
                hf = [sb("hf%d" % i, [128, D], F32) for i in range(2)]
                for kc in range(8):
                    P.dma("pool", wfi[:, kc, :], wfi_d.ap()[l, kc * 128:(kc + 1) * 128, :], writes=["wfi"], key="c2w%d" % kc)
                P.dma("pool", wfo[:, 0:11, :], wfo_d.ap()[l, 0:11 * 128, :].rearrange("(c p) n -> p c n", p=128), writes=["wfo"], key="c2wo0")
                P.dma("pool", wfo[:, 11:22, :], wfo_d.ap()[l, 11 * 128:22 * 128, :].rearrange("(c p) n -> p c n", p=128), writes=["wfo"], key="c2wo1")
                P.dma("sp", gf[:], bcrow(norms_d, 4 * l + 3), writes=["gf"], key="c2g0")
                P.dma("sp", gfin[:], bcrow(norms_d, 4 * L), writes=["gfin"], key="c2g1")
                for g2 in range(S // 256):
                    for b2 in range(2):
                        blk = g2 * 2 + b2
                        P.dma("sp", hmb[:, b2, :], HM.ap()[blk * 128:(blk + 1) * 128, :], reads=["scrC"], writes=["hmb%d" % b2], key="hmb%d" % b2)
                        rmsnorm_T(hmb[:, b2, :], "hmb%d" % b2, gf, "gf", uT3[:, :, b2 * 128:(b2 + 1) * 128], "uT3", (junk, ubf))
                    for c in range(22):
                        bk = c % 2
                        P.op("pe", [(lambda e, kc=kc, c=c, bk=bk: e.matmul(pb[bk][:, 0:256], lhsT=wfi[:, kc, c * 128:(c + 1) * 128], rhs=uT3[:, kc, :], start=(kc == 0), stop=(kc == 7)))
                                    for kc in range(8)] +
                                   [(lambda e, kc=kc, c=c, bk=bk: e.matmul(pb[bk][:, 256:512], lhsT=wfi[:, kc, DFF + c * 128:DFF + (c + 1) * 128], rhs=uT3[:, kc, :], start=False, stop=(kc == 7)))
                                    for kc in range(8)], reads=["wfi", "uT3"], writes=["pb%d" % bk])
                        P.op("act", lambda e, bk=bk: e.activation(out=sil[bk][:], in_=pb[bk][:, 0:256], func=AF.Silu), reads=["pb%d" % bk], writes=["sil%d" % bk])
                        P.op("dve", lambda e, bk=bk, c=c: e.tensor_tensor(out=actT[:, c, :], in0=pb[bk][:, 256:512], in1=sil[bk][:], op=ALU.mult),
                             reads=["pb%d" % bk, "sil%d" % bk], writes=["actT%d" % c])
                        for b2 in range(2):
                            for half in range(2):
                                ob = 2 + b2 * 2 + half
                                P.op("pe", lambda e, c=c, b2=b2, half=half, ob=ob: e.matmul(pb[ob][:], lhsT=actT[:, c, b2 * 128:(b2 + 1) * 128], rhs=wfo[:, c, half * 512:(half + 1) * 512],
                                                                                           start=(c == 0), stop=(c == 21)), reads=["actT%d" % c, "wfo"], writes=["pb%d" % ob])
                    for b2 in range(2):
                        blk = g2 * 2 + b2
                        hft = hf[b2]
                        for half in range(2):
                            ob = 2 + b2 * 2 + half
                            P.op("dve", lambda e, ob=ob, half=half, b2=b2, hft=hft: e.tensor_tensor(out=hft[:, half * 512:(half + 1) * 512], in0=pb[ob][:], in1=hmb[:, b2, half * 512:(half + 1) * 512], op=ALU.add),
                                 reads=["pb%d" % ob, "hmb%d" % b2], writes=["hf%d" % b2])
                        if not last:
                            P.dma("sp", HO.ap()[blk * 128:(blk + 1) * 128, :], hft[:], reads=["hf%d" % b2], writes=[], key="hf%d" % b2)
                        else:
                            P.op("act", lambda e, hft=hft: e.activation(out=junk[:], in_=hft[:], func=AF.Square, accum_out=stat[:, 4:5]), reads=["hf%d" % b2], writes=["junk", "stat4"])
                            P.op("act", lambda e: e.activation(out=stat[:, 5:6], in_=stat[:, 4:5], func=AF.Sqrt, scale=1.0 / D, bias=1e-6), reads=["stat4"], writes=["stat5"])
                            P.op("dve", lambda e: e.reciprocal(out=stat[:, 6:7], in_=stat[:, 5:6]), reads=["stat5"], writes=["stat6"])
                            P.op("dve", lambda e, hft=hft: e.scalar_tensor_tensor(out=hft[:], in0=hft[:], scalar=stat[:, 6:7], in1=gfin[:], op0=ALU.mult, op1=ALU.mult),
                                 reads=["hf%d" % b2, "stat6", "gfin"], writes=["hf%d" % b2])
                            P.dma("sp", out_d.ap()[blk * 128:(blk + 1) * 128, :], hft[:], reads=["hf%d" % b2], writes=[], key="hf%d" % b2)
                P.barrier()
                P.emit_all()
    return nc


NBIS = 16

_CACHE = {}


def prep_shared(inputs, S, layers):
    cols = _col_index()
    sh = {}
    w_in = np.asarray(inputs['w_in'], np.float32)
    sh['wA'] = np.ascontiguousarray(np.stack([w_in[l][:, cols] for l in layers], 0))
    for k in ('cmp_w1_k', 'cmp_w2_k', 'cmp_pos_k', 'cmp_w1_v', 'cmp_w2_v', 'cmp_pos_v', 'w_up', 'w_out',
              'w_mem_q', 'w_mem_kv', 'w_mem_o', 'w_ffn_in', 'w_ffn_out'):
        a = np.asarray(inputs[k], np.float32)
        sh[k] = np.ascontiguousarray(np.stack([a[l] for l in layers], 0))
    rows = []
    for l in layers:
        rows += [np.asarray(inputs[k], np.float32)[l] for k in ('norm_mix', 'norm_mem_q', 'norm_mem_kv', 'norm_ffn')]
    rows.append(np.asarray(inputs['norm_final'], np.float32))
    sh['norms'] = np.ascontiguousarray(np.stack(rows, 0))
    sh.update(make_consts(S))
    return sh


def kernel(**inputs):
    x = np.asarray(inputs['x'], np.float32)
    mem = np.asarray(inputs['mem'], np.float32)
    B, S, _ = x.shape
    layers = [0, 1]
    key = (S, tuple(layers))
    if key not in _CACHE:
        _CACHE[key] = build(S, layers)
    nc = _CACHE[key]
    sh = prep_shared(inputs, S, layers)
    in_maps = []
    for b in range(B):
        m = dict(sh)
        m['x'] = np.ascontiguousarray(x[b])
        m['mem'] = np.ascontiguousarray(mem[b])
        in_maps.append(m)
    res = run_bass_kernel_spmd(nc, in_maps, core_ids=list(range(B)))
    return np.stack([np.asarray(r['out'], np.float32) for r in res.results], 0)
```

```python
import numpy as np
import concourse.bass as bass
import concourse.mybir as mybir
from concourse.bass_utils import run_bass_kernel_spmd
from contextlib import ExitStack
import os

F32 = mybir.dt.float32
BF16 = mybir.dt.bfloat16
AF = mybir.ActivationFunctionType
ALU = mybir.AluOpType
AX = mybir.AxisListType

EPOCH = 30000
D = 1024
DFF = 2816
NEG = -30000.0
BIGF = 1.0e30
NMEM = 256
TOPK = 256
NA_FM = 9 * 64 + 17 * 2 * 64 + 12 + 3072
NA = NA_FM + 452


class _Rec:
    def __init__(self):
        self.calls = []

    def __getattr__(self, name):
        def f(*a, **kw):
            self.calls.append((name, a, kw))
            return self
        return f


class Prog:
    ENGS = ("pe", "act", "dve", "pool", "sp")

    def __init__(self, nc, es):
        self.nc = nc
        self.es = es
        self.ops = {e: [] for e in self.ENGS}
        self.cnt = {e: 0 for e in self.ENGS}
        self.seen = {e: {} for e in self.ENGS}
        self.sems = {}
        self.latest = {}
        self.res_w = {}
        self.res_r = {}
        self.dcnt = {}
        self.capture = None

    def sem(self, key):
        if key not in self.sems:
            self.sems[key] = self.es.enter_context(
                self.nc.semaphore("s_" + key.replace("#", "_").replace(":", "_")))
        return self.sems[key]

    def _deps(self, eng, reads, writes):
        deps = {}
        for r in reads:
            ev = self.res_w.get(r)
            if ev is not None:
                deps[ev[0]] = max(deps.get(ev[0], 0), ev[1])
        for w in writes:
            ev = self.res_w.get(w)
            if ev is not None:
                deps[ev[0]] = max(deps.get(ev[0], 0), ev[1])
            for ev in self.res_r.get(w, ()):
                deps[ev[0]] = max(deps.get(ev[0], 0), ev[1])
        out = []
        for k, v in deps.items():
            if eng == "pe" and k.startswith("pe#"):
                continue
            if self.seen[eng].get(k, 0) >= v:
                continue
            self.seen[eng][k] = v
            out.append((k, v))
        return out

    def _commit(self, ev, reads, writes):
        self.latest[ev[0]] = max(self.latest.get(ev[0], 0), ev[1])
        for w in writes:
            self.res_w[w] = ev
            self.res_r[w] = []
        for r in reads:
            if r in writes:
                continue
            lst = self.res_r.setdefault(r, [])
            lst.append(ev)
            if len(lst) > 16:
                d = {}
                for k, v in lst:
                    d[k] = max(d.get(k, 0), v)
                self.res_r[r] = list(d.items())

    def op(self, eng, fns, reads=(), writes=()):
        if callable(fns):
            fns = [fns]
        rec = _Rec()
        for f in fns:
            f(rec)
        calls = rec.calls
        if self.capture is not None:
            self.capture.append(("op", eng, calls, list(reads), list(writes)))
            return None
        return self._op_commit(eng, calls, reads, writes)

    def _op_commit(self, eng, calls, reads, writes):
        px = [r for r in reads if r.startswith("pb") or r == "ptr"]
        if px:
            reads = [r for r in reads if r not in px]
            writes = list(writes) + [r for r in px if r not in writes]
        waits = self._deps(eng, reads, writes)
        c = self.cnt[eng] + 1
        self.cnt[eng] = c
        key = "%s#%d" % (eng, (c - 1) // EPOCH)
        val = (c - 1) % EPOCH + 1
        semh = self.sem(key)
        wl = [(self.sem(k), v) for k, v in waits]

        def emit(e, calls=calls, wl=wl, semh=semh):
            for s, v in wl:
                e.wait_ge(s, v)
            ins = None
            for name, a, kw in calls:
                if name == "matmul":
                    kw = dict(kw)
                    kw["skip_group_check"] = True
                ins = getattr(e, name)(*a, **kw)
            ins.then_inc(semh, 1)

        self.ops[eng].append(emit)
        ev = (key, val)
        self._commit(ev, reads, writes)
        return ev

    def dma(self, q, out, in_, reads=(), writes=(), key=None):
        if self.capture is not None:
            self.capture.append(("dma", q, out, in_, list(reads), list(writes), key))
            return None
        return self._dma_commit(q, out, in_, reads, writes, key)

    def commit(self, item):
        if item[0] == "op":
            return self._op_commit(*item[1:])
        return self._dma_commit(*item[1:])

    def _dma_commit(self, q, out, in_, reads=(), writes=(), key=None):
        waits = self._deps(q, reads, writes)
        dk = "dma:" + key
        semh = self.sem(dk)
        self.dcnt[dk] = self.dcnt.get(dk, 0) + 16
        wl = [(self.sem(k), v) for k, v in waits]

        def emit(e, wl=wl, semh=semh, out=out, in_=in_):
            for s, v in wl:
                e.wait_ge(s, v)
            e.dma_start(out=out, in_=in_).then_inc(semh, 16)

        self.ops[q].append(emit)
        ev = (dk, self.dcnt[dk])
        self._commit(ev, reads, writes)
        return ev

    def barrier(self):
        for eng in self.ENGS:
            wl = []
            for k, v in self.latest.items():
                if self.seen[eng].get(k, 0) >= v:
                    continue
                self.seen[eng][k] = v
                wl.append((self.sem(k), v))

            def emit(e, wl=wl):
                for s, v in wl:
                    e.wait_ge(s, v)

            self.ops[eng].append(emit)

    def emit_all(self):
        nc = self.nc
        ops = self.ops
        with nc.Block() as block:
            @block.tensor
            def _(e):
                for f in ops["pe"]:
                    f(e)

            @block.scalar
            def _(e):
                for f in ops["act"]:
                    f(e)

            @block.vector
            def _(e):
                for f in ops["dve"]:
                    f(e)

            @block.gpsimd
            def _(e):
                for f in ops["pool"]:
                    f(e)

            @block.sync
            def _(e):
                for f in ops["sp"]:
                    f(e)
        self.ops = {e: [] for e in self.ENGS}


OFF = {}
_o = 0
for _n, _w in (('q_a', 256), ('k_a', 256), ('v_a', 256), ('q_b', 256), ('k_b', 64), ('v_b', 64),
               ('q_idx', 256), ('k_idx', 64), ('w_idx', 4), ('q_c', 256), ('k_cmp', 64), ('v_cmp', 64),
               ('k_sel', 64), ('v_sel', 64), ('k_win', 64), ('v_win', 64), ('g_c', 12), ('g_merge', 3072)):
    OFF[_n] = _o
    _o += _w

FM_PLAIN = [('q_a', h) for h in range(4)] + [('k_a', h) for h in range(4)] + [('v_cmp', 0)]
FM_ROPE = ([('q_b', h) for h in range(4)] + [('k_b', 0)] + [('q_idx', h) for h in range(4)] + [('k_idx', 0)] +
           [('q_c', h) for h in range(4)] + [('k_cmp', 0), ('k_sel', 0), ('k_win', 0)])


def _col_index():
    cols = []
    for n, h in FM_PLAIN:
        c0 = OFF[n] + 64 * h
        cols += list(range(c0, c0 + 64))
    for n, h in FM_ROPE:
        c0 = OFF[n] + 64 * h
        cols += list(range(c0, c0 + 64))
        cols += [c0 + (d + 32) % 64 for d in range(64)]
    cols += [OFF['g_c'] + h * 3 + br for br in range(3) for h in range(4)]
    cols += list(range(OFF['g_merge'], OFF['g_merge'] + 3072))
    cols += list(range(OFF['v_a'], OFF['v_a'] + 256))
    cols += list(range(OFF['v_b'], OFF['v_b'] + 64))
    cols += list(range(OFF['v_sel'], OFF['v_sel'] + 64))
    cols += list(range(OFF['v_win'], OFF['v_win'] + 64))
    cols += list(range(OFF['w_idx'], OFF['w_idx'] + 4))
    assert len(cols) == NA
    return np.asarray(cols)


def make_consts(S):
    NB = S // 128
    c = {}
    c['c_ident4'] = np.tile(np.eye(128, dtype=np.float32), (1, 4))
    t = np.arange(128)[:, None]
    s = np.arange(128)[None, :]
    m = np.zeros((128, 5, 128), np.float32)
    m[:, 0, :] = np.where(s >= t, NEG, 0.0)
    m[:, 1, :] = np.where(s > t, NEG, 0.0)
    m[:, 2, :] = np.where(s <= t, NEG, 0.0)
    j = np.arange(128)[:, None]
    m[:, 3, :] = np.where(j >= s, -1.0, 0.0)
    m[:, 4, :] = -1.0
    c['c_masks'] = m
    c['c_tokcausal'] = np.where(s > t, -BIGF, 0.0).astype(np.float32)
    inv = 10000.0 ** (-np.arange(0, 64, 2, dtype=np.float32) / 64)
    ang = np.arange(S, dtype=np.float32)[None, :] * inv[:, None]
    cos = np.cos(ang).astype(np.float32)
    sin = np.sin(ang).astype(np.float32)
    c['c_rope'] = np.stack([np.concatenate([cos, cos], 0), np.concatenate([-sin, sin], 0)], 0)
    n_sel = S // 64
    ex = np.zeros((64, S), np.float32)
    for jj in range(min(n_sel, 64)):
        ex[jj, jj * 64:(jj + 1) * 64] = 1.0
    c['c_expand'] = ex
    n_cmp = (S - 32) // 16 + 1
    ncp = ((n_cmp + 1 + 127) // 128) * 128
    cs = np.arange(n_cmp) * 16
    ss = np.arange(n_sel) * 64
    ov = np.zeros((ncp, 64), np.float32)
    ov[:n_cmp, :n_sel] = ((cs[:, None] < ss[None, :] + 64) & (cs[:, None] + 32 > ss[None, :])).astype(np.float32)
    c['c_ov'] = ov
    tg = np.arange(S)
    n = np.arange(ncp)
    adm = (n[None, :] * 16 + 31 <= tg[:, None]) & (n[None, :] < n_cmp)
    c['c_cmpmask'] = np.where(adm, 0.0, NEG).astype(np.float32).reshape(NB, 128, ncp)
    jj = np.arange(64)
    cur = tg // 64
    forced0 = (jj[None, :] == 0)
    forced1 = (jj[None, :] == cur[:, None])
    forced2 = (jj[None, :] == cur[:, None] - 1)
    forced = forced0 | forced1 | forced2
    admis = (jj[None, :] * 64 <= tg[:, None]) & (jj[None, :] < n_sel)
    mul = np.where(forced | ~admis, 0.0, 1.0).astype(np.float32)
    add = np.zeros((S, 64), np.float32)
    add = np.where(forced0, 10000.0, add)
    add = np.where(forced2, 10001.0, add)
    add = np.where(forced1, 10002.0, add)
    add = np.where(admis, add, -BIGF).astype(np.float32)
    c['c_selmul'] = mul.reshape(NB, 128, 64)
    c['c_seladd'] = add.reshape(NB, 128, 64)
    return c


def build(S, layers, debug=False):
    NB = S // 128
    NG = S // 512
    L = len(layers)
    n_sel = S // 64
    topn = min(16, n_sel)
    topk = min(TOPK, S // 4)
    n_cmp = (S - 32) // 16 + 1
    NCP = ((n_cmp + 1 + 127) // 128) * 128
    NCC = NCP // 128
    nc = bass.Bass("TRN2", target_bir_lowering=False)

    def din(name, shape, dt=F32):
        return nc.dram_tensor(name, list(shape), dt, kind="ExternalInput")

    dbg_kind = "ExternalOutput" if debug else None

    def dscr(name, shape, dt):
        if debug:
            return nc.dram_tensor(name, list(shape), dt, kind="ExternalOutput")
        return nc.dram_tensor(name, list(shape), dt)

    x_d = din("x", [S, D])
    mem_d = din("mem", [NMEM, D])
    wA_d = din("wA", [L, D, NA])
    w1k_d = din("cmp_w1_k", [L, 2048, 128]); w2k_d = din("cmp_w2_k", [L, 128, 64]); posk_d = din("cmp_pos_k", [L, 32, 64])
    w1v_d = din("cmp_w1_v", [L, 2048, 128]); w2v_d = din("cmp_w2_v", [L, 128, 64]); posv_d = din("cmp_pos_v", [L, 32, 64])
    wup_d = din("w_up", [L, 3, 256, D]); wout_d = din("w_out", [L, D, D])
    wmq_d = din("w_mem_q", [L, D, 256]); wmkv_d = din("w_mem_kv", [L, D, 512]); wmo_d = din("w_mem_o", [L, 256, D])
    wfi_d = din("w_ffn_in", [L, D, 2 * DFF]); wfo_d = din("w_ffn_out", [L, DFF, D])
    norms_d = din("norms", [4 * L + 1, D])
    c_ident4 = din("c_ident4", [128, 512]); c_masks = din("c_masks", [128, 5, 128]); c_tokc = din("c_tokcausal", [128, 128])
    c_rope = din("c_rope", [2, 64, S]); c_expand = din("c_expand", [64, S]); c_ov = din("c_ov", [NCP, 64])
    c_cmpmask = din("c_cmpmask", [NB, 128, NCP]); c_selmul = din("c_selmul", [NB, 128, 64]); c_seladd = din("c_seladd", [NB, 128, 64])
    out_d = nc.dram_tensor("out", [S, D], F32, kind="ExternalOutput")

    QTA = dscr("QTA", [4, 64, S], BF16); KTA = dscr("KTA", [4, 64, S], BF16)
    QTB = dscr("QTB", [4, 64, S], BF16); KTB = dscr("KTB", [64, S], BF16)
    QTI = dscr("QTI", [4, 64, S], BF16); KTI = dscr("KTI", [64, S], BF16)
    QTC = dscr("QTC", [4, 64, S], BF16)
    KTCMP = dscr("KTCMP", [64, S], BF16); VTCMP = dscr("VTCMP", [64, S], BF16)
    KTSEL = dscr("KTSEL", [64, S], BF16); KTWIN = dscr("KTWIN", [64, S], BF16)
    VA = dscr("VA", [S, 256], BF16); V3 = dscr("V3", [S, 3, 64], BF16)
    WI = dscr("WI", [S, 4], F32); GC = dscr("GC", [12, S], F32); GT = dscr("GT", [3072, S], BF16)
    OT = dscr("OT", [12, 64, S], BF16)
    HM = dscr("HM", [S, D], F32); HO = dscr("HO", [S, D], F32)
    fm_dst = {}
    for n, h in FM_PLAIN + FM_ROPE:
        fm_dst[(n, h)] = {'q_a': QTA, 'k_a': KTA, 'q_b': QTB, 'q_idx': QTI, 'q_c': QTC}.get(n)
    single = {'v_cmp': VTCMP, 'k_b': KTB, 'k_idx': KTI, 'k_cmp': KTCMP, 'k_sel': KTSEL, 'k_win': KTWIN}

    def bcrow(handle, row, n=D, parts=128):
        return bass.AP(handle, row * n, [[0, parts], [1, n]])

    with ExitStack() as es:
        P = Prog(nc, es)
        sbg = lambda n, s, d: es.enter_context(nc.sbuf_tensor(n, list(s), d))
        psg = lambda n, s, d: es.enter_context(nc.psum_tensor(n, list(s), d))
        pb = [None] * 8
        ptrh = [None]

        def alloc_psum(stk, tag, nfp):
            for i_ in range(nfp):
                pb[i_] = stk.enter_context(nc.psum_tensor("pb%d_%s" % (i_, tag), [128, 512], F32))
            if nfp < 8:
                ptrh[0] = stk.enter_context(nc.psum_tensor("ptr_%s" % tag, [128, 1024], BF16))
        ident = sbg("ident", [128, 512], BF16)
        masks = sbg("masks", [128, 5, 128], BF16)
        ones32 = sbg("ones32", [128, 64], F32)
        stat = sbg("stat", [128, 16], F32)
        P.dma("pool", ident[:], c_ident4.ap(), writes=["ident"], key="c0")
        P.dma("pool", masks[:], c_masks.ap(), writes=["masks"], key="c1")
        P.op("pool", lambda e: e.memset(ones32[:], 1.0), writes=["ones32"])

        def rmsnorm_T(src, src_res, g_bc, g_res, dstT, dst_res, tmp):
            junk, ubf = tmp
            P.op("act", lambda e: e.activation(out=junk[:], in_=src, func=AF.Square, accum_out=stat[:, 0:1]),
                 reads=[src_res], writes=["junk", "stat0"])
            P.op("act", lambda e: e.activation(out=stat[:, 1:2], in_=stat[:, 0:1], func=AF.Sqrt, scale=1.0 / D, bias=1e-6),
                 reads=["stat0"], writes=["stat1"])
            P.op("dve", lambda e: e.reciprocal(out=stat[:, 2:3], in_=stat[:, 1:2]), reads=["stat1"], writes=["stat2"])
            P.op("dve", lambda e: e.scalar_tensor_tensor(out=ubf[:], in0=src, scalar=stat[:, 2:3], in1=g_bc[:],
                                                          op0=ALU.mult, op1=ALU.mult),
                 reads=[src_res, "stat2", g_res], writes=["ubf"])
            P.op("pe", [(lambda e, kc=kc: e.transpose(out=ptrh[0][:, kc * 128:(kc + 1) * 128], in_=ubf[:, kc * 128:(kc + 1) * 128],
                                                      identity=ident[:, 0:128])) for kc in range(8)],
                 reads=["ubf", "ident"], writes=["ptr"])
            P.op("act", lambda e: e.activation(out=dstT, in_=ptrh[0][:].rearrange("p (k t) -> p k t", k=8), func=AF.Copy),
                 reads=["ptr"], writes=[dst_res])

        def attn_finish(obank, obres, dst, dst_res, work, gate=None, guard=False, extra_reads=(), bcbank=6):
            rrow, bcs, accum = work
            P.op("act", lambda e: e.activation(out=rrow[64:65, :], in_=obank[64:65, :], func=AF.Ln), reads=[obres], writes=["rrow"])
            P.op("act", lambda e: e.activation(out=rrow[64:65, :], in_=rrow[64:65, :], func=AF.Exp, scale=-1.0), reads=["rrow"], writes=["rrow"])
            if gate is not None:
                gap, gres = gate
                P.op("dve", lambda e: e.tensor_tensor(out=rrow[64:65, :], in0=rrow[64:65, :], in1=gap, op=ALU.mult),
                     reads=["rrow", gres], writes=["rrow"])
            P.op("pe", lambda e: e.matmul(pb[bcbank][0:64, :], lhsT=ones32[64:65, 0:64], rhs=rrow[64:65, :], start=True, stop=True),
                 reads=["rrow", "ones32"], writes=["pb%d" % bcbank])
            P.op("act", lambda e: e.activation(out=bcs[0:64, :], in_=pb[bcbank][0:64, :], func=AF.Copy), reads=["pb%d" % bcbank], writes=["bcs"])
            if accum is None:
                P.op("dve", lambda e: e.tensor_tensor(out=dst, in0=obank[0:64, :], in1=bcs[0:64, :], op=ALU.mult),
                     reads=[obres, "bcs"] + list(extra_reads), writes=[dst_res])
            else:
                acc, first = accum
                if first:
                    P.op("dve", lambda e: e.tensor_tensor(out=acc[0:64, :], in0=obank[0:64, :], in1=bcs[0:64, :], op=ALU.mult),
                         reads=[obres, "bcs"], writes=["oacc"])
                else:
                    P.op("dve", lambda e: e.tensor_tensor(out=bcs[0:64, :], in0=obank[0:64, :], in1=bcs[0:64, :], op=ALU.mult),
                         reads=[obres, "bcs"], writes=["bcs"])
                    P.op("pool", lambda e: e.tensor_tensor(out=acc[0:64, :], in0=acc[0:64, :], in1=bcs[0:64, :], op=ALU.add),
                         reads=["bcs", "oacc"], writes=["oacc"])

        for li, l in enumerate(layers):
            h_in = x_d if li == 0 else HO
            last = (li == L - 1)
            with ExitStack() as ea:
                sb = lambda n, s, d: ea.enter_context(nc.sbuf_tensor(n + "_L%d" % li, list(s), d))
                alloc_psum(ea, "A%d" % li, 7)
                wA = sb("wA_sb", [128, 8, NA], BF16)
                gA = sb("gA", [128, D], F32)
                rope = sb("rope", [64, 2, S], F32)
                hb = [sb("hbA%d" % i, [128, D], F32) for i in range(2)]
                junk = sb("junkA", [128, D], F32)
                ubf = sb("ubfA", [128, D], BF16)
                uT = sb("uTA", [128, 8, 512], BF16)
                stg = [sb("stgA%d" % i, [128, 512], BF16) for i in range(4)]
                stgf = sb("stgfA", [128, 512], F32)
                t1 = sb("ropeT1", [64, 512], F32)
                t2 = sb("ropeT2", [64, 512], F32)
                for kc in range(8):
                    P.dma("pool", wA[:, kc, :], wA_d.ap()[l, kc * 128:(kc + 1) * 128, :], writes=["wA%d" % kc], key="wA%d" % kc)
                wA_res = ["wA%d" % kc for kc in range(8)]
                P.dma("sp", gA[:], bcrow(norms_d, 4 * l + 0), writes=["gA"], key="gA")
                P.dma("sp", rope[:], c_rope.ap().rearrange("c d s -> d c s"), writes=["rope"], key="rope")
                sctr = [0]

                def stage():
                    i = sctr[0] % 4
                    sctr[0] += 1
                    return stg[i], "stgA%d" % i

                def proj_fm(bank, c0, M):
                    P.op("pe", [(lambda e, kc=kc: e.matmul(pb[bank][0:M, :], lhsT=wA[:, kc, c0:c0 + M], rhs=uT[:, kc, :],
                                                            start=(kc == 0), stop=(kc == 7))) for kc in range(8)],
                         reads=["uT"] + wA_res, writes=["pb%d" % bank])

                LIM = int(os.environ.get("LIMA", "99"))
                for g in range(NG if LIM > 0 else 0):
                    gs = slice(g * 512, (g + 1) * 512)
                    for b4 in range(4):
                        blk = g * 4 + b4
                        hbt = hb[blk % 2]
                        P.dma("sp", hbt[:], h_in.ap()[blk * 128:(blk + 1) * 128, :], writes=["hb%d" % (blk % 2)], key="hb%d" % (blk % 2))
                        rmsnorm_T(hbt[:], "hb%d" % (blk % 2), gA, "gA", uT[:, :, b4 * 128:(b4 + 1) * 128], "uT", (junk, ubf))
                    col = 0
                    bank = 0
                    if LIM <= 1:
                        continue
                    for (n, h) in FM_PLAIN:
                        proj_fm(bank, col, 64)
                        st, sr = stage()
                        sc = 0.125 if n == 'q_a' else 1.0
                        P.op("act", lambda e, bank=bank, st=st, sc=sc: e.activation(out=st[0:64, :], in_=pb[bank][0:64, :], func=AF.Copy, scale=sc),
                             reads=["pb%d" % bank], writes=[sr])
                        dst = fm_dst[(n, h)].ap()[h, :, gs] if fm_dst[(n, h)] is not None else single[n].ap()[:, gs]
                        P.dma("sp", dst, st[0:64, :], reads=[sr], writes=[], key=sr)
                        col += 64
                        bank = (bank + 1) % 6
                    if LIM <= 2:
                        continue
                    for (n, h) in FM_ROPE:
                        b0 = bank
                        b1 = (bank + 1) % 6
                        proj_fm(b0, col, 64)
                        proj_fm(b1, col + 64, 64)
                        st, sr = stage()
                        P.op("dve", lambda e, b0=b0: e.tensor_tensor(out=t1[:], in0=pb[b0][0:64, :], in1=rope[:, 0, gs], op=ALU.mult),
                             reads=["pb%d" % b0, "rope"], writes=["t1"])
                        P.op("dve", lambda e, b1=b1: e.tensor_tensor(out=t2[:], in0=pb[b1][0:64, :], in1=rope[:, 1, gs], op=ALU.mult),
                             reads=["pb%d" % b1, "rope"], writes=["t2"])
                        P.op("pool", lambda e, st=st: e.tensor_tensor(out=st[0:64, :], in0=t1[:], in1=t2[:], op=ALU.add),
                             reads=["t1", "t2"], writes=[sr])
                        dst = fm_dst[(n, h)].ap()[h, :, gs] if fm_dst[(n, h)] is not None else single[n].ap()[:, gs]
                        P.dma("sp", dst, st[0:64, :], reads=[sr], writes=[], key=sr)
                        col += 128
                        bank = (bank + 2) % 6
                    if LIM <= 3:
                        continue
                    proj_fm(bank, col, 12)
                    P.op("act", lambda e, bank=bank: e.activation(out=stgf[0:12, :], in_=pb[bank][0:12, :], func=AF.Sigmoid),
                         reads=["pb%d" % bank], writes=["stgf"])
                    P.dma("sp", GC.ap()[:, gs], stgf[0:12, :], reads=["stgf"], writes=[], key="stgf")
                    col += 12
                    bank = (bank + 1) % 6
                    for gg in range(24):
                        proj_fm(bank, col, 128)
                        st, sr = stage()
                        P.op("act", lambda e, bank=bank, st=st: e.activation(out=st[:], in_=pb[bank][:], func=AF.Sigmoid),
                             reads=["pb%d" % bank], writes=[sr])
                        P.dma("sp", GT.ap()[gg * 128:(gg + 1) * 128, gs], st[:], reads=[sr], writes=[], key=sr)
                        col += 128
                        bank = (bank + 1) % 6
                    assert col == NA_FM
                    if LIM <= 4:
                        continue
                    for b4 in range(4):
                        blk = g * 4 + b4
                        P.op("pe", [(lambda e, kc=kc, b4=b4, bank=bank: e.matmul(pb[bank][:, 0:452], lhsT=uT[:, kc, b4 * 128:(b4 + 1) * 128],
                                                                                  rhs=wA[:, kc, NA_FM:NA], start=(kc == 0), stop=(kc == 7)))
                                    for kc in range(8)], reads=["uT"] + wA_res, writes=["pb%d" % bank])
                        st, sr = stage()
                        P.op("act", lambda e, bank=bank, st=st: e.activation(out=st[:, 0:448], in_=pb[bank][:, 0:448], func=AF.Copy),
                             reads=["pb%d" % bank], writes=[sr])
                        P.op("dve", lambda e, bank=bank: e.tensor_scalar(out=stgf[:, 0:4], in0=pb[bank][:, 448:452], scalar1=1.0 / 16, scalar2=None, op0=ALU.mult),
                             reads=["pb%d" % bank], writes=["stgf"])
                        P.dma("sp", VA.ap()[blk * 128:(blk + 1) * 128, :], st[:, 0:256], reads=[sr], writes=[], key=sr)
                        P.dma("sp", V3.ap()[blk * 128:(blk + 1) * 128, :, :], st[:, 256:448].rearrange("p (a d) -> p a d", a=3), reads=[sr], writes=[], key=sr)
                        P.dma("sp", WI.ap()[blk * 128:(blk + 1) * 128, :], stgf[:, 0:4], reads=["stgf"], writes=[], key="stgf")
                        bank = (bank + 1) % 6
                P.barrier()
                P.emit_all()
            if debug == "A":
                break
            with ExitStack() as eb:
                sb = lambda n, s, d: eb.enter_context(nc.sbuf_tensor(n + "_L%d" % li, list(s), d))
                alloc_psum(eb, "B%d" % li, 8)
                KTAs = sb("KTAs", [64, 4, S], BF16); VAs = sb("VAs", [128, NB, 256], BF16)
                KTBs = sb("KTBs", [64, S], BF16); KTIs = sb("KTIs", [64, S], BF16)
                KTSs = sb("KTSs", [128, S], BF16); KTWs = sb("KTWs", [64, S], BF16)
                V3s = [sb("V3s%d" % j, [128, NB, 65], BF16) for j in range(3)]
                KCT = sb("KCT", [64, NCP], BF16); VCs = sb("VCs", [128, NCC, 65], BF16); OVs = sb("OVs", [128, NCC, 64], BF16)
                tokc = sb("tokc", [128, 128], F32)
                score = sb("score", [128, S], F32); junkb = sb("junkb", [128, S], BF16); mneg = [sb("mneg%d" % i_, [128, S], BF16) for i_ in range(2)]
                QA = [sb("QA%d" % i, [64, 512], BF16) for i in range(2)]
                QB = [sb("QB%d" % i, [64, 512], BF16) for i in range(2)]
                QI = [sb("QI%d" % i, [64, 512], BF16) for i in range(2)]
                QC = [sb("QC%d" % i, [128, 512], BF16) for i in range(2)]
                wi = [sb("wi%d" % i, [128, 4], F32) for i in range(2)]
                grow = [sb("grow%d" % i_, [128, 3, 512], F32) for i_ in range(2)]
                cmk = [sb("cmk%d" % i, [128, NCP], BF16) for i in range(2)]
                smul = [sb("smul%d" % i, [128, 64], F32) for i in range(2)]
                sadd = [sb("sadd%d" % i, [128, 64], F32) for i in range(2)]
                e1 = [sb("e1_%d" % i, [128, 512], F32) for i in range(2)]
                sp_ = [sb("sp_%d" % i, [128, 512], BF16) for i in range(2)]
                asb = [sb("asb%d" % i, [128, 512], F32) for i in range(2)]
                Wt = [sb("Wt%d" % i, [128, 512], BF16) for i in range(2)]
                chi = sb("chi", [2, 512], BF16); crow2 = sb("crow2", [2, 512], BF16); negm = sb("negm", [2, 1], F32); onesb = sb("onesb", [2, 128], BF16)
                P.op("pool", lambda e: e.memset(negm[0:2, :], -1.0), writes=["negm"])
                P.op("pool", lambda e: e.memset(negm[0:1, :], 0.0), writes=["negm"])
                P.op("pool", lambda e: e.memset(onesb[:], 1.0), writes=["onesb"])
                Eb = [sb("Eb%d" % i, [128, 512], BF16) for i in range(3)]
                tmpi = [sb("tmpi%d" % i_, [128, 512], F32) for i_ in range(2)]
                rrow = sb("rrow", [128, 512], F32); bcs = sb("bcs", [64, 512], F32); oacc = sb("oacc", [64, 512], F32)
                ost = [sb("ost%d" % i, [64, 3, 512], BF16) for i in range(2)]
                bis = sb("bis", [128, 8], F32)
                impT = sb("impT", [64, 128], F32); uimp = sb("uimp", [64, 512], F32)
                imp = sb("imp", [128, 64], F32); imp2 = sb("imp2", [128, 64], F32); m8 = sb("m8", [128, 16], F32)
                msel = sb("msel", [128, 128], F32)
                identf = sb("identf", [128, 128], F32)
                w2 = sb("w2", [128, 64], BF16); posT = sb("posT", [64, 32], BF16)
                w1 = (mneg[0][0:64, 0:4096] if S >= 4096 else sb("w1x", [64, 4096], BF16)[:, :]).rearrange("d (a h) -> d a h", a=32)
                TCs = junkb[0:64, :]
                hid = sb("hid", [128, NCP], BF16); bcol = sb("bcol", [128, 1], F32)

                P.dma("sp", KTAs[:], KTA.ap().rearrange("h d s -> d h s"), reads=["scrA"], writes=["KTAs"], key="ldB0")
                P.dma("sp", VAs[:], VA.ap().rearrange("(b p) c -> p b c", p=128), reads=["scrA"], writes=["VAs"], key="ldB1")
                P.dma("sp", KTBs[:], KTB.ap(), reads=["scrA"], writes=["KTBs"], key="ldB2")
                P.dma("sp", KTIs[:], KTI.ap(), reads=["scrA"], writes=["KTIs"], key="ldB3")
                P.dma("sp", KTSs[0:64, :], KTSEL.ap(), reads=["scrA"], writes=["KTSs"], key="ldB4")
                P.op("pool", lambda e: e.memset(msel[:], 0.0), writes=["msel"])
                P.dma("sp", KTWs[:], KTWIN.ap(), reads=["scrA"], writes=["KTWs"], key="ldB5")
                for j in range(3):
                    P.op("pool", lambda e, j=j: e.memset(V3s[j][:], 1.0), writes=["V3s%d" % j])
                    P.dma("sp", V3s[j][:, :, 0:64], V3.ap()[:, j, :].rearrange("(b p) d -> p b d", p=128), reads=["scrA"], writes=["V3s%d" % j], key="ldB6%d" % j)
                P.dma("pool", KTSs[64:128, :], c_expand.ap(), writes=["KTSx"], key="ldB7")
                P.dma("pool", OVs[:], c_ov.ap().rearrange("(c p) j -> p c j", p=128), writes=["OVs"], key="ldB8")
                P.dma("sp", tokc[:], c_tokc.ap(), writes=["tokc"], key="ldB9")
                P.dma("sp", identf[:], c_ident4.ap()[:, 0:128], writes=["identf"], key="ldB10")
                P.op("pool", lambda e: e.memset(VCs[:], 1.0), writes=["VCs"])
                for which in range(2 if int(os.environ.get("LIMB", "99")) > 0 else 0):
                    w1_d, w2_d, pos_d, src = ((w1k_d, w2k_d, posk_d, KTCMP), (w1v_d, w2v_d, posv_d, VTCMP))[which]
                    P.dma("pool", w1, w1_d.ap()[l].rearrange("(a d) h -> d a h", d=64), writes=["mneg0"], key="cw1")
                    P.dma("pool", w2[:], w2_d.ap()[l], writes=["w2"], key="cw2")
                    for a in range(32):
                        P.dma("pool", posT[:, a:a + 1], pos_d.ap()[l, a:a + 1, :].rearrange("a d -> d a"), writes=["posT"], key="cpos")
                    P.dma("sp", TCs, src.ap(), reads=["scrA"], writes=["junkb"], key="cTC")
                    P.op("pe", [(lambda e, a=a: e.matmul(pb[1][:, 0:1], lhsT=w1[:, a, :], rhs=posT[:, a:a + 1], start=(a == 0), stop=(a == 31)))
                                for a in range(32)], reads=["mneg0", "posT"], writes=["pb1"])
                    P.op("act", lambda e: e.activation(out=bcol[:], in_=pb[1][:, 0:1], func=AF.Copy), reads=["pb1"], writes=["bcol"])
                    P.op("pe", [(lambda e, a=a: e.matmul(pb[0][:, 0:n_cmp], lhsT=w1[:, a, :], rhs=TCs[:, a:a + 16 * (n_cmp - 1) + 1:16],
                                                         start=(a == 0), stop=(a == 31))) for a in range(32)],
                         reads=["mneg0", "junkb"], writes=["pb0"])
                    P.op("pool", lambda e: e.memset(hid[:], 0.0), writes=["hid"])
                    P.op("act", lambda e: e.activation(out=hid[:, 0:n_cmp], in_=pb[0][:, 0:n_cmp], func=AF.Gelu_apprx_tanh, bias=bcol[:, 0:1]),
                         reads=["pb0", "bcol"], writes=["hid"])
                    if which == 0:
                        P.op("pe", lambda e: e.matmul(pb[2][0:64, 0:NCP], lhsT=w2[:], rhs=hid[:], start=True, stop=True),
                             reads=["w2", "hid"], writes=["pb2"])
                        P.op("act", lambda e: e.activation(out=KCT[:], in_=pb[2][0:64, 0:NCP], func=AF.Copy), reads=["pb2"], writes=["KCT"])
                    else:
                        for c in range(NCC):
                            P.op("pe", lambda e, c=c: e.matmul(pb[2][:, 0:64], lhsT=hid[:, c * 128:(c + 1) * 128], rhs=w2[:], start=True, stop=True),
                                 reads=["w2", "hid"], writes=["pb2"])
                            P.op("act", lambda e, c=c: e.activation(out=VCs[:, c, 0:64], in_=pb[2][:, 0:64], func=AF.Copy), reads=["pb2"], writes=["VCs"])

                ectr = [0]
                tctr = [0]
                LIMB = int(os.environ.get("LIMB", "99"))

                def load_block(i):
                    pi = i % 2
                    ts = slice(i * 128, (i + 1) * 128)
                    for (qt, qn, src) in ((QI, "QI", QTI), (QA, "QA", QTA), (QB, "QB", QTB), (QC, "QC", QTC)):
                        P.dma("sp", qt[pi][0:64, :].rearrange("d (h t) -> d h t", h=4), src.ap()[:, :, ts].rearrange("h d t -> d h t"),
                              reads=[], writes=["%s%d" % (qn, pi)], key="%s%d" % (qn, pi))
                    P.dma("sp", wi[pi][:], WI.ap()[ts, :], writes=["wi%d" % pi], key="wi%d" % pi)
                    P.dma("sp", grow[pi][64:65, :, :].rearrange("o b (h t) -> o b h t", h=4),
                          GC.ap()[:, ts].rearrange("(o b h) t -> o b h t", o=1, b=3), writes=["grow%d" % pi], key="grow%d" % pi)
                    P.dma("pool", cmk[pi][:], c_cmpmask.ap()[i], writes=["cmk%d" % pi], key="cmk%d" % pi)
                    P.dma("sp", smul[pi][:], c_selmul.ap()[i], writes=["smul%d" % pi], key="smul%d" % pi)
                    P.dma("sp", sadd[pi][:], c_seladd.ap()[i], writes=["sadd%d" % pi], key="sadd%d" % pi)

                def chain_P(i):
                    U = []
                    pi = i % 2
                    Lk = (i + 1) * 128
                    qi = QI[pi]
                    qir = "QI%d" % pi
                    mn = mneg[pi]
                    mres = "mneg%d" % pi
                    wres = "wi%d" % pi
                    nch = (Lk + 511) // 512
                    for c in range(nch):
                        w_ = min(512, Lk - c * 512)
                        cs = slice(c * 512, c * 512 + w_)
                        for h in range(4):
                            def u(c=c, h=h, w_=w_, cs=cs):
                                P.op("pe", lambda e: e.matmul(pb[7][:, 0:w_], lhsT=qi[:, h * 128:(h + 1) * 128], rhs=KTIs[:, cs], start=True, stop=True),
                                     reads=[qir, "KTIs"], writes=["pb7"])
                                if h == 0:
                                    P.op("dve", lambda e: e.tensor_scalar(out=score[:, cs], in0=pb[7][:, 0:w_], scalar1=0.0, scalar2=wi[pi][:, 0:1], op0=ALU.max, op1=ALU.mult),
                                         reads=["pb7", wres], writes=["score"])
                                else:
                                    tb = tctr[0] % 2
                                    tctr[0] += 1
                                    P.op("dve", lambda e: e.tensor_scalar(out=tmpi[tb][:, 0:w_], in0=pb[7][:, 0:w_], scalar1=0.0, scalar2=wi[pi][:, h:h + 1], op0=ALU.max, op1=ALU.mult),
                                         reads=["pb7", wres], writes=["tmpi%d" % tb])
                                    P.op("pool", lambda e: e.tensor_tensor(out=score[:, cs], in0=score[:, cs], in1=tmpi[tb][:, 0:w_], op=ALU.add),
                                         reads=["tmpi%d" % tb, "score"], writes=["score"])
                            U.append(u)

                    def u_pre():
                        if Lk > topk:
                            P.op("dve", lambda e: e.tensor_reduce(out=bis[:, 0:1], in_=score[:, 0:Lk], axis=AX.X, op=ALU.min), reads=["score"], writes=["bis"])
                            P.op("dve", lambda e: e.tensor_reduce(out=bis[:, 1:2], in_=score[:, 0:Lk], axis=AX.X, op=ALU.max), reads=["score"], writes=["bis"])
                            P.op("dve", lambda e: e.tensor_tensor(out=bis[:, 1:2], in0=bis[:, 1:2], in1=bis[:, 0:1], op=ALU.subtract), reads=["bis"], writes=["bis"])
                        P.op("pool", lambda e: e.tensor_tensor(out=score[:, i * 128:(i + 1) * 128], in0=score[:, i * 128:(i + 1) * 128], in1=tokc[:], op=ALU.add),
                             reads=["score", "tokc"], writes=["score"])
                    U.append(u_pre)
                    if Lk > topk:
                        def u_it():
                            P.op("dve", lambda e: e.tensor_scalar(out=bis[:, 1:2], in0=bis[:, 1:2], scalar1=0.5, scalar2=None, op0=ALU.mult), reads=["bis"], writes=["bis"])
                            P.op("dve", lambda e: e.tensor_tensor(out=bis[:, 2:3], in0=bis[:, 0:1], in1=bis[:, 1:2], op=ALU.add), reads=["bis"], writes=["bis"])
                            P.op("dve", lambda e: e.tensor_scalar(out=junkb[:, 0:Lk], in0=score[:, 0:Lk], scalar1=bis[:, 2:3], scalar2=None, op0=ALU.is_ge, op1=ALU.add,
                                                                  accum_out=bis[:, 3:4]), reads=["score", "bis"], writes=["junkb", "bis"])
                            P.op("dve", lambda e: e.tensor_scalar(out=bis[:, 4:5], in0=bis[:, 3:4], scalar1=float(topk) - 0.5, scalar2=None, op0=ALU.is_ge), reads=["bis"], writes=["bis"])
                            P.op("dve", lambda e: e.scalar_tensor_tensor(out=bis[:, 0:1], in0=bis[:, 4:5], scalar=bis[:, 1:2], in1=bis[:, 0:1], op0=ALU.mult, op1=ALU.add),
                                 reads=["bis"], writes=["bis"])
                        for it in range(NBIS):
                            U.append(u_it)

                        def u_fin():
                            P.op("dve", lambda e: e.tensor_scalar(out=mn[:, 0:Lk], in0=score[:, 0:Lk], scalar1=bis[:, 0:1], scalar2=NEG, op0=ALU.is_lt, op1=ALU.mult),
                                 reads=["score", "bis"], writes=[mres])
                    else:
                        def u_fin():
                            P.op("dve", lambda e: e.tensor_scalar(out=mn[:, 0:Lk], in0=score[:, 0:Lk], scalar1=-1.0e29, scalar2=NEG, op0=ALU.is_lt, op1=ALU.mult),
                                 reads=["score"], writes=[mres])
                    U.append(u_fin)
                    return U

                def chain_SB(i):
                    U = []
                    pi = i % 2
                    qa = QA[pi]
                    qar = "QA%d" % pi
                    o_st = ost[pi]
                    ostr = "ost%d" % pi
                    kbs = list(range(i, -1, -1))
                    n = len(kbs)

                    def z_ops(kb, bank, start_first):
                        fns = []
                        first = start_first
                        if kb == i:
                            fns.append(lambda e, first=first: e.matmul(pb[bank][:], lhsT=masks[:, 0, :], rhs=ident[:], start=first, stop=False))
                            first = False
                        for h in range(4):
                            fns.append(lambda e, h=h, first=first: e.matmul(
                                pb[bank][:, h * 128:(h + 1) * 128], lhsT=KTAs[:, h, kb * 128:(kb + 1) * 128], rhs=qa[:, h * 128:(h + 1) * 128],
                                start=first, stop=(h == 3)))
                            first = False
                        return fns

                    def stage1(j):
                        kb = kbs[j]
                        bk = j % 2
                        P.op("pe", z_ops(kb, bk, True), reads=["KTAs", qar, "masks", "ident"], writes=["pb%d" % bk])
                        P.op("act", lambda e: e.activation(out=e1[bk][:], in_=pb[bk][:], func=AF.Exp), reads=["pb%d" % bk], writes=["e1_%d" % bk])
                        P.op("act", lambda e: e.activation(out=sp_[bk][:], in_=e1[bk][:], func=AF.Ln, bias=1.0), reads=["e1_%d" % bk], writes=["sp_%d" % bk])

                    def stage2(j):
                        kb = kbs[j]
                        bk = j % 2
                        fns = [lambda e: e.matmul(pb[bk][:], lhsT=masks[:, 3, :], rhs=sp_[bk][:], start=False, stop=(j == 0))]
                        rds = ["sp_%d" % bk, "masks"]
                        if j > 0:
                            P.op("act", lambda e: e.activation(out=chi[0:2, :], in_=pb[2][0:2, :], func=AF.Copy), reads=["pb2"], writes=["chi"])
                            P.op("dve", lambda e: e.scalar_tensor_tensor(out=crow2[0:2, :], in0=chi[0:2, :], scalar=negm[0:2, 0:1], in1=pb[2][0:2, :],
                                                                          op0=ALU.mult, op1=ALU.add), reads=["chi", "negm", "pb2"], writes=["crow2"])
                            fns.append(lambda e: e.matmul(pb[bk][:], lhsT=onesb[0:2, :], rhs=crow2[0:2, :], start=False, stop=True))
                            rds += ["onesb", "crow2"]
                        P.op("pe", fns, reads=rds, writes=["pb%d" % bk])
                        P.op("pe", lambda e: e.matmul(pb[2][:], lhsT=masks[:, 4, :], rhs=sp_[bk][:], start=(j == 0), stop=(j == n - 1)),
                             reads=["sp_%d" % bk, "masks"], writes=["pb2"])
                        P.op("act", lambda e: e.activation(out=Wt[bk][:], in_=pb[bk][:], func=AF.Exp), reads=["pb%d" % bk], writes=["Wt%d" % bk])
                        P.op("pe", [(lambda e, h=h: e.matmul(pb[3][0:64, h * 128:(h + 1) * 128], lhsT=VAs[:, kb, h * 64:(h + 1) * 64],
                                                             rhs=Wt[bk][:, h * 128:(h + 1) * 128], start=(j == 0 and h == 0), stop=(j == n - 1)))
                                    for h in range(4)], reads=["Wt%d" % bk, "VAs"], writes=["pb3"])

                    def u0():
                        stage1(0)
                    U.append(u0)
                    for j in range(n):
                        def u(j=j):
                            if j + 1 < n:
                                stage1(j + 1)
                            stage2(j)
                        U.append(u)

                    def ufin():
                        P.op("act", lambda e: e.activation(out=o_st[:, 0, :], in_=pb[3][0:64, :], func=AF.Copy), reads=["pb3"], writes=[ostr])
                    U.append(ufin)
                    return U

                def attn_units(U, nsteps, s_ops, v_ap, v_res, scale=0.125):
                    def emitS(j):
                        bk = 4 + j % 2
                        fns, rd = s_ops(j, bk)
                        P.op("pe", fns, reads=rd, writes=["pb%d" % bk])

                    def unit(j):
                        if j == 0:
                            emitS(0)
                        if j + 1 < nsteps:
                            emitS(j + 1)
                        bk = 4 + j % 2
                        eb_i = ectr[0] % 3
                        ectr[0] += 1
                        E = Eb[eb_i]
                        P.op("act", lambda e: e.activation(out=E[:], in_=pb[bk][:], func=AF.Exp, scale=scale),
                             reads=["pb%d" % bk], writes=["Eb%d" % eb_i])
                        P.op("pe", lambda e: e.matmul(pb[6][0:65, :], lhsT=v_ap(j), rhs=E[:], start=(j == 0), stop=(j == nsteps - 1)),
                             reads=["Eb%d" % eb_i, v_res], writes=["pb6"])
                    for j in range(nsteps):
                        U.append((lambda j=j: unit(j)))

                def chain_ATT(i):
                    U = []
                    pi = i % 2
                    ts = slice(i * 128, (i + 1) * 128)
                    qb, qc = QB[pi], QC[pi][0:64, :]
                    qbr, qcr = "QB%d" % pi, "QC%d" % pi
                    qaug = QC[pi]
                    msr = "mselT%d" % pi
                    o_st = ost[pi]
                    ostr = "ost%d" % pi
                    mn = mneg[pi]
                    mres = "mneg%d" % pi
                    gres = "grow%d" % pi

                    def dsa_s(j, bk):
                        return ([lambda e: e.matmul(pb[bk][:], lhsT=mn[:, j * 128:(j + 1) * 128], rhs=ident[:], start=True, stop=False),
                                 lambda e: e.matmul(pb[bk][:], lhsT=KTBs[:, j * 128:(j + 1) * 128], rhs=qb[:], start=False, stop=True)],
                                [mres, "ident", "KTBs", qbr])
                    attn_units(U, i + 1, dsa_s, lambda j: V3s[0][:, j, :], "V3s0")
                    U.append(lambda: attn_finish(pb[6], "pb6", o_st[:, 1, :], ostr, (rrow, bcs, None), bcbank=4))

                    def u_cmp():
                        for j in range(NCC):
                            P.op("pe", [lambda e: e.matmul(pb[4][:], lhsT=cmk[pi][:, j * 128:(j + 1) * 128], rhs=ident[:], start=True, stop=False),
                                        lambda e: e.matmul(pb[4][:], lhsT=KCT[:, j * 128:(j + 1) * 128], rhs=qc[:], start=False, stop=True)],
                                 reads=["cmk%d" % pi, "ident", "KCT", qcr], writes=["pb4"])
                            eb_i = ectr[0] % 3
                            ectr[0] += 1
                            E = Eb[eb_i]
                            P.op("act", lambda e: e.activation(out=E[:], in_=pb[4][:], func=AF.Exp, scale=0.125), reads=["pb4"], writes=["Eb%d" % eb_i])
                            P.op("pe", lambda e: e.matmul(pb[6][0:65, :], lhsT=VCs[:, j, :], rhs=E[:], start=(j == 0), stop=(j == NCC - 1)),
                                 reads=["Eb%d" % eb_i, "VCs"], writes=["pb6"])
                            P.op("pe", lambda e: e.matmul(pb[5][0:64, :], lhsT=OVs[:, j, :], rhs=E[:], start=(j == 0), stop=(j == NCC - 1)),
                                 reads=["Eb%d" % eb_i, "OVs"], writes=["pb5"])
                    U.append(u_cmp)

                    def u_imp():
                        P.op("dve", lambda e: e.tensor_scalar(out=rrow[64:65, :], in0=pb[6][64:65, :], scalar1=1e-30, scalar2=None, op0=ALU.max), reads=["pb6"], writes=["rrow"])
                        P.op("act", lambda e: e.activation(out=rrow[64:65, :], in_=rrow[64:65, :], func=AF.Ln), reads=["rrow"], writes=["rrow"])
                        P.op("act", lambda e: e.activation(out=rrow[64:65, :], in_=rrow[64:65, :], func=AF.Exp, scale=-1.0), reads=["rrow"], writes=["rrow"])
                        P.op("pe", lambda e: e.matmul(pb[4][0:64, :], lhsT=ones32[64:65, 0:64], rhs=rrow[64:65, :], start=True, stop=True), reads=["rrow", "ones32"], writes=["pb4"])
                        P.op("act", lambda e: e.activation(out=bcs[0:64, :], in_=pb[4][0:64, :], func=AF.Copy), reads=["pb4"], writes=["bcs"])
                        P.op("dve", lambda e: e.tensor_tensor(out=uimp[:], in0=pb[5][0:64, :], in1=bcs[0:64, :], op=ALU.mult), reads=["pb5", "bcs"], writes=["uimp"])
                        P.op("dve", lambda e: e.tensor_tensor(out=impT[:], in0=uimp[:, 0:128], in1=uimp[:, 128:256], op=ALU.add), reads=["uimp"], writes=["impT"])
                        P.op("dve", lambda e: e.tensor_tensor(out=impT[:], in0=impT[:], in1=uimp[:, 256:384], op=ALU.add), reads=["uimp", "impT"], writes=["impT"])
                        P.op("dve", lambda e: e.tensor_tensor(out=impT[:], in0=impT[:], in1=uimp[:, 384:512], op=ALU.add), reads=["uimp", "impT"], writes=["impT"])
                        P.op("dve", lambda e: e.tensor_tensor(out=rrow[64:65, :], in0=rrow[64:65, :], in1=grow[pi][64:65, 0, :], op=ALU.mult), reads=["rrow", gres], writes=["rrow"])
                        P.op("pe", lambda e: e.matmul(pb[4][0:64, :], lhsT=ones32[64:65, 0:64], rhs=rrow[64:65, :], start=True, stop=True), reads=["rrow", "ones32"], writes=["pb4"])
                        P.op("act", lambda e: e.activation(out=bcs[0:64, :], in_=pb[4][0:64, :], func=AF.Copy), reads=["pb4"], writes=["bcs"])
                        P.op("dve", lambda e: e.tensor_tensor(out=oacc[:], in0=pb[6][0:64, :], in1=bcs[0:64, :], op=ALU.mult), reads=["pb6", "bcs"], writes=["oacc"])
                    U.append(u_imp)

                    def u_top():
                        P.op("pe", lambda e: e.matmul(pb[4][:, 0:64], lhsT=impT[:], rhs=identf[0:64, 0:64], start=True, stop=True), reads=["impT", "identf"], writes=["pb4"])
                        P.op("dve", lambda e: e.tensor_tensor(out=imp[:], in0=pb[4][:, 0:64], in1=smul[pi][:], op=ALU.mult), reads=["pb4", "smul%d" % pi], writes=["imp"])
                        P.op("dve", lambda e: e.tensor_tensor(out=imp[:], in0=imp[:], in1=sadd[pi][:], op=ALU.add), reads=["imp", "sadd%d" % pi], writes=["imp"])
                        P.op("dve", lambda e: e.max(out=m8[:, 0:8], in_=imp[:]), reads=["imp"], writes=["m8"])
                        if topn > 8:
                            P.op("dve", lambda e: e.match_replace(out=imp2[:], in_to_replace=m8[:, 0:8], in_values=imp[:], imm_value=-BIGF), reads=["imp", "m8"], writes=["imp2"])
                            P.op("dve", lambda e: e.max(out=m8[:, 8:16], in_=imp2[:]), reads=["imp2"], writes=["m8"])
                            lo_, hi_ = 8, 8 + (topn - 8)
                        else:
                            lo_, hi_ = 0, topn
                        P.op("dve", lambda e: e.tensor_reduce(out=bis[:, 5:6], in_=m8[:, lo_:hi_], axis=AX.X, op=ALU.min), reads=["m8"], writes=["bis5"])
                        P.op("dve", lambda e: e.tensor_scalar(out=bis[:, 5:6], in0=bis[:, 5:6], scalar1=-1.0e29, scalar2=None, op0=ALU.max), reads=["bis5"], writes=["bis5"])
                        P.op("dve", lambda e: e.tensor_scalar(out=msel[:, 64:128], in0=imp[:], scalar1=bis[:, 5:6], scalar2=NEG, op0=ALU.is_lt, op1=ALU.mult), reads=["imp", "bis5"], writes=["msel"])
                        P.op("pe", lambda e: e.matmul(pb[4][:, 0:128], lhsT=msel[:], rhs=identf[:, :], start=True, stop=True), reads=["msel", "identf"], writes=["pb4"])
                        P.op("act", [(lambda e, h=h: e.activation(out=qaug[64:128, h * 128:(h + 1) * 128], in_=pb[4][64:128, 0:128], func=AF.Copy)) for h in range(4)],
                             reads=["pb4"], writes=[msr])
                    U.append(u_top)

                    def sel_s(j, bk):
                        fns = []
                        if j == i:
                            fns.append(lambda e: e.matmul(pb[bk][:], lhsT=masks[:, 1, :], rhs=ident[:], start=True, stop=False))
                        fns.append(lambda e: e.matmul(pb[bk][:], lhsT=KTSs[:, j * 128:(j + 1) * 128], rhs=qaug[:, :], start=(j != i), stop=True))
                        return fns, ["KTSx", msr, "masks", "ident", "KTSs", qcr]
                    attn_units(U, i + 1, sel_s, lambda j: V3s[1][:, j, :], "V3s1")
                    U.append(lambda: attn_finish(pb[6], "pb6", None, None, (rrow, bcs, (oacc, False)), gate=(grow[pi][64:65, 1, :], gres), bcbank=4))
                    wk0 = max(0, i - 4)

                    def win_s(j, bk):
                        kb = wk0 + j
                        fns = []
                        first = True
                        if kb == i:
                            fns.append(lambda e: e.matmul(pb[bk][:], lhsT=masks[:, 1, :], rhs=ident[:], start=True, stop=False))
                            first = False
                        if kb == i - 4:
                            fns.append(lambda e, first=first: e.matmul(pb[bk][:], lhsT=masks[:, 2, :], rhs=ident[:], start=first, stop=False))
                            first = False
                        fns.append(lambda e, first=first: e.matmul(pb[bk][:], lhsT=KTWs[:, kb * 128:(kb + 1) * 128], rhs=qc[:], start=first, stop=True))
                        return fns, ["masks", "ident", "KTWs", qcr]
                    attn_units(U, i - wk0 + 1, win_s, lambda j: V3s[2][:, wk0 + j, :], "V3s2")

                    def u_end():
                        attn_finish(pb[6], "pb6", None, None, (rrow, bcs, (oacc, False)), gate=(grow[pi][64:65, 2, :], gres), bcbank=4)
                        P.op("act", lambda e: e.activation(out=o_st[:, 2, :], in_=oacc[:], func=AF.Copy), reads=["oacc"], writes=[ostr])
                    U.append(u_end)
                    return U

                def store_o(i):
                    pi = i % 2
                    ts = slice(i * 128, (i + 1) * 128)
                    P.dma("sp", OT.ap()[:, :, ts].rearrange("(b h) d t -> d b h t", b=3), ost[pi][:].rearrange("d b (h t) -> d b h t", h=4),
                          reads=["ost%d" % pi], writes=[], key="ost%d" % pi)

                def run_merged(chains):
                    items = []
                    for ci, U in enumerate(chains):
                        P.capture = []
                        for f in U:
                            f()
                        ops_ = P.capture
                        P.capture = None
                        n = len(ops_)
                        for k, it_ in enumerate(ops_):
                            items.append(((k + 0.5) / n, ci, k, it_))
                    items.sort(key=lambda t: (t[0], t[1], t[2]))
                    for _, _, _, it_ in items:
                        P.commit(it_)

                load_block(0)
                run_merged([chain_P(0)])
                for i in range(NB):
                    if i + 1 < NB:
                        load_block(i + 1)
                    chains = [chain_SB(i), chain_ATT(i)]
                    if i + 1 < NB:
                        chains.append(chain_P(i + 1))
                    run_merged(chains)
                    store_o(i)
                P.barrier()
                P.emit_all()
            if debug == "B":
                break
            with ExitStack() as ec:
                sb = lambda n, s, d: ec.enter_context(nc.sbuf_tensor(n + "_L%d" % li, list(s), d))
                alloc_psum(ec, "C%d" % li, 7)
                wup = sb("wup", [64, 12, D], BF16); wout = sb("wout", [128, 8, D], BF16)
                wmq = sb("wmq", [128, 8, 256], BF16); wmo = sb("wmo", [64, 4, D], BF16); wmkv = sb("wmkv", [128, 8, 512], BF16)
                gq = sb("gq", [128, D], F32); gkv = sb("gkv", [128, D], F32)
                KmT = sb("KmT", [64, 4, NMEM], BF16); Vm = sb("Vm", [128, 2, 4, 65], BF16)
                oT = sb("oT", [64, 12, 512], BF16); gT = sb("gT", [128, 24, 512], BF16)
                mT = sb("mT", [128, 8, 512], BF16)
                tm = [sb("tm%d" % i, [128, 512], F32) for i in range(3)]
                hin = [sb("hin%d" % i, [128, D], F32) for i in range(2)]
                h1 = sb("h1", [128, 4, D], F32)
                junk = sb("junkC", [128, D], F32); ubf = sb("ubfC", [128, D], BF16)
                uT2 = sb("uT2", [128, 8, 512], BF16)
                qmT = sb("qmT", [64, 4, 512], BF16)
                Eb = [sb("EbC%d" % i, [128, 512], BF16) for i in range(2)]
                omT = sb("omT", [64, 4, 512], BF16)
                rrow = sb("rrowC", [128, 512], F32); bcs = sb("bcsC", [64, 512], F32)
                hout = [sb("houtC%d" % i, [128, D], F32) for i in range(2)]
                memb = sb("memb", [128, D], F32); mTm = sb("mTm", [128, 8, NMEM], BF16)
                P.dma("pool", wup[:], wup_d.ap()[l].rearrange("b (h d) n -> d (b h) n", d=64), writes=["wup"], key="c1w0")
                P.dma("pool", wout[:], wout_d.ap()[l].rearrange("(k p) n -> p k n", p=128), writes=["wout"], key="c1w1")
                P.dma("pool", wmq[:], wmq_d.ap()[l].rearrange("(k p) n -> p k n", p=128), writes=["wmq"], key="c1w2")
                P.dma("pool", wmo[:], wmo_d.ap()[l].rearrange("(h d) n -> d h n", d=64), writes=["wmo"], key="c1w3")
                P.dma("pool", wmkv[:], wmkv_d.ap()[l].rearrange("(k p) n -> p k n", p=128), writes=["wmkv"], key="c1w4")
                P.dma("sp", gq[:], bcrow(norms_d, 4 * l + 1), writes=["gq"], key="c1g0")
                P.dma("sp", gkv[:], bcrow(norms_d, 4 * l + 2), writes=["gkv"], key="c1g1")
                P.op("pool", lambda e: e.memset(Vm[:], 1.0), writes=["Vm"])
                for mc in range(2):
                    P.dma("sp", memb[:], mem_d.ap()[mc * 128:(mc + 1) * 128, :], writes=["memb"], key="c1mem")
                    rmsnorm_T(memb[:], "memb", gkv, "gkv", mTm[:, :, mc * 128:(mc + 1) * 128], "mTm", (junk, ubf))
                for h in range(4):
                    P.op("pe", [(lambda e, kc=kc, h=h: e.matmul(pb[0][0:64, 0:NMEM], lhsT=wmkv[:, kc, h * 64:(h + 1) * 64], rhs=mTm[:, kc, :],
                                                                start=(kc == 0), stop=(kc == 7))) for kc in range(8)], reads=["wmkv", "mTm"], writes=["pb0"])
                    P.op("act", lambda e, h=h: e.activation(out=KmT[:, h, :], in_=pb[0][0:64, 0:NMEM], func=AF.Copy), reads=["pb0"], writes=["KmT"])
                for mc in range(2):
                    P.op("pe", [(lambda e, kc=kc, mc=mc: e.matmul(pb[1][:, 0:256], lhsT=mTm[:, kc, mc * 128:(mc + 1) * 128], rhs=wmkv[:, kc, 256:512],
                                                                  start=(kc == 0), stop=(kc == 7))) for kc in range(8)], reads=["wmkv", "mTm"], writes=["pb1"])
                    P.op("act", lambda e, mc=mc: e.activation(out=Vm[:, mc, :, 0:64], in_=pb[1][:, 0:256].rearrange("p (h d) -> p h d", h=4), func=AF.Copy),
                         reads=["pb1"], writes=["Vm"])
                for g in range(NG):
                    gs = slice(g * 512, (g + 1) * 512)
                    P.dma("sp", oT[:], OT.ap()[:, :, gs].rearrange("c d s -> d c s"), reads=["scrB"], writes=["oT"], key="c1oT")
                    P.dma("sp", gT[:], GT.ap()[:, gs].rearrange("(c p) s -> p c s", p=128), reads=["scrA"], writes=["gT"], key="c1gT")
                    for fc in range(8):
                        for br in range(3):
                            P.op("pe", [(lambda e, h=h, br=br, fc=fc: e.matmul(pb[br][:], lhsT=wup[:, br * 4 + h, fc * 128:(fc + 1) * 128], rhs=oT[:, br * 4 + h, :],
                                                                                start=(h == 0), stop=(h == 3))) for h in range(4)], reads=["wup", "oT"], writes=["pb%d" % br])
                            P.op("dve", lambda e, br=br, fc=fc: e.tensor_tensor(out=tm[br][:], in0=pb[br][:], in1=gT[:, br * 8 + fc, :], op=ALU.mult),
                                 reads=["pb%d" % br, "gT"], writes=["tm%d" % br])
                        P.op("pool", lambda e: e.tensor_tensor(out=tm[0][:], in0=tm[0][:], in1=tm[1][:], op=ALU.add), reads=["tm0", "tm1"], writes=["tm0"])
                        P.op("pool", lambda e, fc=fc: e.tensor_tensor(out=mT[:, fc, :], in0=tm[0][:], in1=tm[2][:], op=ALU.add), reads=["tm0", "tm2"], writes=["mT"])
                    for b4 in range(4):
                        blk = g * 4 + b4
                        hi_ = hin[blk % 2]
                        P.dma("sp", hi_[:], h_in.ap()[blk * 128:(blk + 1) * 128, :], writes=["hin%d" % (blk % 2)], key="hin%d" % (blk % 2))
                        for half in range(2):
                            bk = 3 + half
                            P.op("pe", [(lambda e, kc=kc, half=half, bk=bk, b4=b4: e.matmul(pb[bk][:], lhsT=mT[:, kc, b4 * 128:(b4 + 1) * 128], rhs=wout[:, kc, half * 512:(half + 1) * 512],
                                                                                          start=(kc == 0), stop=(kc == 7))) for kc in range(8)], reads=["mT", "wout"], writes=["pb%d" % bk])
                            P.op("dve", lambda e, half=half, bk=bk, b4=b4, hi_=hi_: e.tensor_tensor(out=h1[:, b4, half * 512:(half + 1) * 512], in0=pb[bk][:], in1=hi_[:, half * 512:(half + 1) * 512], op=ALU.add),
                                 reads=["pb%d" % bk, "hin%d" % (blk % 2)], writes=["h1_%d" % b4])
                        rmsnorm_T(h1[:, b4, :], "h1_%d" % b4, gq, "gq", uT2[:, :, b4 * 128:(b4 + 1) * 128], "uT2", (junk, ubf))
                    for h in range(4):
                        bk = h % 2
                        P.op("pe", [(lambda e, kc=kc, h=h, bk=bk: e.matmul(pb[bk][0:64, :], lhsT=wmq[:, kc, h * 64:(h + 1) * 64], rhs=uT2[:, kc, :], start=(kc == 0), stop=(kc == 7)))
                                    for kc in range(8)], reads=["wmq", "uT2"], writes=["pb%d" % bk])
                        P.op("act", lambda e, h=h, bk=bk: e.activation(out=qmT[:, h, :], in_=pb[bk][0:64, :], func=AF.Copy), reads=["pb%d" % bk], writes=["qmT"])
                    for h in range(4):
                        for mc in range(2):
                            bk = mc
                            P.op("pe", lambda e, h=h, mc=mc, bk=bk: e.matmul(pb[bk][:], lhsT=KmT[:, h, mc * 128:(mc + 1) * 128], rhs=qmT[:, h, :], start=True, stop=True),
                                 reads=["KmT", "qmT"], writes=["pb%d" % bk])
                            P.op("act", lambda e, mc=mc, bk=bk: e.activation(out=Eb[mc][:], in_=pb[bk][:], func=AF.Exp, scale=0.125), reads=["pb%d" % bk], writes=["EbC%d" % mc])
                            P.op("pe", lambda e, h=h, mc=mc: e.matmul(pb[5][0:65, :], lhsT=Vm[:, mc, h, :], rhs=Eb[mc][:], start=(mc == 0), stop=(mc == 1)),
                                 reads=["Vm", "EbC%d" % mc], writes=["pb5"])
                        attn_finish(pb[5], "pb5", omT[:, h, :], "omT", (rrow, bcs, None))
                    for b4 in range(4):
                        blk = g * 4 + b4
                        ho = hout[blk % 2]
                        for half in range(2):
                            bk = 3 + half
                            P.op("pe", [(lambda e, h=h, half=half, bk=bk, b4=b4: e.matmul(pb[bk][:], lhsT=omT[:, h, b4 * 128:(b4 + 1) * 128], rhs=wmo[:, h, half * 512:(half + 1) * 512],
                                                                                        start=(h == 0), stop=(h == 3))) for h in range(4)], reads=["omT", "wmo"], writes=["pb%d" % bk])
                            P.op("dve", lambda e, half=half, bk=bk, b4=b4, ho=ho: e.tensor_tensor(out=ho[:, half * 512:(half + 1) * 512], in0=pb[bk][:], in1=h1[:, b4, half * 512:(half + 1) * 512], op=ALU.add),
                                 reads=["pb%d" % bk, "h1_%d" % b4], writes=["hout%d" % (blk % 2)])
                        P.dma("sp", HM.ap()[blk * 128:(blk + 1) * 128, :], ho[:], reads=["hout%d" % (blk % 2)], writes=[], key="hout%d" % (blk % 2))
                P.barrier()
                P.emit_all()
            if debug == "C1":
                break
            with ExitStack() as ef:
                sb = lambda n, s, d: ef.enter_context(nc.sbuf_tensor(n + "_L%d" % li, list(s), d))
                alloc_psum(ef, "F%d" % li, 7)
                wfi = sb("wfi", [128, 8, 2 * DFF], BF16); wfo = sb("wfo", [128, 22, D], BF16)
                gf = sb("gf", [128, D], F32); gfin = sb("gfin", [128, D], F32)
                hmb2 = [sb("hmb%d" % i_, [128, 2, D], F32) for i_ in range(2)]
                junk = sb("junkF", [128, D], F32); ubf = sb("ubfF", [128, D], BF16)
                uT32 = [sb("uT3_%d" % i_, [128, 8, 256], BF16) for i_ in range(2)]
                sil = [sb("sil%d" % i, [128, 256], F32) for i in range(2)]
                actT = sb("actT", [128, 22, 256], BF16)
                hf = [sb("hf%d" % i, [128, D], F32) for i in range(2)]
                for kc in range(8):
                    P.dma("pool", wfi[:, kc, :], wfi_d.ap()[l, kc * 128:(kc + 1) * 128, :], writes=["wfi"], key="c2w%d" % kc)
                P.dma("pool", wfo[:, 0:11, :], wfo_d.ap()[l, 0:11 * 128, :].rearrange("(c p) n -> p c n", p=128), writes=["wfo"], key="c2wo0")
                P.dma("pool", wfo[:, 11:22, :], wfo_d.ap()[l, 11 * 128:22 * 128, :].rearrange("(c p) n -> p c n", p=128), writes=["wfo"], key="c2wo1")
                P.dma("sp", gf[:], bcrow(norms_d, 4 * l + 3), writes=["gf"], key="c2g0")
                P.dma("sp", gfin[:], bcrow(norms_d, 4 * L), writes=["gfin"], key="c2g1")
                def norm_ops(g2):
                    par = g2 % 2
                    for b2 in range(2):
                        blk = g2 * 2 + b2
                        hres = "hmb%d_%d" % (par, b2)
                        P.dma("sp", hmb2[par][:, b2, :], HM.ap()[blk * 128:(blk + 1) * 128, :], writes=[hres], key=hres)
                        rmsnorm_T(hmb2[par][:, b2, :], hres, gf, "gf", uT32[par][:, :, b2 * 128:(b2 + 1) * 128], "uT3_%d" % par, (junk, ubf))

                def cloop_ops(g2):
                    par = g2 % 2
                    uT3 = uT32[par]
                    ures = "uT3_%d" % par

                    def a_ops(c):
                        bk = c % 2
                        P.op("pe", [(lambda e, kc=kc: e.matmul(pb[bk][:, 0:256], lhsT=wfi[:, kc, c * 128:(c + 1) * 128], rhs=uT3[:, kc, :], start=(kc == 0), stop=(kc == 7)))
                                    for kc in range(8)] +
                                   [(lambda e, kc=kc: e.matmul(pb[bk][:, 256:512], lhsT=wfi[:, kc, DFF + c * 128:DFF + (c + 1) * 128], rhs=uT3[:, kc, :], start=False, stop=(kc == 7)))
                                    for kc in range(8)], reads=["wfi", ures], writes=["pb%d" % bk])
                    a_ops(0)
                    for c in range(22):
                        bk = c % 2
                        if c + 1 < 22:
                            a_ops(c + 1)
                        P.op("act", lambda e: e.activation(out=sil[bk][:], in_=pb[bk][:, 0:256], func=AF.Silu), reads=["pb%d" % bk], writes=["sil%d" % bk])
                        P.op("dve", lambda e: e.tensor_tensor(out=actT[:, c, :], in0=pb[bk][:, 256:512], in1=sil[bk][:], op=ALU.mult),
                             reads=["pb%d" % bk, "sil%d" % bk], writes=["actT%d" % c])
                        for b2 in range(2):
                            for half in range(2):
                                ob = 2 + b2 * 2 + half
                                P.op("pe", lambda e: e.matmul(pb[ob][:], lhsT=actT[:, c, b2 * 128:(b2 + 1) * 128], rhs=wfo[:, c, half * 512:(half + 1) * 512],
                                                              start=(c == 0), stop=(c == 21)), reads=["actT%d" % c, "wfo"], writes=["pb%d" % ob])

                def merge_run(thunks):
                    items = []
                    for ci, f in enumerate(thunks):
                        P.capture = []
                        f()
                        ops_ = P.capture
                        P.capture = None
                        n_ = len(ops_)
                        for k, it_ in enumerate(ops_):
                            items.append(((k + 0.5) / n_, ci, k, it_))
                    items.sort(key=lambda t: (t[0], t[1], t[2]))
                    for _, _, _, it_ in items:
                        P.commit(it_)

                norm_ops(0)
                for g2 in range(S // 256):
                    th = [lambda: cloop_ops(g2)]
                    if g2 + 1 < S // 256:
                        th.append(lambda: norm_ops(g2 + 1))
                    merge_run(th)
                    hmb = hmb2[g2 % 2]
                    for b2 in range(2):
                        blk = g2 * 2 + b2
                        hft = hf[b2]
                        for half in range(2):
                            ob = 2 + b2 * 2 + half
                            P.op("dve", lambda e, ob=ob, half=half, b2=b2, hft=hft: e.tensor_tensor(out=hft[:, half * 512:(half + 1) * 512], in0=pb[ob][:], in1=hmb[:, b2, half * 512:(half + 1) * 512], op=ALU.add),
                                 reads=["pb%d" % ob, "hmb%d_%d" % (g2 % 2, b2)], writes=["hf%d" % b2])
                        if not last:
                            P.dma("sp", HO.ap()[blk * 128:(blk + 1) * 128, :], hft[:], reads=["hf%d" % b2], writes=[], key="hf%d" % b2)
                        else:
                            P.op("act", lambda e, hft=hft: e.activation(out=junk[:], in_=hft[:], func=AF.Square, accum_out=stat[:, 4:5]), reads=["hf%d" % b2], writes=["junk", "stat4"])
                            P.op("act", lambda e: e.activation(out=stat[:, 5:6], in_=stat[:, 4:5], func=AF.Sqrt, scale=1.0 / D, bias=1e-6), reads=["stat4"], writes=["stat5"])
                            P.op("dve", lambda e: e.reciprocal(out=stat[:, 6:7], in_=stat[:, 5:6]), reads=["stat5"], writes=["stat6"])
                            P.op("dve", lambda e, hft=hft: e.scalar_tensor_tensor(out=hft[:], in0=hft[:], scalar=stat[:, 6:7], in1=gfin[:], op0=ALU.mult, op1=ALU.mult),
                                 reads=["hf%d" % b2, "stat6", "gfin"], writes=["hf%d" % b2])
                            P.dma("sp", out_d.ap()[blk * 128:(blk + 1) * 128, :], hft[:], reads=["hf%d" % b2], writes=[], key="hf%d" % b2)
                P.barrier()
                P.emit_all()
    return nc


NBIS = 16

_CACHE = {}


def prep_shared(inputs, S, layers):
    cols = _col_index()
    sh = {}
    w_in = np.asarray(inputs['w_in'], np.float32)
    sh['wA'] = np.ascontiguousarray(np.stack([w_in[l][:, cols] for l in layers], 0))
    for k in ('cmp_w1_k', 'cmp_w2_k', 'cmp_pos_k', 'cmp_w1_v', 'cmp_w2_v', 'cmp_pos_v', 'w_up', 'w_out',
              'w_mem_q', 'w_mem_kv', 'w_mem_o', 'w_ffn_in', 'w_ffn_out'):
        a = np.asarray(inputs[k], np.float32)
        sh[k] = np.ascontiguousarray(np.stack([a[l] for l in layers], 0))
    rows = []
    for l in layers:
        rows += [np.asarray(inputs[k], np.float32)[l] for k in ('norm_mix', 'norm_mem_q', 'norm_mem_kv', 'norm_ffn')]
    rows.append(np.asarray(inputs['norm_final'], np.float32))
    sh['norms'] = np.ascontiguousarray(np.stack(rows, 0))
    sh.update(make_consts(S))
    return sh


def kernel(**inputs):
    x = np.asarray(inputs['x'], np.float32)
    mem = np.asarray(inputs['mem'], np.float32)
    B, S, _ = x.shape
    layers = [0, 1]
    key = (S, tuple(layers))
    if key not in _CACHE:
        _CACHE[key] = build(S, layers)
    nc = _CACHE[key]
    sh = prep_shared(inputs, S, layers)
    in_maps = []
    for b in range(B):
        m = dict(sh)
        m['x'] = np.ascontiguousarray(x[b])
        m['mem'] = np.ascontiguousarray(mem[b])
        in_maps.append(m)
    res = run_bass_kernel_spmd(nc, in_maps, core_ids=list(range(B)))
    return np.stack([np.asarray(r['out'], np.float32) for r in res.results], 0)
```

```python
import numpy as np
import concourse.bass as bass
import concourse.mybir as mybir
from concourse.bass_utils import run_bass_kernel_spmd
from contextlib import ExitStack
import os

F32 = mybir.dt.float32
BF16 = mybir.dt.bfloat16
AF = mybir.ActivationFunctionType
ALU = mybir.AluOpType
AX = mybir.AxisListType

EPOCH = 30000
D = 1024
DFF = 2816
NEG = -30000.0
BIGF = 1.0e30
NMEM = 256
TOPK = 256
NA_FM = 9 * 64 + 17 * 2 * 64 + 12 + 3072
NA = NA_FM + 452


class _Rec:
    def __init__(self):
        self.calls = []

    def __getattr__(self, name):
        def f(*a, **kw):
            self.calls.append((name, a, kw))
            return self
        return f


class Prog:
    ENGS = ("pe", "act", "dve", "pool", "sp")

    def __init__(self, nc, es):
        self.nc = nc
        self.es = es
        self.ops = {e: [] for e in self.ENGS}
        self.cnt = {e: 0 for e in self.ENGS}
        self.seen = {e: {} for e in self.ENGS}
        self.sems = {}
        self.latest = {}
        self.res_w = {}
        self.res_r = {}
        self.dcnt = {}
        self.capture = None

    def sem(self, key):
        if key not in self.sems:
            self.sems[key] = self.es.enter_context(
                self.nc.semaphore("s_" + key.replace("#", "_").replace(":", "_")))
        return self.sems[key]

    def _deps(self, eng, reads, writes):
        deps = {}
        for r in reads:
            ev = self.res_w.get(r)
            if ev is not None:
                deps[ev[0]] = max(deps.get(ev[0], 0), ev[1])
        for w in writes:
            ev = self.res_w.get(w)
            if ev is not None:
                deps[ev[0]] = max(deps.get(ev[0], 0), ev[1])
            for ev in self.res_r.get(w, ()):
                deps[ev[0]] = max(deps.get(ev[0], 0), ev[1])
        out = []
        for k, v in deps.items():
            if eng == "pe" and k.startswith("pe#"):
                continue
            if self.seen[eng].get(k, 0) >= v:
                continue
            self.seen[eng][k] = v
            out.append((k, v))
        return out

    def _commit(self, ev, reads, writes):
        self.latest[ev[0]] = max(self.latest.get(ev[0], 0), ev[1])
        for w in writes:
            self.res_w[w] = ev
            self.res_r[w] = []
        for r in reads:
            if r in writes:
                continue
            lst = self.res_r.setdefault(r, [])
            lst.append(ev)
            if len(lst) > 16:
                d = {}
                for k, v in lst:
                    d[k] = max(d.get(k, 0), v)
                self.res_r[r] = list(d.items())

    def op(self, eng, fns, reads=(), writes=()):
        if callable(fns):
            fns = [fns]
        rec = _Rec()
        for f in fns:
            f(rec)
        calls = rec.calls
        if self.capture is not None:
            self.capture.append(("op", eng, calls, list(reads), list(writes)))
            return None
        return self._op_commit(eng, calls, reads, writes)

    def _op_commit(self, eng, calls, reads, writes):
        px = [r for r in reads if r.startswith("pb") or r == "ptr"]
        if px:
            reads = [r for r in reads if r not in px]
            writes = list(writes) + [r for r in px if r not in writes]
        waits = self._deps(eng, reads, writes)
        c = self.cnt[eng] + 1
        self.cnt[eng] = c
        key = "%s#%d" % (eng, (c - 1) // EPOCH)
        val = (c - 1) % EPOCH + 1
        semh = self.sem(key)
        wl = [(self.sem(k), v) for k, v in waits]

        def emit(e, calls=calls, wl=wl, semh=semh):
            for s, v in wl:
                e.wait_ge(s, v)
            ins = None
            for name, a, kw in calls:
                if name == "matmul":
                    kw = dict(kw)
                    kw["skip_group_check"] = True
                ins = getattr(e, name)(*a, **kw)
            ins.then_inc(semh, 1)

        self.ops[eng].append(emit)
        ev = (key, val)
        self._commit(ev, reads, writes)
        return ev

    def dma(self, q, out, in_, reads=(), writes=(), key=None):
        if self.capture is not None:
            self.capture.append(("dma", q, out, in_, list(reads), list(writes), key))
            return None
        return self._dma_commit(q, out, in_, reads, writes, key)

    def commit(self, item):
        if item[0] == "op":
            return self._op_commit(*item[1:])
        return self._dma_commit(*item[1:])

    def _dma_commit(self, q, out, in_, reads=(), writes=(), key=None):
        waits = self._deps(q, reads, writes)
        dk = "dma:" + key
        semh = self.sem(dk)
        self.dcnt[dk] = self.dcnt.get(dk, 0) + 16
        wl = [(self.sem(k), v) for k, v in waits]

        def emit(e, wl=wl, semh=semh, out=out, in_=in_):
            for s, v in wl:
                e.wait_ge(s, v)
            e.dma_start(out=out, in_=in_).then_inc(semh, 16)

        self.ops[q].append(emit)
        ev = (dk, self.dcnt[dk])
        self._commit(ev, reads, writes)
        return ev

    def barrier(self):
        for eng in self.ENGS:
            wl = []
            for k, v in self.latest.items():
                if self.seen[eng].get(k, 0) >= v:
                    continue
                self.seen[eng][k] = v
                wl.append((self.sem(k), v))

            def emit(e, wl=wl):
                for s, v in wl:
                    e.wait_ge(s, v)

            self.ops[eng].append(emit)

    def emit_all(self):
        nc = self.nc
        ops = self.ops
        with nc.Block() as block:
            @block.tensor
            def _(e):
                for f in ops["pe"]:
                    f(e)

            @block.scalar
            def _(e):
                for f in ops["act"]:
                    f(e)

            @block.vector
            def _(e):
                for f in ops["dve"]:
                    f(e)

            @block.gpsimd
            def _(e):
                for f in ops["pool"]:
                    f(e)

            @block.sync
            def _(e):
                for f in ops["sp"]:
                    f(e)
        self.ops = {e: [] for e in self.ENGS}


OFF = {}
_o = 0
for _n, _w in (('q_a', 256), ('k_a', 256), ('v_a', 256), ('q_b', 256), ('k_b', 64), ('v_b', 64),
               ('q_idx', 256), ('k_idx', 64), ('w_idx', 4), ('q_c', 256), ('k_cmp', 64), ('v_cmp', 64),
               ('k_sel', 64), ('v_sel', 64), ('k_win', 64), ('v_win', 64), ('g_c', 12), ('g_merge', 3072)):
    OFF[_n] = _o
    _o += _w

FM_PLAIN = [('q_a', h) for h in range(4)] + [('k_a', h) for h in range(4)] + [('v_cmp', 0)]
FM_ROPE = ([('q_b', h) for h in range(4)] + [('k_b', 0)] + [('q_idx', h) for h in range(4)] + [('k_idx', 0)] +
           [('q_c', h) for h in range(4)] + [('k_cmp', 0), ('k_sel', 0), ('k_win', 0)])


def _col_index():
    cols = []
    for n, h in FM_PLAIN:
        c0 = OFF[n] + 64 * h
        cols += list(range(c0, c0 + 64))
    for n, h in FM_ROPE:
        c0 = OFF[n] + 64 * h
        cols += list(range(c0, c0 + 64))
        cols += [c0 + (d + 32) % 64 for d in range(64)]
    cols += [OFF['g_c'] + h * 3 + br for br in range(3) for h in range(4)]
    cols += list(range(OFF['g_merge'], OFF['g_merge'] + 3072))
    cols += list(range(OFF['v_a'], OFF['v_a'] + 256))
    cols += list(range(OFF['v_b'], OFF['v_b'] + 64))
    cols += list(range(OFF['v_sel'], OFF['v_sel'] + 64))
    cols += list(range(OFF['v_win'], OFF['v_win'] + 64))
    cols += list(range(OFF['w_idx'], OFF['w_idx'] + 4))
    assert len(cols) == NA
    return np.asarray(cols)


def make_consts(S):
    NB = S // 128
    c = {}
    c['c_ident4'] = np.tile(np.eye(128, dtype=np.float32), (1, 4))
    t = np.arange(128)[:, None]
    s = np.arange(128)[None, :]
    m = np.zeros((128, 5, 128), np.float32)
    m[:, 0, :] = np.where(s >= t, NEG, 0.0)
    m[:, 1, :] = np.where(s > t, NEG, 0.0)
    m[:, 2, :] = np.where(s <= t, NEG, 0.0)
    j = np.arange(128)[:, None]
    m[:, 3, :] = np.where(j >= s, -1.0, 0.0)
    m[:, 4, :] = -1.0
    c['c_masks'] = m
    c['c_tokcausal'] = np.where(s > t, -BIGF, 0.0).astype(np.float32)
    inv = 10000.0 ** (-np.arange(0, 64, 2, dtype=np.float32) / 64)
    ang = np.arange(S, dtype=np.float32)[None, :] * inv[:, None]
    cos = np.cos(ang).astype(np.float32)
    sin = np.sin(ang).astype(np.float32)
    c['c_rope'] = np.stack([np.concatenate([cos, cos], 0), np.concatenate([-sin, sin], 0)], 0)
    n_sel = S // 64
    ex = np.zeros((64, S), np.float32)
    for jj in range(min(n_sel, 64)):
        ex[jj, jj * 64:(jj + 1) * 64] = 1.0
    c['c_expand'] = ex
    n_cmp = (S - 32) // 16 + 1
    ncp = ((n_cmp + 1 + 127) // 128) * 128
    cs = np.arange(n_cmp) * 16
    ss = np.arange(n_sel) * 64
    ov = np.zeros((ncp, 64), np.float32)
    ov[:n_cmp, :n_sel] = ((cs[:, None] < ss[None, :] + 64) & (cs[:, None] + 32 > ss[None, :])).astype(np.float32)
    c['c_ov'] = ov
    tg = np.arange(S)
    n = np.arange(ncp)
    adm = (n[None, :] * 16 + 31 <= tg[:, None]) & (n[None, :] < n_cmp)
    c['c_cmpmask'] = np.where(adm, 0.0, NEG).astype(np.float32).reshape(NB, 128, ncp)
    jj = np.arange(64)
    cur = tg // 64
    forced0 = (jj[None, :] == 0)
    forced1 = (jj[None, :] == cur[:, None])
    forced2 = (jj[None, :] == cur[:, None] - 1)
    forced = forced0 | forced1 | forced2
    admis = (jj[None, :] * 64 <= tg[:, None]) & (jj[None, :] < n_sel)
    mul = np.where(forced | ~admis, 0.0, 1.0).astype(np.float32)
    add = np.zeros((S, 64), np.float32)
    add = np.where(forced0, 10000.0, add)
    add = np.where(forced2, 10001.0, add)
    add = np.where(forced1, 10002.0, add)
    add = np.where(admis, add, -BIGF).astype(np.float32)
    c['c_selmul'] = mul.reshape(NB, 128, 64)
    c['c_seladd'] = add.reshape(NB, 128, 64)
    return c


def build(S, layers, debug=False):
    NB = S // 128
    NG = S // 512
    L = len(layers)
    n_sel = S // 64
    topn = min(16, n_sel)
    topk = min(TOPK, S // 4)
    n_cmp = (S - 32) // 16 + 1
    NCP = ((n_cmp + 1 + 127) // 128) * 128
    NCC = NCP // 128
    nc = bass.Bass("TRN2", target_bir_lowering=False)

    def din(name, shape, dt=F32):
        return nc.dram_tensor(name, list(shape), dt, kind="ExternalInput")

    dbg_kind = "ExternalOutput" if debug else None

    def dscr(name, shape, dt):
        if debug:
            return nc.dram_tensor(name, list(shape), dt, kind="ExternalOutput")
        return nc.dram_tensor(name, list(shape), dt)

    x_d = din("x", [S, D])
    mem_d = din("mem", [NMEM, D])
    wA_d = din("wA", [L, D, NA])
    w1k_d = din("cmp_w1_k", [L, 2048, 128]); w2k_d = din("cmp_w2_k", [L, 128, 64]); posk_d = din("cmp_pos_k", [L, 32, 64])
    w1v_d = din("cmp_w1_v", [L, 2048, 128]); w2v_d = din("cmp_w2_v", [L, 128, 64]); posv_d = din("cmp_pos_v", [L, 32, 64])
    wup_d = din("w_up", [L, 3, 256, D]); wout_d = din("w_out", [L, D, D])
    wmq_d = din("w_mem_q", [L, D, 256]); wmkv_d = din("w_mem_kv", [L, D, 512]); wmo_d = din("w_mem_o", [L, 256, D])
    wfi_d = din("w_ffn_in", [L, D, 2 * DFF]); wfo_d = din("w_ffn_out", [L, DFF, D])
    norms_d = din("norms", [4 * L + 1, D])
    c_ident4 = din("c_ident4", [128, 512]); c_masks = din("c_masks", [128, 5, 128]); c_tokc = din("c_tokcausal", [128, 128])
    c_rope = din("c_rope", [2, 64, S]); c_expand = din("c_expand", [64, S]); c_ov = din("c_ov", [NCP, 64])
    c_cmpmask = din("c_cmpmask", [NB, 128, NCP]); c_selmul = din("c_selmul", [NB, 128, 64]); c_seladd = din("c_seladd", [NB, 128, 64])
    out_d = nc.dram_tensor("out", [S, D], F32, kind="ExternalOutput")

    QTA = dscr("QTA", [4, 64, S], BF16); KTA = dscr("KTA", [4, 64, S], BF16)
    QTB = dscr("QTB", [4, 64, S], BF16); KTB = dscr("KTB", [64, S], BF16)
    QTI = dscr("QTI", [4, 64, S], BF16); KTI = dscr("KTI", [64, S], BF16)
    QTC = dscr("QTC", [4, 64, S], BF16)
    KTCMP = dscr("KTCMP", [64, S], BF16); VTCMP = dscr("VTCMP", [64, S], BF16)
    KTSEL = dscr("KTSEL", [64, S], BF16); KTWIN = dscr("KTWIN", [64, S], BF16)
    VA = dscr("VA", [S, 256], BF16); V3 = dscr("V3", [S, 3, 64], BF16)
    WI = dscr("WI", [S, 4], F32); GC = dscr("GC", [12, S], F32); GT = dscr("GT", [3072, S], BF16)
    OT = dscr("OT", [12, 64, S], BF16)
    HM = dscr("HM", [S, D], F32); HO = dscr("HO", [S, D], F32)
    fm_dst = {}
    for n, h in FM_PLAIN + FM_ROPE:
        fm_dst[(n, h)] = {'q_a': QTA, 'k_a': KTA, 'q_b': QTB, 'q_idx': QTI, 'q_c': QTC}.get(n)
    single = {'v_cmp': VTCMP, 'k_b': KTB, 'k_idx': KTI, 'k_cmp': KTCMP, 'k_sel': KTSEL, 'k_win': KTWIN}

    def bcrow(handle, row, n=D, parts=128):
        return bass.AP(handle, row * n, [[0, parts], [1, n]])

    with ExitStack() as es:
        P = Prog(nc, es)
        sbg = lambda n, s, d: es.enter_context(nc.sbuf_tensor(n, list(s), d))
        psg = lambda n, s, d: es.enter_context(nc.psum_tensor(n, list(s), d))
        pb = [None] * 8
        ptrh = [None]

        def alloc_psum(stk, tag, nfp):
            for i_ in range(nfp):
                pb[i_] = stk.enter_context(nc.psum_tensor("pb%d_%s" % (i_, tag), [128, 512], F32))
            if nfp < 8:
                ptrh[0] = stk.enter_context(nc.psum_tensor("ptr_%s" % tag, [128, 1024], BF16))
        ident = sbg("ident", [128, 512], BF16)
        masks = sbg("masks", [128, 5, 128], BF16)
        ones32 = sbg("ones32", [128, 64], F32)
        stat = sbg("stat", [128, 16], F32)
        P.dma("pool", ident[:], c_ident4.ap(), writes=["ident"], key="c0")
        P.dma("pool", masks[:], c_masks.ap(), writes=["masks"], key="c1")
        P.op("pool", lambda e: e.memset(ones32[:], 1.0), writes=["ones32"])

        def rmsnorm_T(src, src_res, g_bc, g_res, dstT, dst_res, tmp):
            junk, ubf = tmp
            P.op("act", lambda e: e.activation(out=junk[:], in_=src, func=AF.Square, accum_out=stat[:, 0:1]),
                 reads=[src_res], writes=["junk", "stat0"])
            P.op("act", lambda e: e.activation(out=stat[:, 1:2], in_=stat[:, 0:1], func=AF.Sqrt, scale=1.0 / D, bias=1e-6),
                 reads=["stat0"], writes=["stat1"])
            P.op("dve", lambda e: e.reciprocal(out=stat[:, 2:3], in_=stat[:, 1:2]), reads=["stat1"], writes=["stat2"])
            P.op("dve", lambda e: e.scalar_tensor_tensor(out=ubf[:], in0=src, scalar=stat[:, 2:3], in1=g_bc[:],
                                                          op0=ALU.mult, op1=ALU.mult),
                 reads=[src_res, "stat2", g_res], writes=["ubf"])
            P.op("pe", [(lambda e, kc=kc: e.transpose(out=ptrh[0][:, kc * 128:(kc + 1) * 128], in_=ubf[:, kc * 128:(kc + 1) * 128],
                                                      identity=ident[:, 0:128])) for kc in range(8)],
                 reads=["ubf", "ident"], writes=["ptr"])
            P.op("act", lambda e: e.activation(out=dstT, in_=ptrh[0][:].rearrange("p (k t) -> p k t", k=8), func=AF.Copy),
                 reads=["ptr"], writes=[dst_res])

        def attn_finish(obank, obres, dst, dst_res, work, gate=None, guard=False, extra_reads=(), bcbank=6):
            rrow, bcs, accum = work
            P.op("act", lambda e: e.activation(out=rrow[64:65, :], in_=obank[64:65, :], func=AF.Ln), reads=[obres], writes=["rrow"])
            P.op("act", lambda e: e.activation(out=rrow[64:65, :], in_=rrow[64:65, :], func=AF.Exp, scale=-1.0), reads=["rrow"], writes=["rrow"])
            if gate is not None:
                gap, gres = gate
                P.op("dve", lambda e: e.tensor_tensor(out=rrow[64:65, :], in0=rrow[64:65, :], in1=gap, op=ALU.mult),
                     reads=["rrow", gres], writes=["rrow"])
            P.op("pe", lambda e: e.matmul(pb[bcbank][0:64, :], lhsT=ones32[64:65, 0:64], rhs=rrow[64:65, :], start=True, stop=True),
                 reads=["rrow", "ones32"], writes=["pb%d" % bcbank])
            P.op("act", lambda e: e.activation(out=bcs[0:64, :], in_=pb[bcbank][0:64, :], func=AF.Copy), reads=["pb%d" % bcbank], writes=["bcs"])
            if accum is None:
                P.op("dve", lambda e: e.tensor_tensor(out=dst, in0=obank[0:64, :], in1=bcs[0:64, :], op=ALU.mult),
                     reads=[obres, "bcs"] + list(extra_reads), writes=[dst_res])
            else:
                acc, first = accum
                if first:
                    P.op("dve", lambda e: e.tensor_tensor(out=acc[0:64, :], in0=obank[0:64, :], in1=bcs[0:64, :], op=ALU.mult),
                         reads=[obres, "bcs"], writes=["oacc"])
                else:
                    P.op("dve", lambda e: e.tensor_tensor(out=bcs[0:64, :], in0=obank[0:64, :], in1=bcs[0:64, :], op=ALU.mult),
                         reads=[obres, "bcs"], writes=["bcs"])
                    P.op("pool", lambda e: e.tensor_tensor(out=acc[0:64, :], in0=acc[0:64, :], in1=bcs[0:64, :], op=ALU.add),
                         reads=["bcs", "oacc"], writes=["oacc"])

        def merge_run0(thunks):
            items = []
            for ci, f in enumerate(thunks):
                P.capture = []
                f()
                ops_ = P.capture
                P.capture = None
                n_ = len(ops_)
                for k, it_ in enumerate(ops_):
                    items.append(((k + 0.5) / n_, ci, k, it_))
            items.sort(key=lambda t: (t[0], t[1], t[2]))
            for _, _, _, it_ in items:
                P.commit(it_)

        for li, l in enumerate(layers):
            h_in = x_d if li == 0 else HO
            last = (li == L - 1)
            with ExitStack() as ea:
                sb = lambda n, s, d: ea.enter_context(nc.sbuf_tensor(n + "_L%d" % li, list(s), d))
                alloc_psum(ea, "A%d" % li, 7)
                wA = sb("wA_sb", [128, 8, NA], BF16)
                gA = sb("gA", [128, D], F32)
                rope = sb("rope", [64, 2, S], F32)
                hb = [sb("hbA%d" % i, [128, D], F32) for i in range(2)]
                junk = sb("junkA", [128, D], F32)
                ubf = sb("ubfA", [128, D], BF16)
                uT2 = [sb("uTA%d" % i_, [128, 8, 512], BF16) for i_ in range(2)]
                cur = {}
                stg = [sb("stgA%d" % i, [128, 512], BF16) for i in range(4)]
                stgf = sb("stgfA", [128, 512], F32)
                t1 = sb("ropeT1", [64, 512], F32)
                t2 = sb("ropeT2", [64, 512], F32)
                for kc in range(8):
                    P.dma("pool", wA[:, kc, :], wA_d.ap()[l, kc * 128:(kc + 1) * 128, :], writes=["wA%d" % kc], key="wA%d" % kc)
                wA_res = ["wA%d" % kc for kc in range(8)]
                P.dma("sp", gA[:], bcrow(norms_d, 4 * l + 0), writes=["gA"], key="gA")
                P.dma("sp", rope[:], c_rope.ap().rearrange("c d s -> d c s"), writes=["rope"], key="rope")
                sctr = [0]

                def stage():
                    i = sctr[0] % 4
                    sctr[0] += 1
                    return stg[i], "stgA%d" % i

                def proj_fm(bank, c0, M):
                    P.op("pe", [(lambda e, kc=kc: e.matmul(pb[bank][0:M, :], lhsT=wA[:, kc, c0:c0 + M], rhs=cur["uT"][:, kc, :],
                                                            start=(kc == 0), stop=(kc == 7))) for kc in range(8)],
                         reads=[cur["r"]] + wA_res, writes=["pb%d" % bank])

                LIM = int(os.environ.get("LIMA", "99"))
                def normA(g):
                    uTg = uT2[g % 2]
                    for b4 in range(4):
                        blk = g * 4 + b4
                        hbt = hb[blk % 2]
                        P.dma("sp", hbt[:], h_in.ap()[blk * 128:(blk + 1) * 128, :], writes=["hb%d" % (blk % 2)], key="hb%d" % (blk % 2))
                        rmsnorm_T(hbt[:], "hb%d" % (blk % 2), gA, "gA", uTg[:, :, b4 * 128:(b4 + 1) * 128], "uT%d" % (g % 2), (junk, ubf))

                def projA(g):
                    gs = slice(g * 512, (g + 1) * 512)
                    uT = uT2[g % 2]
                    uTr = "uT%d" % (g % 2)
                    cur["uT"] = uT
                    cur["r"] = uTr
                    col = 0
                    bank = 0
                    if LIM <= 1:
                        return
                    for (n, h) in FM_PLAIN:
                        proj_fm(bank, col, 64)
                        st, sr = stage()
                        sc = 0.125 if n == 'q_a' else 1.0
                        P.op("act", lambda e, bank=bank, st=st, sc=sc: e.activation(out=st[0:64, :], in_=pb[bank][0:64, :], func=AF.Copy, scale=sc),
                             reads=["pb%d" % bank], writes=[sr])
                        dst = fm_dst[(n, h)].ap()[h, :, gs] if fm_dst[(n, h)] is not None else single[n].ap()[:, gs]
                        P.dma("sp", dst, st[0:64, :], reads=[sr], writes=[], key=sr)
                        col += 64
                        bank = (bank + 1) % 6
                    if LIM <= 2:
                        return
                    for (n, h) in FM_ROPE:
                        b0 = bank
                        b1 = (bank + 1) % 6
                        proj_fm(b0, col, 64)
                        proj_fm(b1, col + 64, 64)
                        st, sr = stage()
                        P.op("dve", lambda e, b0=b0: e.tensor_tensor(out=t1[:], in0=pb[b0][0:64, :], in1=rope[:, 0, gs], op=ALU.mult),
                             reads=["pb%d" % b0, "rope"], writes=["t1"])
                        P.op("dve", lambda e, b1=b1: e.tensor_tensor(out=t2[:], in0=pb[b1][0:64, :], in1=rope[:, 1, gs], op=ALU.mult),
                             reads=["pb%d" % b1, "rope"], writes=["t2"])
                        P.op("pool", lambda e, st=st: e.tensor_tensor(out=st[0:64, :], in0=t1[:], in1=t2[:], op=ALU.add),
                             reads=["t1", "t2"], writes=[sr])
                        dst = fm_dst[(n, h)].ap()[h, :, gs] if fm_dst[(n, h)] is not None else single[n].ap()[:, gs]
                        P.dma("sp", dst, st[0:64, :], reads=[sr], writes=[], key=sr)
                        col += 128
                        bank = (bank + 2) % 6
                    if LIM <= 3:
                        return
                    proj_fm(bank, col, 12)
                    P.op("act", lambda e, bank=bank: e.activation(out=stgf[0:12, :], in_=pb[bank][0:12, :], func=AF.Sigmoid),
                         reads=["pb%d" % bank], writes=["stgf"])
                    P.dma("sp", GC.ap()[:, gs], stgf[0:12, :], reads=["stgf"], writes=[], key="stgf")
                    col += 12
                    bank = (bank + 1) % 6
                    for gg in range(24):
                        proj_fm(bank, col, 128)
                        st, sr = stage()
                        P.op("act", lambda e, bank=bank, st=st: e.activation(out=st[:], in_=pb[bank][:], func=AF.Sigmoid),
                             reads=["pb%d" % bank], writes=[sr])
                        P.dma("sp", GT.ap()[gg * 128:(gg + 1) * 128, gs], st[:], reads=[sr], writes=[], key=sr)
                        col += 128
                        bank = (bank + 1) % 6
                    assert col == NA_FM
                    if LIM <= 4:
                        return
                    for b4 in range(4):
                        blk = g * 4 + b4
                        P.op("pe", [(lambda e, kc=kc, b4=b4, bank=bank: e.matmul(pb[bank][:, 0:452], lhsT=uT[:, kc, b4 * 128:(b4 + 1) * 128],
                                                                                  rhs=wA[:, kc, NA_FM:NA], start=(kc == 0), stop=(kc == 7)))
                                    for kc in range(8)], reads=[uTr] + wA_res, writes=["pb%d" % bank])
                        st, sr = stage()
                        P.op("act", lambda e, bank=bank, st=st: e.activation(out=st[:, 0:448], in_=pb[bank][:, 0:448], func=AF.Copy),
                             reads=["pb%d" % bank], writes=[sr])
                        P.op("dve", lambda e, bank=bank: e.tensor_scalar(out=stgf[:, 0:4], in0=pb[bank][:, 448:452], scalar1=1.0 / 16, scalar2=None, op0=ALU.mult),
                             reads=["pb%d" % bank], writes=["stgf"])
                        P.dma("sp", VA.ap()[blk * 128:(blk + 1) * 128, :], st[:, 0:256], reads=[sr], writes=[], key=sr)
                        P.dma("sp", V3.ap()[blk * 128:(blk + 1) * 128, :, :], st[:, 256:448].rearrange("p (a d) -> p a d", a=3), reads=[sr], writes=[], key=sr)
                        P.dma("sp", WI.ap()[blk * 128:(blk + 1) * 128, :], stgf[:, 0:4], reads=["stgf"], writes=[], key="stgf")
                        bank = (bank + 1) % 6
                if LIM > 0:
                    normA(0)
                for g in range(NG if LIM > 0 else 0):
                    th = [lambda: projA(g)]
                    if g + 1 < NG:
                        th.append(lambda: normA(g + 1))
                    merge_run0(th)
                P.barrier()
                P.emit_all()
            if debug == "A":
                break
            with ExitStack() as eb:
                sb = lambda n, s, d: eb.enter_context(nc.sbuf_tensor(n + "_L%d" % li, list(s), d))
                alloc_psum(eb, "B%d" % li, 8)
                KTAs = sb("KTAs", [64, 4, S], BF16); VAs = sb("VAs", [128, NB, 256], BF16)
                KTBs = sb("KTBs", [64, S], BF16); KTIs = sb("KTIs", [64, S], BF16)
                KTSs = sb("KTSs", [128, S], BF16); KTWs = sb("KTWs", [64, S], BF16)
                V3s = [sb("V3s%d" % j, [128, NB, 65], BF16) for j in range(3)]
                KCT = sb("KCT", [64, NCP], BF16); VCs = sb("VCs", [128, NCC, 65], BF16); OVs = sb("OVs", [128, NCC, 64], BF16)
                tokc = sb("tokc", [128, 128], F32)
                score = sb("score", [128, S], F32); junkb = sb("junkb", [128, S], BF16); mneg = [sb("mneg%d" % i_, [128, S], BF16) for i_ in range(2)]
                QA = [sb("QA%d" % i, [64, 512], BF16) for i in range(2)]
                QB = [sb("QB%d" % i, [64, 512], BF16) for i in range(2)]
                QI = [sb("QI%d" % i, [64, 512], BF16) for i in range(2)]
                QC = [sb("QC%d" % i, [128, 512], BF16) for i in range(2)]
                wi = [sb("wi%d" % i, [128, 4], F32) for i in range(2)]
                grow = [sb("grow%d" % i_, [128, 3, 512], F32) for i_ in range(2)]
                cmk = [sb("cmk%d" % i, [128, NCP], BF16) for i in range(2)]
                smul = [sb("smul%d" % i, [128, 64], F32) for i in range(2)]
                sadd = [sb("sadd%d" % i, [128, 64], F32) for i in range(2)]
                e1 = [sb("e1_%d" % i, [128, 512], F32) for i in range(2)]
                sp_ = [sb("sp_%d" % i, [128, 512], BF16) for i in range(2)]
                asb = [sb("asb%d" % i, [128, 512], F32) for i in range(2)]
                Wt = [sb("Wt%d" % i, [128, 512], BF16) for i in range(2)]
                chi = sb("chi", [2, 512], BF16); crow2 = sb("crow2", [2, 512], BF16); negm = sb("negm", [2, 1], F32); onesb = sb("onesb", [2, 128], BF16)
                P.op("pool", lambda e: e.memset(negm[0:2, :], -1.0), writes=["negm"])
                P.op("pool", lambda e: e.memset(negm[0:1, :], 0.0), writes=["negm"])
                P.op("pool", lambda e: e.memset(onesb[:], 1.0), writes=["onesb"])
                Eb = [sb("Eb%d" % i, [128, 512], BF16) for i in range(3)]
                tmpi = [sb("tmpi%d" % i_, [128, 512], F32) for i_ in range(2)]
                rrow = sb("rrow", [128, 512], F32); bcs = sb("bcs", [64, 512], F32); oacc = sb("oacc", [64, 512], F32)
                ost = [sb("ost%d" % i, [64, 3, 512], BF16) for i in range(2)]
                bis = sb("bis", [128, 8], F32)
                impT = sb("impT", [64, 128], F32); uimp = sb("uimp", [64, 512], F32)
                imp = sb("imp", [128, 64], F32); imp2 = sb("imp2", [128, 64], F32); m8 = sb("m8", [128, 16], F32)
                msel = sb("msel", [128, 128], F32)
                identf = sb("identf", [128, 128], F32)
                w2 = sb("w2", [128, 64], BF16); posT = sb("posT", [64, 32], BF16)
                w1 = (mneg[0][0:64, 0:4096] if S >= 4096 else sb("w1x", [64, 4096], BF16)[:, :]).rearrange("d (a h) -> d a h", a=32)
                TCs = junkb[0:64, :]
                hid = sb("hid", [128, NCP], BF16); bcol = sb("bcol", [128, 1], F32)

                P.dma("sp", KTAs[:], KTA.ap().rearrange("h d s -> d h s"), reads=["scrA"], writes=["KTAs"], key="ldB0")
                P.dma("sp", VAs[:], VA.ap().rearrange("(b p) c -> p b c", p=128), reads=["scrA"], writes=["VAs"], key="ldB1")
                P.dma("sp", KTBs[:], KTB.ap(), reads=["scrA"], writes=["KTBs"], key="ldB2")
                P.dma("sp", KTIs[:], KTI.ap(), reads=["scrA"], writes=["KTIs"], key="ldB3")
                P.dma("sp", KTSs[0:64, :], KTSEL.ap(), reads=["scrA"], writes=["KTSs"], key="ldB4")
                P.op("pool", lambda e: e.memset(msel[:], 0.0), writes=["msel"])
                P.dma("sp", KTWs[:], KTWIN.ap(), reads=["scrA"], writes=["KTWs"], key="ldB5")
                for j in range(3):
                    P.op("pool", lambda e, j=j: e.memset(V3s[j][:], 1.0), writes=["V3s%d" % j])
                    P.dma("sp", V3s[j][:, :, 0:64], V3.ap()[:, j, :].rearrange("(b p) d -> p b d", p=128), reads=["scrA"], writes=["V3s%d" % j], key="ldB6%d" % j)
                P.dma("pool", KTSs[64:128, :], c_expand.ap(), writes=["KTSx"], key="ldB7")
                P.dma("pool", OVs[:], c_ov.ap().rearrange("(c p) j -> p c j", p=128), writes=["OVs"], key="ldB8")
                P.dma("sp", tokc[:], c_tokc.ap(), writes=["tokc"], key="ldB9")
                P.dma("sp", identf[:], c_ident4.ap()[:, 0:128], writes=["identf"], key="ldB10")
                P.op("pool", lambda e: e.memset(VCs[:], 1.0), writes=["VCs"])
                for which in range(2 if int(os.environ.get("LIMB", "99")) > 0 else 0):
                    w1_d, w2_d, pos_d, src = ((w1k_d, w2k_d, posk_d, KTCMP), (w1v_d, w2v_d, posv_d, VTCMP))[which]
                    P.dma("pool", w1, w1_d.ap()[l].rearrange("(a d) h -> d a h", d=64), writes=["mneg0"], key="cw1")
                    P.dma("pool", w2[:], w2_d.ap()[l], writes=["w2"], key="cw2")
                    for a in range(32):
                        P.dma("pool", posT[:, a:a + 1], pos_d.ap()[l, a:a + 1, :].rearrange("a d -> d a"), writes=["posT"], key="cpos")
                    P.dma("sp", TCs, src.ap(), reads=["scrA"], writes=["junkb"], key="cTC")
                    P.op("pe", [(lambda e, a=a: e.matmul(pb[1][:, 0:1], lhsT=w1[:, a, :], rhs=posT[:, a:a + 1], start=(a == 0), stop=(a == 31)))
                                for a in range(32)], reads=["mneg0", "posT"], writes=["pb1"])
                    P.op("act", lambda e: e.activation(out=bcol[:], in_=pb[1][:, 0:1], func=AF.Copy), reads=["pb1"], writes=["bcol"])
                    P.op("pe", [(lambda e, a=a: e.matmul(pb[0][:, 0:n_cmp], lhsT=w1[:, a, :], rhs=TCs[:, a:a + 16 * (n_cmp - 1) + 1:16],
                                                         start=(a == 0), stop=(a == 31))) for a in range(32)],
                         reads=["mneg0", "junkb"], writes=["pb0"])
                    P.op("pool", lambda e: e.memset(hid[:], 0.0), writes=["hid"])
                    P.op("act", lambda e: e.activation(out=hid[:, 0:n_cmp], in_=pb[0][:, 0:n_cmp], func=AF.Gelu_apprx_tanh, bias=bcol[:, 0:1]),
                         reads=["pb0", "bcol"], writes=["hid"])
                    if which == 0:
                        P.op("pe", lambda e: e.matmul(pb[2][0:64, 0:NCP], lhsT=w2[:], rhs=hid[:], start=True, stop=True),
                             reads=["w2", "hid"], writes=["pb2"])
                        P.op("act", lambda e: e.activation(out=KCT[:], in_=pb[2][0:64, 0:NCP], func=AF.Copy), reads=["pb2"], writes=["KCT"])
                    else:
                        for c in range(NCC):
                            P.op("pe", lambda e, c=c: e.matmul(pb[2][:, 0:64], lhsT=hid[:, c * 128:(c + 1) * 128], rhs=w2[:], start=True, stop=True),
                                 reads=["w2", "hid"], writes=["pb2"])
                            P.op("act", lambda e, c=c: e.activation(out=VCs[:, c, 0:64], in_=pb[2][:, 0:64], func=AF.Copy), reads=["pb2"], writes=["VCs"])

                ectr = [0]
                tctr = [0]
                LIMB = int(os.environ.get("LIMB", "99"))

                def load_block(i):
                    pi = i % 2
                    ts = slice(i * 128, (i + 1) * 128)
                    for (qt, qn, src) in ((QI, "QI", QTI), (QA, "QA", QTA), (QB, "QB", QTB), (QC, "QC", QTC)):
                        P.dma("sp", qt[pi][0:64, :].rearrange("d (h t) -> d h t", h=4), src.ap()[:, :, ts].rearrange("h d t -> d h t"),
                              reads=[], writes=["%s%d" % (qn, pi)], key="%s%d" % (qn, pi))
                    P.dma("sp", wi[pi][:], WI.ap()[ts, :], writes=["wi%d" % pi], key="wi%d" % pi)
                    P.dma("sp", grow[pi][64:65, :, :].rearrange("o b (h t) -> o b h t", h=4),
                          GC.ap()[:, ts].rearrange("(o b h) t -> o b h t", o=1, b=3), writes=["grow%d" % pi], key="grow%d" % pi)
                    P.dma("pool", cmk[pi][:], c_cmpmask.ap()[i], writes=["cmk%d" % pi], key="cmk%d" % pi)
                    P.dma("sp", smul[pi][:], c_selmul.ap()[i], writes=["smul%d" % pi], key="smul%d" % pi)
                    P.dma("sp", sadd[pi][:], c_seladd.ap()[i], writes=["sadd%d" % pi], key="sadd%d" % pi)

                def chain_P(i):
                    U = []
                    pi = i % 2
                    Lk = (i + 1) * 128
                    qi = QI[pi]
                    qir = "QI%d" % pi
                    mn = mneg[pi]
                    mres = "mneg%d" % pi
                    wres = "wi%d" % pi
                    nch = (Lk + 511) // 512
                    for c in range(nch):
                        w_ = min(512, Lk - c * 512)
                        cs = slice(c * 512, c * 512 + w_)
                        for h in range(4):
                            def u(c=c, h=h, w_=w_, cs=cs):
                                P.op("pe", lambda e: e.matmul(pb[7][:, 0:w_], lhsT=qi[:, h * 128:(h + 1) * 128], rhs=KTIs[:, cs], start=True, stop=True),
                                     reads=[qir, "KTIs"], writes=["pb7"])
                                if h == 0:
                                    P.op("dve", lambda e: e.tensor_scalar(out=score[:, cs], in0=pb[7][:, 0:w_], scalar1=0.0, scalar2=wi[pi][:, 0:1], op0=ALU.max, op1=ALU.mult),
                                         reads=["pb7", wres], writes=["score"])
                                else:
                                    tb = tctr[0] % 2
                                    tctr[0] += 1
                                    P.op("dve", lambda e: e.tensor_scalar(out=tmpi[tb][:, 0:w_], in0=pb[7][:, 0:w_], scalar1=0.0, scalar2=wi[pi][:, h:h + 1], op0=ALU.max, op1=ALU.mult),
                                         reads=["pb7", wres], writes=["tmpi%d" % tb])
                                    P.op("pool", lambda e: e.tensor_tensor(out=score[:, cs], in0=score[:, cs], in1=tmpi[tb][:, 0:w_], op=ALU.add),
                                         reads=["tmpi%d" % tb, "score"], writes=["score"])
                            U.append(u)

                    def u_pre():
                        if Lk > topk:
                            P.op("dve", lambda e: e.tensor_reduce(out=bis[:, 0:1], in_=score[:, 0:Lk], axis=AX.X, op=ALU.min), reads=["score"], writes=["bis"])
                            P.op("dve", lambda e: e.tensor_reduce(out=bis[:, 1:2], in_=score[:, 0:Lk], axis=AX.X, op=ALU.max), reads=["score"], writes=["bis"])
                            P.op("dve", lambda e: e.tensor_tensor(out=bis[:, 1:2], in0=bis[:, 1:2], in1=bis[:, 0:1], op=ALU.subtract), reads=["bis"], writes=["bis"])
                        P.op("pool", lambda e: e.tensor_tensor(out=score[:, i * 128:(i + 1) * 128], in0=score[:, i * 128:(i + 1) * 128], in1=tokc[:], op=ALU.add),
                             reads=["score", "tokc"], writes=["score"])
                    U.append(u_pre)
                    if Lk > topk:
                        def u_it():
                            P.op("dve", lambda e: e.tensor_scalar(out=bis[:, 1:2], in0=bis[:, 1:2], scalar1=0.5, scalar2=None, op0=ALU.mult), reads=["bis"], writes=["bis"])
                            P.op("dve", lambda e: e.tensor_tensor(out=bis[:, 2:3], in0=bis[:, 0:1], in1=bis[:, 1:2], op=ALU.add), reads=["bis"], writes=["bis"])
                            P.op("dve", lambda e: e.tensor_scalar(out=junkb[:, 0:Lk], in0=score[:, 0:Lk], scalar1=bis[:, 2:3], scalar2=None, op0=ALU.is_ge, op1=ALU.add,
                                                                  accum_out=bis[:, 3:4]), reads=["score", "bis"], writes=["junkb", "bis"])
                            P.op("dve", lambda e: e.tensor_scalar(out=bis[:, 4:5], in0=bis[:, 3:4], scalar1=float(topk) - 0.5, scalar2=None, op0=ALU.is_ge), reads=["bis"], writes=["bis"])
                            P.op("dve", lambda e: e.scalar_tensor_tensor(out=bis[:, 0:1], in0=bis[:, 4:5], scalar=bis[:, 1:2], in1=bis[:, 0:1], op0=ALU.mult, op1=ALU.add),
                                 reads=["bis"], writes=["bis"])
                        for it in range(NBIS):
                            U.append(u_it)

                        def u_fin():
                            P.op("dve", lambda e: e.tensor_scalar(out=mn[:, 0:Lk], in0=score[:, 0:Lk], scalar1=bis[:, 0:1], scalar2=NEG, op0=ALU.is_lt, op1=ALU.mult),
                                 reads=["score", "bis"], writes=[mres])
                    else:
                        def u_fin():
                            P.op("dve", lambda e: e.tensor_scalar(out=mn[:, 0:Lk], in0=score[:, 0:Lk], scalar1=-1.0e29, scalar2=NEG, op0=ALU.is_lt, op1=ALU.mult),
                                 reads=["score"], writes=[mres])
                    U.append(u_fin)
                    return U

                def chain_SB(i):
                    U = []
                    pi = i % 2
                    qa = QA[pi]
                    qar = "QA%d" % pi
                    o_st = ost[pi]
                    ostr = "ost%d" % pi
                    kbs = list(range(i, -1, -1))
                    n = len(kbs)

                    def z_ops(kb, bank, start_first):
                        fns = []
                        first = start_first
                        if kb == i:
                            fns.append(lambda e, first=first: e.matmul(pb[bank][:], lhsT=masks[:, 0, :], rhs=ident[:], start=first, stop=False))
                            first = False
                        for h in range(4):
                            fns.append(lambda e, h=h, first=first: e.matmul(
                                pb[bank][:, h * 128:(h + 1) * 128], lhsT=KTAs[:, h, kb * 128:(kb + 1) * 128], rhs=qa[:, h * 128:(h + 1) * 128],
                                start=first, stop=(h == 3)))
                            first = False
                        return fns

                    def stage1(j):
                        kb = kbs[j]
                        bk = j % 2
                        P.op("pe", z_ops(kb, bk, True), reads=["KTAs", qar, "masks", "ident"], writes=["pb%d" % bk])
                        P.op("act", lambda e: e.activation(out=e1[bk][:], in_=pb[bk][:], func=AF.Exp), reads=["pb%d" % bk], writes=["e1_%d" % bk])
                        P.op("act", lambda e: e.activation(out=sp_[bk][:], in_=e1[bk][:], func=AF.Ln, bias=1.0), reads=["e1_%d" % bk], writes=["sp_%d" % bk])

                    def stage2(j):
                        kb = kbs[j]
                        bk = j % 2
                        fns = [lambda e: e.matmul(pb[bk][:], lhsT=masks[:, 3, :], rhs=sp_[bk][:], start=False, stop=(j == 0))]
                        rds = ["sp_%d" % bk, "masks"]
                        if j > 0:
                            P.op("act", lambda e: e.activation(out=chi[0:2, :], in_=pb[2][0:2, :], func=AF.Copy), reads=["pb2"], writes=["chi"])
                            P.op("dve", lambda e: e.scalar_tensor_tensor(out=crow2[0:2, :], in0=chi[0:2, :], scalar=negm[0:2, 0:1], in1=pb[2][0:2, :],
                                                                          op0=ALU.mult, op1=ALU.add), reads=["chi", "negm", "pb2"], writes=["crow2"])
                            fns.append(lambda e: e.matmul(pb[bk][:], lhsT=onesb[0:2, :], rhs=crow2[0:2, :], start=False, stop=True))
                            rds += ["onesb", "crow2"]
                        P.op("pe", fns, reads=rds, writes=["pb%d" % bk])
                        P.op("pe", lambda e: e.matmul(pb[2][:], lhsT=masks[:, 4, :], rhs=sp_[bk][:], start=(j == 0), stop=(j == n - 1)),
                             reads=["sp_%d" % bk, "masks"], writes=["pb2"])
                        P.op("act", lambda e: e.activation(out=Wt[bk][:], in_=pb[bk][:], func=AF.Exp), reads=["pb%d" % bk], writes=["Wt%d" % bk])
                        P.op("pe", [(lambda e, h=h: e.matmul(pb[3][0:64, h * 128:(h + 1) * 128], lhsT=VAs[:, kb, h * 64:(h + 1) * 64],
                                                             rhs=Wt[bk][:, h * 128:(h + 1) * 128], start=(j == 0 and h == 0), stop=(j == n - 1)))
                                    for h in range(4)], reads=["Wt%d" % bk, "VAs"], writes=["pb3"])

                    def u0():
                        stage1(0)
                    U.append(u0)
                    for j in range(n):
                        def u(j=j):
                            if j + 1 < n:
                                stage1(j + 1)
                            stage2(j)
                        U.append(u)

                    def ufin():
                        P.op("act", lambda e: e.activation(out=o_st[:, 0, :], in_=pb[3][0:64, :], func=AF.Copy), reads=["pb3"], writes=[ostr])
                    U.append(ufin)
                    return U

                def attn_units(U, nsteps, s_ops, v_ap, v_res, scale=0.125):
                    def emitS(j):
                        bk = 4 + j % 2
                        fns, rd = s_ops(j, bk)
                        P.op("pe", fns, reads=rd, writes=["pb%d" % bk])

                    def unit(j):
                        if j == 0:
                            emitS(0)
                        if j + 1 < nsteps:
                            emitS(j + 1)
                        bk = 4 + j % 2
                        eb_i = ectr[0] % 3
                        ectr[0] += 1
                        E = Eb[eb_i]
                        P.op("act", lambda e: e.activation(out=E[:], in_=pb[bk][:], func=AF.Exp, scale=scale),
                             reads=["pb%d" % bk], writes=["Eb%d" % eb_i])
                        P.op("pe", lambda e: e.matmul(pb[6][0:65, :], lhsT=v_ap(j), rhs=E[:], start=(j == 0), stop=(j == nsteps - 1)),
                             reads=["Eb%d" % eb_i, v_res], writes=["pb6"])
                    for j in range(nsteps):
                        U.append((lambda j=j: unit(j)))

                def chain_ATT(i):
                    U = []
                    pi = i % 2
                    ts = slice(i * 128, (i + 1) * 128)
                    qb, qc = QB[pi], QC[pi][0:64, :]
                    qbr, qcr = "QB%d" % pi, "QC%d" % pi
                    qaug = QC[pi]
                    msr = "mselT%d" % pi
                    o_st = ost[pi]
                    ostr = "ost%d" % pi
                    mn = mneg[pi]
                    mres = "mneg%d" % pi
                    gres = "grow%d" % pi

                    def dsa_s(j, bk):
                        return ([lambda e: e.matmul(pb[bk][:], lhsT=mn[:, j * 128:(j + 1) * 128], rhs=ident[:], start=True, stop=False),
                                 lambda e: e.matmul(pb[bk][:], lhsT=KTBs[:, j * 128:(j + 1) * 128], rhs=qb[:], start=False, stop=True)],
                                [mres, "ident", "KTBs", qbr])
                    attn_units(U, i + 1, dsa_s, lambda j: V3s[0][:, j, :], "V3s0")
                    U.append(lambda: attn_finish(pb[6], "pb6", o_st[:, 1, :], ostr, (rrow, bcs, None), bcbank=4))

                    def u_cmp():
                        for j in range(NCC):
                            P.op("pe", [lambda e: e.matmul(pb[4][:], lhsT=cmk[pi][:, j * 128:(j + 1) * 128], rhs=ident[:], start=True, stop=False),
                                        lambda e: e.matmul(pb[4][:], lhsT=KCT[:, j * 128:(j + 1) * 128], rhs=qc[:], start=False, stop=True)],
                                 reads=["cmk%d" % pi, "ident", "KCT", qcr], writes=["pb4"])
                            eb_i = ectr[0] % 3
                            ectr[0] += 1
                            E = Eb[eb_i]
                            P.op("act", lambda e: e.activation(out=E[:], in_=pb[4][:], func=AF.Exp, scale=0.125), reads=["pb4"], writes=["Eb%d" % eb_i])
                            P.op("pe", lambda e: e.matmul(pb[6][0:65, :], lhsT=VCs[:, j, :], rhs=E[:], start=(j == 0), stop=(j == NCC - 1)),
                                 reads=["Eb%d" % eb_i, "VCs"], writes=["pb6"])
                            P.op("pe", lambda e: e.matmul(pb[5][0:64, :], lhsT=OVs[:, j, :], rhs=E[:], start=(j == 0), stop=(j == NCC - 1)),
                                 reads=["Eb%d" % eb_i, "OVs"], writes=["pb5"])
                    U.append(u_cmp)

                    def u_imp():
                        P.op("dve", lambda e: e.tensor_scalar(out=rrow[64:65, :], in0=pb[6][64:65, :], scalar1=1e-30, scalar2=None, op0=ALU.max), reads=["pb6"], writes=["rrow"])
                        P.op("act", lambda e: e.activation(out=rrow[64:65, :], in_=rrow[64:65, :], func=AF.Ln), reads=["rrow"], writes=["rrow"])
                        P.op("act", lambda e: e.activation(out=rrow[64:65, :], in_=rrow[64:65, :], func=AF.Exp, scale=-1.0), reads=["rrow"], writes=["rrow"])
                        P.op("pe", lambda e: e.matmul(pb[4][0:64, :], lhsT=ones32[64:65, 0:64], rhs=rrow[64:65, :], start=True, stop=True), reads=["rrow", "ones32"], writes=["pb4"])
                        P.op("act", lambda e: e.activation(out=bcs[0:64, :], in_=pb[4][0:64, :], func=AF.Copy), reads=["pb4"], writes=["bcs"])
                        P.op("dve", lambda e: e.tensor_tensor(out=uimp[:], in0=pb[5][0:64, :], in1=bcs[0:64, :], op=ALU.mult), reads=["pb5", "bcs"], writes=["uimp"])
                        P.op("dve", lambda e: e.tensor_tensor(out=impT[:], in0=uimp[:, 0:128], in1=uimp[:, 128:256], op=ALU.add), reads=["uimp"], writes=["impT"])
                        P.op("dve", lambda e: e.tensor_tensor(out=impT[:], in0=impT[:], in1=uimp[:, 256:384], op=ALU.add), reads=["uimp", "impT"], writes=["impT"])
                        P.op("dve", lambda e: e.tensor_tensor(out=impT[:], in0=impT[:], in1=uimp[:, 384:512], op=ALU.add), reads=["uimp", "impT"], writes=["impT"])
                        P.op("dve", lambda e: e.tensor_tensor(out=rrow[64:65, :], in0=rrow[64:65, :], in1=grow[pi][64:65, 0, :], op=ALU.mult), reads=["rrow", gres], writes=["rrow"])
                        P.op("pe", lambda e: e.matmul(pb[4][0:64, :], lhsT=ones32[64:65, 0:64], rhs=rrow[64:65, :], start=True, stop=True), reads=["rrow", "ones32"], writes=["pb4"])
                        P.op("act", lambda e: e.activation(out=bcs[0:64, :], in_=pb[4][0:64, :], func=AF.Copy), reads=["pb4"], writes=["bcs"])
                        P.op("dve", lambda e: e.tensor_tensor(out=oacc[:], in0=pb[6][0:64, :], in1=bcs[0:64, :], op=ALU.mult), reads=["pb6", "bcs"], writes=["oacc"])
                    U.append(u_imp)

                    def u_top():
                        P.op("pe", lambda e: e.matmul(pb[4][:, 0:64], lhsT=impT[:], rhs=identf[0:64, 0:64], start=True, stop=True), reads=["impT", "identf"], writes=["pb4"])
                        P.op("dve", lambda e: e.tensor_tensor(out=imp[:], in0=pb[4][:, 0:64], in1=smul[pi][:], op=ALU.mult), reads=["pb4", "smul%d" % pi], writes=["imp"])
                        P.op("dve", lambda e: e.tensor_tensor(out=imp[:], in0=imp[:], in1=sadd[pi][:], op=ALU.add), reads=["imp", "sadd%d" % pi], writes=["imp"])
                        P.op("dve", lambda e: e.max(out=m8[:, 0:8], in_=imp[:]), reads=["imp"], writes=["m8"])
                        if topn > 8:
                            P.op("dve", lambda e: e.match_replace(out=imp2[:], in_to_replace=m8[:, 0:8], in_values=imp[:], imm_value=-BIGF), reads=["imp", "m8"], writes=["imp2"])
                            P.op("dve", lambda e: e.max(out=m8[:, 8:16], in_=imp2[:]), reads=["imp2"], writes=["m8"])
                            lo_, hi_ = 8, 8 + (topn - 8)
                        else:
                            lo_, hi_ = 0, topn
                        P.op("dve", lambda e: e.tensor_reduce(out=bis[:, 5:6], in_=m8[:, lo_:hi_], axis=AX.X, op=ALU.min), reads=["m8"], writes=["bis5"])
                        P.op("dve", lambda e: e.tensor_scalar(out=bis[:, 5:6], in0=bis[:, 5:6], scalar1=-1.0e29, scalar2=None, op0=ALU.max), reads=["bis5"], writes=["bis5"])
                        P.op("dve", lambda e: e.tensor_scalar(out=msel[:, 64:128], in0=imp[:], scalar1=bis[:, 5:6], scalar2=NEG, op0=ALU.is_lt, op1=ALU.mult), reads=["imp", "bis5"], writes=["msel"])
                        P.op("pe", lambda e: e.matmul(pb[4][:, 0:128], lhsT=msel[:], rhs=identf[:, :], start=True, stop=True), reads=["msel", "identf"], writes=["pb4"])
                        P.op("act", [(lambda e, h=h: e.activation(out=qaug[64:128, h * 128:(h + 1) * 128], in_=pb[4][64:128, 0:128], func=AF.Copy)) for h in range(4)],
                             reads=["pb4"], writes=[msr])
                    U.append(u_top)

                    def sel_s(j, bk):
                        fns = []
                        if j == i:
                            fns.append(lambda e: e.matmul(pb[bk][:], lhsT=masks[:, 1, :], rhs=ident[:], start=True, stop=False))
                        fns.append(lambda e: e.matmul(pb[bk][:], lhsT=KTSs[:, j * 128:(j + 1) * 128], rhs=qaug[:, :], start=(j != i), stop=True))
                        return fns, ["KTSx", msr, "masks", "ident", "KTSs", qcr]
                    attn_units(U, i + 1, sel_s, lambda j: V3s[1][:, j, :], "V3s1")
                    U.append(lambda: attn_finish(pb[6], "pb6", None, None, (rrow, bcs, (oacc, False)), gate=(grow[pi][64:65, 1, :], gres), bcbank=4))
                    wk0 = max(0, i - 4)

                    def win_s(j, bk):
                        kb = wk0 + j
                        fns = []
                        first = True
                        if kb == i:
                            fns.append(lambda e: e.matmul(pb[bk][:], lhsT=masks[:, 1, :], rhs=ident[:], start=True, stop=False))
                            first = False
                        if kb == i - 4:
                            fns.append(lambda e, first=first: e.matmul(pb[bk][:], lhsT=masks[:, 2, :], rhs=ident[:], start=first, stop=False))
                            first = False
                        fns.append(lambda e, first=first: e.matmul(pb[bk][:], lhsT=KTWs[:, kb * 128:(kb + 1) * 128], rhs=qc[:], start=first, stop=True))
                        return fns, ["masks", "ident", "KTWs", qcr]
                    attn_units(U, i - wk0 + 1, win_s, lambda j: V3s[2][:, wk0 + j, :], "V3s2")

                    def u_end():
                        attn_finish(pb[6], "pb6", None, None, (rrow, bcs, (oacc, False)), gate=(grow[pi][64:65, 2, :], gres), bcbank=4)
                        P.op("act", lambda e: e.activation(out=o_st[:, 2, :], in_=oacc[:], func=AF.Copy), reads=["oacc"], writes=[ostr])
                    U.append(u_end)
                    return U

                def store_o(i):
                    pi = i % 2
                    ts = slice(i * 128, (i + 1) * 128)
                    P.dma("sp", OT.ap()[:, :, ts].rearrange("(b h) d t -> d b h t", b=3), ost[pi][:].rearrange("d b (h t) -> d b h t", h=4),
                          reads=["ost%d" % pi], writes=[], key="ost%d" % pi)

                def run_merged(chains):
                    items = []
                    for ci, U in enumerate(chains):
                        P.capture = []
                        for f in U:
                            f()
                        ops_ = P.capture
                        P.capture = None
                        n = len(ops_)
                        for k, it_ in enumerate(ops_):
                            items.append(((k + 0.5) / n, ci, k, it_))
                    items.sort(key=lambda t: (t[0], t[1], t[2]))
                    for _, _, _, it_ in items:
                        P.commit(it_)

                load_block(0)
                run_merged([chain_P(0)])
                for i in range(NB):
                    if i + 1 < NB:
                        load_block(i + 1)
                    chains = [chain_SB(i), chain_ATT(i)]
                    if i + 1 < NB:
                        chains.append(chain_P(i + 1))
                    run_merged(chains)
                    store_o(i)
                P.barrier()
                P.emit_all()
            if debug == "B":
                break
            with ExitStack() as ec:
                sb = lambda n, s, d: ec.enter_context(nc.sbuf_tensor(n + "_L%d" % li, list(s), d))
                alloc_psum(ec, "C%d" % li, 7)
                wup = sb("wup", [64, 12, D], BF16); wout = sb("wout", [128, 8, D], BF16)
                wmq = sb("wmq", [128, 8, 256], BF16); wmo = sb("wmo", [64, 4, D], BF16); wmkv = sb("wmkv", [128, 8, 512], BF16)
                gq = sb("gq", [128, D], F32); gkv = sb("gkv", [128, D], F32)
                KmT = sb("KmT", [64, 4, NMEM], BF16); Vm = sb("Vm", [128, 2, 4, 65], BF16)
                oT = sb("oT", [64, 12, 512], BF16); gT = sb("gT", [128, 24, 512], BF16)
                mT = sb("mT", [128, 8, 512], BF16)
                tm = [sb("tm%d" % i, [128, 512], F32) for i in range(3)]
                hin = [sb("hin%d" % i, [128, D], F32) for i in range(2)]
                h1 = sb("h1", [128, 4, D], F32)
                junk = sb("junkC", [128, D], F32); ubf = sb("ubfC", [128, D], BF16)
                uT2 = sb("uT2", [128, 8, 512], BF16)
                qmT = sb("qmT", [64, 4, 512], BF16)
                Eb = [sb("EbC%d" % i, [128, 512], BF16) for i in range(2)]
                omT = sb("omT", [64, 4, 512], BF16)
                rrow = sb("rrowC", [128, 512], F32); bcs = sb("bcsC", [64, 512], F32)
                hout = [sb("houtC%d" % i, [128, D], F32) for i in range(2)]
                memb = sb("memb", [128, D], F32); mTm = sb("mTm", [128, 8, NMEM], BF16)
                P.dma("pool", wup[:], wup_d.ap()[l].rearrange("b (h d) n -> d (b h) n", d=64), writes=["wup"], key="c1w0")
                P.dma("pool", wout[:], wout_d.ap()[l].rearrange("(k p) n -> p k n", p=128), writes=["wout"], key="c1w1")
                P.dma("pool", wmq[:], wmq_d.ap()[l].rearrange("(k p) n -> p k n", p=128), writes=["wmq"], key="c1w2")
                P.dma("pool", wmo[:], wmo_d.ap()[l].rearrange("(h d) n -> d h n", d=64), writes=["wmo"], key="c1w3")
                P.dma("pool", wmkv[:], wmkv_d.ap()[l].rearrange("(k p) n -> p k n", p=128), writes=["wmkv"], key="c1w4")
                P.dma("sp", gq[:], bcrow(norms_d, 4 * l + 1), writes=["gq"], key="c1g0")
                P.dma("sp", gkv[:], bcrow(norms_d, 4 * l + 2), writes=["gkv"], key="c1g1")
                P.op("pool", lambda e: e.memset(Vm[:], 1.0), writes=["Vm"])
                for mc in range(2):
                    P.dma("sp", memb[:], mem_d.ap()[mc * 128:(mc + 1) * 128, :], writes=["memb"], key="c1mem")
                    rmsnorm_T(memb[:], "memb", gkv, "gkv", mTm[:, :, mc * 128:(mc + 1) * 128], "mTm", (junk, ubf))
                for h in range(4):
                    P.op("pe", [(lambda e, kc=kc, h=h: e.matmul(pb[0][0:64, 0:NMEM], lhsT=wmkv[:, kc, h * 64:(h + 1) * 64], rhs=mTm[:, kc, :],
                                                                start=(kc == 0), stop=(kc == 7))) for kc in range(8)], reads=["wmkv", "mTm"], writes=["pb0"])
                    P.op("act", lambda e, h=h: e.activation(out=KmT[:, h, :], in_=pb[0][0:64, 0:NMEM], func=AF.Copy), reads=["pb0"], writes=["KmT"])
                for mc in range(2):
                    P.op("pe", [(lambda e, kc=kc, mc=mc: e.matmul(pb[1][:, 0:256], lhsT=mTm[:, kc, mc * 128:(mc + 1) * 128], rhs=wmkv[:, kc, 256:512],
                                                                  start=(kc == 0), stop=(kc == 7))) for kc in range(8)], reads=["wmkv", "mTm"], writes=["pb1"])
                    P.op("act", lambda e, mc=mc: e.activation(out=Vm[:, mc, :, 0:64], in_=pb[1][:, 0:256].rearrange("p (h d) -> p h d", h=4), func=AF.Copy),
                         reads=["pb1"], writes=["Vm"])
                for g in range(NG):
                    gs = slice(g * 512, (g + 1) * 512)
                    P.dma("sp", oT[:], OT.ap()[:, :, gs].rearrange("c d s -> d c s"), reads=["scrB"], writes=["oT"], key="c1oT")
                    P.dma("sp", gT[:], GT.ap()[:, gs].rearrange("(c p) s -> p c s", p=128), reads=["scrA"], writes=["gT"], key="c1gT")
                    for fc in range(8):
                        for br in range(3):
                            P.op("pe", [(lambda e, h=h, br=br, fc=fc: e.matmul(pb[br][:], lhsT=wup[:, br * 4 + h, fc * 128:(fc + 1) * 128], rhs=oT[:, br * 4 + h, :],
                                                                                start=(h == 0), stop=(h == 3))) for h in range(4)], reads=["wup", "oT"], writes=["pb%d" % br])
                            P.op("dve", lambda e, br=br, fc=fc: e.tensor_tensor(out=tm[br][:], in0=pb[br][:], in1=gT[:, br * 8 + fc, :], op=ALU.mult),
                                 reads=["pb%d" % br, "gT"], writes=["tm%d" % br])
                        P.op("pool", lambda e: e.tensor_tensor(out=tm[0][:], in0=tm[0][:], in1=tm[1][:], op=ALU.add), reads=["tm0", "tm1"], writes=["tm0"])
                        P.op("pool", lambda e, fc=fc: e.tensor_tensor(out=mT[:, fc, :], in0=tm[0][:], in1=tm[2][:], op=ALU.add), reads=["tm0", "tm2"], writes=["mT"])
                    for b4 in range(4):
                        blk = g * 4 + b4
                        hi_ = hin[blk % 2]
                        P.dma("sp", hi_[:], h_in.ap()[blk * 128:(blk + 1) * 128, :], writes=["hin%d" % (blk % 2)], key="hin%d" % (blk % 2))
                        for half in range(2):
                            bk = 3 + half
                            P.op("pe", [(lambda e, kc=kc, half=half, bk=bk, b4=b4: e.matmul(pb[bk][:], lhsT=mT[:, kc, b4 * 128:(b4 + 1) * 128], rhs=wout[:, kc, half * 512:(half + 1) * 512],
                                                                                          start=(kc == 0), stop=(kc == 7))) for kc in range(8)], reads=["mT", "wout"], writes=["pb%d" % bk])
                            P.op("dve", lambda e, half=half, bk=bk, b4=b4, hi_=hi_: e.tensor_tensor(out=h1[:, b4, half * 512:(half + 1) * 512], in0=pb[bk][:], in1=hi_[:, half * 512:(half + 1) * 512], op=ALU.add),
                                 reads=["pb%d" % bk, "hin%d" % (blk % 2)], writes=["h1_%d" % b4])
                        rmsnorm_T(h1[:, b4, :], "h1_%d" % b4, gq, "gq", uT2[:, :, b4 * 128:(b4 + 1) * 128], "uT2", (junk, ubf))
                    for h in range(4):
                        bk = h % 2
                        P.op("pe", [(lambda e, kc=kc, h=h, bk=bk: e.matmul(pb[bk][0:64, :], lhsT=wmq[:, kc, h * 64:(h + 1) * 64], rhs=uT2[:, kc, :], start=(kc == 0), stop=(kc == 7)))
                                    for kc in range(8)], reads=["wmq", "uT2"], writes=["pb%d" % bk])
                        P.op("act", lambda e, h=h, bk=bk: e.activation(out=qmT[:, h, :], in_=pb[bk][0:64, :], func=AF.Copy), reads=["pb%d" % bk], writes=["qmT"])
                    for h in range(4):
                        for mc in range(2):
                            bk = mc
                            P.op("pe", lambda e, h=h, mc=mc, bk=bk: e.matmul(pb[bk][:], lhsT=KmT[:, h, mc * 128:(mc + 1) * 128], rhs=qmT[:, h, :], start=True, stop=True),
                                 reads=["KmT", "qmT"], writes=["pb%d" % bk])
                            P.op("act", lambda e, mc=mc, bk=bk: e.activation(out=Eb[mc][:], in_=pb[bk][:], func=AF.Exp, scale=0.125), reads=["pb%d" % bk], writes=["EbC%d" % mc])
                            P.op("pe", lambda e, h=h, mc=mc: e.matmul(pb[5][0:65, :], lhsT=Vm[:, mc, h, :], rhs=Eb[mc][:], start=(mc == 0), stop=(mc == 1)),
                                 reads=["Vm", "EbC%d" % mc], writes=["pb5"])
                        attn_finish(pb[5], "pb5", omT[:, h, :], "omT", (rrow, bcs, None))
                    for b4 in range(4):
                        blk = g * 4 + b4
                        ho = hout[blk % 2]
                        for half in range(2):
                            bk = 3 + half
                            P.op("pe", [(lambda e, h=h, half=half, bk=bk, b4=b4: e.matmul(pb[bk][:], lhsT=omT[:, h, b4 * 128:(b4 + 1) * 128], rhs=wmo[:, h, half * 512:(half + 1) * 512],
                                                                                        start=(h == 0), stop=(h == 3))) for h in range(4)], reads=["omT", "wmo"], writes=["pb%d" % bk])
                            P.op("dve", lambda e, half=half, bk=bk, b4=b4, ho=ho: e.tensor_tensor(out=ho[:, half * 512:(half + 1) * 512], in0=pb[bk][:], in1=h1[:, b4, half * 512:(half + 1) * 512], op=ALU.add),
                                 reads=["pb%d" % bk, "h1_%d" % b4], writes=["hout%d" % (blk % 2)])
                        P.dma("sp", HM.ap()[blk * 128:(blk + 1) * 128, :], ho[:], reads=["hout%d" % (blk % 2)], writes=[], key="hout%d" % (blk % 2))
                P.barrier()
                P.emit_all()
            if debug == "C1":
                break
            with ExitStack() as ef:
                sb = lambda n, s, d: ef.enter_context(nc.sbuf_tensor(n + "_L%d" % li, list(s), d))
                alloc_psum(ef, "F%d" % li, 7)
                wfi = sb("wfi", [128, 8, 2 * DFF], BF16); wfo = sb("wfo", [128, 22, D], BF16)
                gf = sb("gf", [128, D], F32); gfin = sb("gfin", [128, D], F32)
                hmb2 = [sb("hmb%d" % i_, [128, 2, D], F32) for i_ in range(2)]
                junk = sb("junkF", [128, D], F32); ubf = sb("ubfF", [128, D], BF16)
                uT32 = [sb("uT3_%d" % i_, [128, 8, 256], BF16) for i_ in range(2)]
                sil = [sb("sil%d" % i, [128, 256], F32) for i in range(2)]
                actT = sb("actT", [128, 22, 256], BF16)
                hf = [sb("hf%d" % i, [128, D], F32) for i in range(2)]
                for kc in range(8):
                    P.dma("pool", wfi[:, kc, :], wfi_d.ap()[l, kc * 128:(kc + 1) * 128, :], writes=["wfi"], key="c2w%d" % kc)
                P.dma("pool", wfo[:, 0:11, :], wfo_d.ap()[l, 0:11 * 128, :].rearrange("(c p) n -> p c n", p=128), writes=["wfo"], key="c2wo0")
                P.dma("pool", wfo[:, 11:22, :], wfo_d.ap()[l, 11 * 128:22 * 128, :].rearrange("(c p) n -> p c n", p=128), writes=["wfo"], key="c2wo1")
                P.dma("sp", gf[:], bcrow(norms_d, 4 * l + 3), writes=["gf"], key="c2g0")
                P.dma("sp", gfin[:], bcrow(norms_d, 4 * L), writes=["gfin"], key="c2g1")
                def norm_ops(g2):
                    par = g2 % 2
                    for b2 in range(2):
                        blk = g2 * 2 + b2
                        hres = "hmb%d_%d" % (par, b2)
                        P.dma("sp", hmb2[par][:, b2, :], HM.ap()[blk * 128:(blk + 1) * 128, :], writes=[hres], key=hres)
                        rmsnorm_T(hmb2[par][:, b2, :], hres, gf, "gf", uT32[par][:, :, b2 * 128:(b2 + 1) * 128], "uT3_%d" % par, (junk, ubf))

                def cloop_ops(g2):
                    par = g2 % 2
                    uT3 = uT32[par]
                    ures = "uT3_%d" % par

                    def a_ops(c):
                        bk = c % 2
                        P.op("pe", [(lambda e, kc=kc: e.matmul(pb[bk][:, 0:256], lhsT=wfi[:, kc, c * 128:(c + 1) * 128], rhs=uT3[:, kc, :], start=(kc == 0), stop=(kc == 7)))
                                    for kc in range(8)] +
                                   [(lambda e, kc=kc: e.matmul(pb[bk][:, 256:512], lhsT=wfi[:, kc, DFF + c * 128:DFF + (c + 1) * 128], rhs=uT3[:, kc, :], start=False, stop=(kc == 7)))
                                    for kc in range(8)], reads=["wfi", ures], writes=["pb%d" % bk])
                    a_ops(0)
                    for c in range(22):
                        bk = c % 2
                        if c + 1 < 22:
                            a_ops(c + 1)
                        P.op("act", lambda e: e.activation(out=sil[bk][:], in_=pb[bk][:, 0:256], func=AF.Silu), reads=["pb%d" % bk], writes=["sil%d" % bk])
                        P.op("dve", lambda e: e.tensor_tensor(out=actT[:, c, :], in0=pb[bk][:, 256:512], in1=sil[bk][:], op=ALU.mult),
                             reads=["pb%d" % bk, "sil%d" % bk], writes=["actT%d" % c])
                        for b2 in range(2):
                            for half in range(2):
                                ob = 2 + b2 * 2 + half
                                P.op("pe", lambda e: e.matmul(pb[ob][:], lhsT=actT[:, c, b2 * 128:(b2 + 1) * 128], rhs=wfo[:, c, half * 512:(half + 1) * 512],
                                                              start=(c == 0), stop=(c == 21)), reads=["actT%d" % c, "wfo"], writes=["pb%d" % ob])

                def merge_run(thunks):
                    items = []
                    for ci, f in enumerate(thunks):
                        P.capture = []
                        f()
                        ops_ = P.capture
                        P.capture = None
                        n_ = len(ops_)
                        for k, it_ in enumerate(ops_):
                            items.append(((k + 0.5) / n_, ci, k, it_))
                    items.sort(key=lambda t: (t[0], t[1], t[2]))
                    for _, _, _, it_ in items:
                        P.commit(it_)

                norm_ops(0)
                for g2 in range(S // 256):
                    th = [lambda: cloop_ops(g2)]
                    if g2 + 1 < S // 256:
                        th.append(lambda: norm_ops(g2 + 1))
                    merge_run(th)
                    hmb = hmb2[g2 % 2]
                    for b2 in range(2):
                        blk = g2 * 2 + b2
                        hft = hf[b2]
                        for half in range(2):
                            ob = 2 + b2 * 2 + half
                            P.op("dve", lambda e, ob=ob, half=half, b2=b2, hft=hft: e.tensor_tensor(out=hft[:, half * 512:(half + 1) * 512], in0=pb[ob][:], in1=hmb[:, b2, half * 512:(half + 1) * 512], op=ALU.add),
                                 reads=["pb%d" % ob, "hmb%d_%d" % (g2 % 2, b2)], writes=["hf%d" % b2])
                        if not last:
                            P.dma("sp", HO.ap()[blk * 128:(blk + 1) * 128, :], hft[:], reads=["hf%d" % b2], writes=[], key="hf%d" % b2)
                        else:
                            P.op("act", lambda e, hft=hft: e.activation(out=junk[:], in_=hft[:], func=AF.Square, accum_out=stat[:, 4:5]), reads=["hf%d" % b2], writes=["junk", "stat4"])
                            P.op("act", lambda e: e.activation(out=stat[:, 5:6], in_=stat[:, 4:5], func=AF.Sqrt, scale=1.0 / D, bias=1e-6), reads=["stat4"], writes=["stat5"])
                            P.op("dve", lambda e: e.reciprocal(out=stat[:, 6:7], in_=stat[:, 5:6]), reads=["stat5"], writes=["stat6"])
                            P.op("dve", lambda e, hft=hft: e.scalar_tensor_tensor(out=hft[:], in0=hft[:], scalar=stat[:, 6:7], in1=gfin[:], op0=ALU.mult, op1=ALU.mult),
                                 reads=["hf%d" % b2, "stat6", "gfin"], writes=["hf%d" % b2])
                            P.dma("sp", out_d.ap()[blk * 128:(blk + 1) * 128, :], hft[:], reads=["hf%d" % b2], writes=[], key="hf%d" % b2)
                P.barrier()
                P.emit_all()
    return nc


NBIS = 16

_CACHE = {}


def prep_shared(inputs, S, layers):
    cols = _col_index()
    sh = {}
    w_in = np.asarray(inputs['w_in'], np.float32)
    sh['wA'] = np.ascontiguousarray(np.stack([w_in[l][:, cols] for l in layers], 0))
    for k in ('cmp_w1_k', 'cmp_w2_k', 'cmp_pos_k', 'cmp_w1_v', 'cmp_w2_v', 'cmp_pos_v', 'w_up', 'w_out',
              'w_mem_q', 'w_mem_kv', 'w_mem_o', 'w_ffn_in', 'w_ffn_out'):
        a = np.asarray(inputs[k], np.float32)
        sh[k] = np.ascontiguousarray(np.stack([a[l] for l in layers], 0))
    rows = []
    for l in layers:
        rows += [np.asarray(inputs[k], np.float32)[l] for k in ('norm_mix', 'norm_mem_q', 'norm_mem_kv', 'norm_ffn')]
    rows.append(np.asarray(inputs['norm_final'], np.float32))
    sh['norms'] = np.ascontiguousarray(np.stack(rows, 0))
    sh.update(make_consts(S))
    return sh


def kernel(**inputs):
    x = np.asarray(inputs['x'], np.float32)
    mem = np.asarray(inputs['mem'], np.float32)
    B, S, _ = x.shape
    layers = [0, 1]
    key = (S, tuple(layers))
    if key not in _CACHE:
        _CACHE[key] = build(S, layers)
    nc = _CACHE[key]
    sh = prep_shared(inputs, S, layers)
    in_maps = []
    for b in range(B):
        m = dict(sh)
        m['x'] = np.ascontiguousarray(x[b])
        m['mem'] = np.ascontiguousarray(mem[b])
        in_maps.append(m)
    res = run_bass_kernel_spmd(nc, in_maps, core_ids=list(range(B)))
    return np.stack([np.asarray(r['out'], np.float32) for r in res.results], 0)
```
